# Optimizing a Trainium2 kernel written in Bass

```python
import math
import jax
import jax.numpy as jnp
from jax import lax
import numpy as np

D_MODEL = 1024
BATCH = 16
SEQ = 256
DEPTH = 2
DEC_BATCH = 8
DEC_SEQ = 2048
PAST_LEN = 512

F32 = jnp.float32
GRID_W = 64
EPS = 1e-6
W_A = D_MODEL // 2
S5_H = 16
S5_G = W_A // S5_H
S5_P = 64
N_DIR = 2
W_B = D_MODEL // 2
HY_ORDER = 2
HY_SHORT = 3
HY_BANDS = 16
HY_EMB = 1 + 2 * HY_BANDS
HY_FH = 64
HY_FAST_PCT = 0.3
HY_SLOW_PCT = 1.5
HY_TARGET = 1e-2
HY_SHIFT = 0.05
W_C = D_MODEL // 2
POOL_WINDOWS = (2, 4, 8, 16)
POOL_G = W_C // 4
N_BRANCH = 3
IN_SPLIT_POINTS = (W_A, 2 * W_A, 2 * W_A + (HY_ORDER + 1) * W_B, 2 * W_A + (HY_ORDER + 2) * W_B,
                   2 * W_A + (HY_ORDER + 2) * W_B + W_C, 2 * W_A + (HY_ORDER + 2) * W_B + 2 * W_C)
IN_W = 2 * W_A + (HY_ORDER + 2) * W_B + 2 * W_C + N_BRANCH * D_MODEL

kernel_name = 'hybrid_s5_hyena_pool_diffusion_step'


def rms_norm(x, g):
    xf = x.astype(F32)
    r = lax.rsqrt(jnp.mean(xf * xf, axis=-1, keepdims=True) + EPS)
    return (xf * r * g.astype(F32)).astype(x.dtype)


def modulation(cond, w, b):
    return jax.nn.silu(cond.astype(F32)) @ w.astype(F32) + b.astype(F32)


def grid_pos_embed(L):
    rows = L // GRID_W
    r = jnp.broadcast_to(jnp.arange(rows, dtype=F32)[:, None], (rows, GRID_W)).reshape(-1)
    col = jnp.broadcast_to(jnp.arange(GRID_W, dtype=F32)[None, :], (rows, GRID_W)).reshape(-1)
    q = D_MODEL // 4
    omega = 1.0 / (10000.0 ** (jnp.arange(q, dtype=F32) / q))
    ar = r[:, None] * omega[None, :]
    ac = col[:, None] * omega[None, :]
    return jnp.concatenate([jnp.sin(ar), jnp.cos(ar), jnp.sin(ac), jnp.cos(ac)], axis=-1)


def s5_discretise(lam_re, lam_im, log_dt, b_re, b_im):
    lam_re = lam_re.astype(F32)
    lam_im = lam_im.astype(F32)
    dt = jnp.exp(log_dt.astype(F32))[:, None]
    mag = jnp.exp(lam_re * dt)
    ang = lam_im * dt
    a_re = mag * jnp.cos(ang)
    a_im = mag * jnp.sin(ang)
    n_re = a_re - 1.0
    n_im = a_im
    den = lam_re * lam_re + lam_im * lam_im
    k_re = (n_re * lam_re + n_im * lam_im) / den
    k_im = (n_im * lam_re - n_re * lam_im) / den
    br = b_re.astype(F32)
    bi = b_im.astype(F32)
    bb_re = k_re[..., None] * br - k_im[..., None] * bi
    bb_im = k_re[..., None] * bi + k_im[..., None] * br
    return a_re, a_im, bb_re, bb_im


def _complex_affine_combine(e1, e2):
    a1r, a1i, b1r, b1i = e1
    a2r, a2i, b2r, b2i = e2
    return (a2r * a1r - a2i * a1i,
            a2r * a1i + a2i * a1r,
            a2r * b1r - a2i * b1i + b2r,
            a2r * b1i + a2i * b1r + b2i)


def s5_direction(u, h0_re, h0_im, a_re, a_im, bb_re, bb_im, c_re, c_im, reverse):
    L = u.shape[1]
    bu_re = jnp.einsum('blgh,gph->blgp', u, bb_re)
    bu_im = jnp.einsum('blgh,gph->blgp', u, bb_im)
    inj_re = a_re * h0_re - a_im * h0_im
    inj_im = a_re * h0_im + a_im * h0_re
    first = L - 1 if reverse else 0
    bu_re = bu_re.at[:, first].add(inj_re)
    bu_im = bu_im.at[:, first].add(inj_im)
    ar = jnp.broadcast_to(a_re, (1, L) + a_re.shape)
    ai = jnp.broadcast_to(a_im, (1, L) + a_im.shape)
    _, _, x_re, x_im = lax.associative_scan(_complex_affine_combine, (ar, ai, bu_re, bu_im),
                                            reverse=reverse, axis=1)
    y = (jnp.einsum('blgp,ghp->blgh', x_re, c_re) - jnp.einsum('blgp,ghp->blgh', x_im, c_im))
    last = 0 if reverse else L - 1
    return y, x_re[:, last], x_im[:, last]


def s5_branch(u, h0_re, h0_im, lam_re, lam_im, log_dt, b_re, b_im, c_re, c_im, d_skip, w_glu, b_glu):
    Bn, L, _ = u.shape
    uf = u.astype(F32)
    ug = uf.reshape(Bn, L, S5_G, S5_H)
    y = uf * d_skip.astype(F32)
    fin_re = []
    fin_im = []
    for d in range(N_DIR):
        a_re, a_im, bb_re, bb_im = s5_discretise(lam_re[d], lam_im[d], log_dt[d], b_re[d], b_im[d])
        yd, fr, fi = s5_direction(ug, h0_re[:, d], h0_im[:, d], a_re, a_im, bb_re, bb_im,
                                  c_re[d].astype(F32), c_im[d].astype(F32), reverse=(d == 1))
        y = y + yd.reshape(Bn, L, W_A)
        fin_re.append(fr)
        fin_im.append(fi)
    y = jax.nn.gelu(y)
    y = y * jax.nn.sigmoid(y @ w_glu.astype(F32) + b_glu.astype(F32))
    return y, jnp.stack(fin_re, axis=1), jnp.stack(fin_im, axis=1)


def hyena_filters(L, w1, b1, w2, b2, freq, w3):
    t = jnp.arange(L, dtype=F32)
    tn = t / (L - 1)
    f = jnp.linspace(1e-4, HY_BANDS - 1, HY_BANDS, dtype=F32)
    ang = (2.0 * math.pi / L) * t[:, None] * f[None, :]
    z = jnp.concatenate([tn[:, None], jnp.cos(ang), -jnp.sin(ang)], axis=-1)
    fr = freq.astype(F32)
    h = jnp.sin(fr * (z @ w1.astype(F32) + b1.astype(F32)))
    h = jnp.sin(fr * (h @ w2.astype(F32) + b2.astype(F32)))
    h = (h @ w3.astype(F32)).reshape(L, HY_ORDER, W_B)
    max_decay = math.log(HY_TARGET) / HY_FAST_PCT
    min_decay = math.log(HY_TARGET) / HY_SLOW_PCT
    deltas = jnp.abs(jnp.linspace(min_decay, max_decay, W_B, dtype=F32))
    half = L // 2
    off = jnp.abs(t - half) / half
    win = jnp.exp(-off[:, None] * deltas[None, :]) + HY_SHIFT
    return h * win[:, None, :]


def short_conv(z, w, b):
    L = z.shape[1]
    pad = HY_SHORT // 2
    zp = jnp.pad(z, ((0, 0), (pad, HY_SHORT - 1 - pad), (0, 0)))
    out = b.astype(F32)
    for k in range(HY_SHORT):
        out = out + zp[:, k:k + L] * w[k].astype(F32)
    return out


def fft_long_conv(u, h, bias):
    L = u.shape[1]
    n = 2 * L
    uf = jnp.fft.rfft(u, n=n, axis=1)
    hf = jnp.fft.rfft(h, n=n, axis=0)
    y = jnp.fft.irfft(uf * hf[None], n=n, axis=1)[:, L // 2:L // 2 + L]
    return y + u * bias.astype(F32)


def hyena_branch(u, conv_w, conv_b, w1, b1, w2, b2, freq, w3, bias):
    L = u.shape[1]
    uc = short_conv(u.astype(F32), conv_w, conv_b)
    v, x1, x2 = jnp.split(uc, HY_ORDER + 1, axis=-1)
    h = hyena_filters(L, w1, b1, w2, b2, freq, w3)
    gates = (x1, x2)
    z = v
    for o in range(HY_ORDER):
        z = gates[o] * fft_long_conv(z, h[:, o], bias[o])
    return z


def pool_branch(u, w, scale):
    Bn, L, _ = u.shape
    uf = u.astype(F32)
    cs = jnp.concatenate([jnp.zeros((Bn, 1, W_C), F32), jnp.cumsum(uf, axis=1)], axis=1)
    t = jnp.arange(L)
    outs = []
    for gi, win in enumerate(POOL_WINDOWS):
        sl = slice(gi * POOL_G, (gi + 1) * POOL_G)
        lo = jnp.clip(t - win // 2, 0, L)
        hi = jnp.clip(t - win // 2 + win, 0, L)
        csg = cs[..., sl]
        mean = (csg[:, hi] - csg[:, lo]) / (hi - lo).astype(F32)[:, None]
        outs.append((mean - uf[..., sl]) @ w[gi].astype(F32))
    return jnp.concatenate(outs, axis=-1) * scale.astype(F32)


def trunk_layer(x, mod, h0_re, h0_im, p, l):
    dt = x.dtype
    shift, scale, gate = jnp.split(mod[:, None, :], 3, axis=-1)
    h = (rms_norm(x, p['norm_g'][l]).astype(F32) * (1.0 + scale) + shift).astype(dt)
    proj = h @ p['w_in'][l]
    u_a, g_a, u_b, g_b, u_c, g_c, m = jnp.split(proj, IN_SPLIT_POINTS, axis=-1)
    y_a, st_re, st_im = s5_branch(u_a, h0_re, h0_im, p['s5_lam_re'][l], p['s5_lam_im'][l], p['s5_log_dt'][l],
                                  p['s5_b_re'][l], p['s5_b_im'][l], p['s5_c_re'][l], p['s5_c_im'][l],
                                  p['s5_d'][l], p['s5_w_glu'][l], p['s5_b_glu'][l])
    y_b = hyena_branch(u_b, p['hy_conv_w'][l], p['hy_conv_b'][l], p['hy_f_w1'][l], p['hy_f_b1'][l],
                       p['hy_f_w2'][l], p['hy_f_b2'][l], p['hy_f_freq'][l], p['hy_f_w3'][l], p['hy_bias'][l])
    y_c = pool_branch(u_c, p['pool_w'][l], p['pool_scale'][l])
    y_a = (y_a * jax.nn.silu(g_a.astype(F32))).astype(dt)
    y_b = (y_b * jax.nn.silu(g_b.astype(F32))).astype(dt)
    y_c = (y_c * jax.nn.silu(g_c.astype(F32))).astype(dt)
    m_a, m_b, m_c = jnp.split(jax.nn.sigmoid(m.astype(F32)), N_BRANCH, axis=-1)
    merged = (m_a * (y_a @ p['w_br_a'][l]).astype(F32)
              + m_b * (y_b @ p['w_br_b'][l]).astype(F32)
              + m_c * (y_c @ p['w_br_c'][l]).astype(F32))
    out = (merged.astype(dt) @ p['w_out'][l]).astype(F32)
    x_new = (x.astype(F32) + gate * out).astype(dt)
    return x_new, st_re, st_im


def setup_inputs(seed: int = 0) -> dict:
    key = jax.random.key(seed)
    ks = jax.random.split(key, 40)

    def nrm(k, shape, s):
        return jax.random.normal(k, shape, F32) * s

    D = D_MODEL
    st_shape = (DEC_BATCH, DEPTH, N_DIR, S5_G, S5_P)
    pv = (DEPTH, N_DIR, S5_G, S5_P)
    return {
        'x_prompt': nrm(ks[0], (BATCH, SEQ, D), 1.0),
        'x_sample': nrm(ks[1], (DEC_BATCH, DEC_SEQ, D), 1.0),
        'c': nrm(ks[2], (DEC_BATCH, D), 1.0),
        'state_s5_re': nrm(ks[3], st_shape, 0.5),
        'state_s5_im': nrm(ks[4], st_shape, 0.5),
        'c_ctx': nrm(ks[5], (D,), 1.0),
        'norm_g': 1.0 + nrm(ks[6], (DEPTH, D), 0.02),
        'w_mod': nrm(ks[7], (DEPTH, D, 3 * D), 0.5 * D ** -0.5),
        'b_mod': nrm(ks[8], (DEPTH, 3 * D), 0.02),
        'w_in': nrm(ks[9], (DEPTH, D, IN_W), D ** -0.5),
        's5_lam_re': -0.5 + nrm(ks[10], pv, 0.01),
        's5_lam_im': jnp.pi * jnp.arange(S5_P, dtype=F32) + nrm(ks[11], pv, 0.01),
        's5_log_dt': jax.random.uniform(ks[12], (DEPTH, N_DIR, S5_G), F32, math.log(1e-3), math.log(1e-1)),
        's5_b_re': nrm(ks[13], (DEPTH, N_DIR, S5_G, S5_P, S5_H), (2 * S5_H) ** -0.5),
        's5_b_im': nrm(ks[14], (DEPTH, N_DIR, S5_G, S5_P, S5_H), (2 * S5_H) ** -0.5),
        's5_c_re': nrm(ks[15], (DEPTH, N_DIR, S5_G, S5_H, S5_P), (2 * S5_P) ** -0.5),
        's5_c_im': nrm(ks[16], (DEPTH, N_DIR, S5_G, S5_H, S5_P), (2 * S5_P) ** -0.5),
        's5_d': nrm(ks[17], (DEPTH, W_A), 1.0),
        's5_w_glu': nrm(ks[18], (DEPTH, W_A, W_A), W_A ** -0.5),
        's5_b_glu': nrm(ks[19], (DEPTH, W_A), 0.02),
        'hy_conv_w': nrm(ks[20], (DEPTH, HY_SHORT, (HY_ORDER + 1) * W_B), HY_SHORT ** -0.5),
        'hy_conv_b': nrm(ks[21], (DEPTH, (HY_ORDER + 1) * W_B), 0.02),
        'hy_f_w1': nrm(ks[22], (DEPTH, HY_EMB, HY_FH), HY_EMB ** -0.5),
        'hy_f_b1': nrm(ks[23], (DEPTH, HY_FH), 0.02),
        'hy_f_w2': nrm(ks[24], (DEPTH, HY_FH, HY_FH), HY_FH ** -0.5),
        'hy_f_b2': nrm(ks[25], (DEPTH, HY_FH), 0.02),
        'hy_f_freq': 1.0 + nrm(ks[26], (DEPTH, HY_FH), 0.1),
        'hy_f_w3': nrm(ks[27], (DEPTH, HY_FH, HY_ORDER * W_B), 0.02),
        'hy_bias': nrm(ks[28], (DEPTH, HY_ORDER, W_B), 1.0),
        'pool_w': nrm(ks[29], (DEPTH, len(POOL_WINDOWS), POOL_G, POOL_G), POOL_G ** -0.5),
        'pool_scale': 1.0 + nrm(ks[30], (DEPTH, W_C), 0.1),
        'w_br_a': nrm(ks[31], (DEPTH, W_A, D), W_A ** -0.5),
        'w_br_b': nrm(ks[32], (DEPTH, W_B, D), W_B ** -0.5),
        'w_br_c': nrm(ks[33], (DEPTH, W_C, D), W_C ** -0.5),
        'w_out': nrm(ks[34], (DEPTH, D, D), D ** -0.5),
        'final_g': 1.0 + nrm(ks[35], (D,), 0.02),
    }


def reference(x_prompt, x_sample, c, state_s5_re, state_s5_im, c_ctx, norm_g, w_mod, b_mod, w_in,
              s5_lam_re, s5_lam_im, s5_log_dt, s5_b_re, s5_b_im, s5_c_re, s5_c_im, s5_d, s5_w_glu, s5_b_glu,
              hy_conv_w, hy_conv_b, hy_f_w1, hy_f_b1, hy_f_w2, hy_f_b2, hy_f_freq, hy_f_w3, hy_bias,
              pool_w, pool_scale, w_br_a, w_br_b, w_br_c, w_out, final_g):
    p = {'norm_g': norm_g, 'w_in': w_in,
         's5_lam_re': s5_lam_re, 's5_lam_im': s5_lam_im, 's5_log_dt': s5_log_dt,
         's5_b_re': s5_b_re, 's5_b_im': s5_b_im, 's5_c_re': s5_c_re, 's5_c_im': s5_c_im,
         's5_d': s5_d, 's5_w_glu': s5_w_glu, 's5_b_glu': s5_b_glu,
         'hy_conv_w': hy_conv_w, 'hy_conv_b': hy_conv_b, 'hy_f_w1': hy_f_w1, 'hy_f_b1': hy_f_b1,
         'hy_f_w2': hy_f_w2, 'hy_f_b2': hy_f_b2, 'hy_f_freq': hy_f_freq, 'hy_f_w3': hy_f_w3,
         'hy_bias': hy_bias, 'pool_w': pool_w, 'pool_scale': pool_scale,
         'w_br_a': w_br_a, 'w_br_b': w_br_b, 'w_br_c': w_br_c, 'w_out': w_out}

    xc = x_prompt
    zeros_state = jnp.zeros((x_prompt.shape[0], N_DIR, S5_G, S5_P), F32)
    st_re_layers = []
    st_im_layers = []
    for l in range(DEPTH):
        mod_ctx = modulation(c_ctx[None, :], w_mod[l], b_mod[l])
        xc, sr, si = trunk_layer(xc, mod_ctx, zeros_state, zeros_state, p, l)
        st_re_layers.append(sr)
        st_im_layers.append(si)
    y_prompt = rms_norm(xc, final_g)
    new_s5_re = jnp.stack(st_re_layers, axis=1)
    new_s5_im = jnp.stack(st_im_layers, axis=1)

    L = x_sample.shape[1]
    xs = (x_sample.astype(F32) + grid_pos_embed(L)[None]).astype(x_sample.dtype)
    for l in range(DEPTH):
        mod_lat = modulation(c, w_mod[l], b_mod[l])
        xs, _, _ = trunk_layer(xs, mod_lat, state_s5_re[:, l].astype(F32), state_s5_im[:, l].astype(F32), p, l)
    y_sample = rms_norm(xs, final_g)

    return (y_prompt, y_sample, new_s5_re, new_s5_im)
```

```python
import math
from contextlib import ExitStack

import numpy as np
import concourse.bass as bass
import concourse.mybir as mybir
from concourse.bass_utils import run_bass_kernel_spmd

F32 = mybir.dt.float32
BF16 = mybir.dt.bfloat16
I32 = mybir.dt.int32
AF = mybir.ActivationFunctionType
ALU = mybir.AluOpType

NCORES = 8
D = 1024
KT = D // 128
DEPTH = 2
LS = 2048
LP = 256
T = LS + 2 * LP
NTB = T // 512
IN_W = 7168
EPS = 1e-6
S5_G = 32
S5_P = 64
S5_H = 16
R1N = 32256
MAGIC = 12582912.0
TWO_PI_S = 2 * math.pi * (1 - 1e-6)
HY_SHIFT = 0.05

C_UA, C_GA, C_UB, C_GB, C_UC, C_GC, C_M = 0, 512, 1024, 2560, 3072, 3584, 4096


class StopBuild(Exception):
    pass


class Sched:
    def __init__(self, nc, es, n_dma_sems=24):
        self.nc = nc
        self.eng = {"pe": nc.tensor, "dve": nc.vector, "act": nc.scalar, "pool": nc.gpsimd, "sp": nc.sync}
        self.sem = {k: es.enter_context(nc.semaphore("s_" + k)) for k in self.eng}
        self.cnt = {k: 0 for k in self.eng}
        self.dsem = [es.enter_context(nc.semaphore("d%d" % i)) for i in range(n_dma_sems)]
        self.dcnt = [0] * n_dma_sems
        self.dnext = 0
        self.semobj = {}
        for k in self.eng:
            self.semobj[("e", k)] = self.sem[k]
        for i, s in enumerate(self.dsem):
            self.semobj[("d", i)] = s
        self.waited = {k: {} for k in self.eng}
        self.last_w = {}
        self.readers = {}
        self.nwaits = 0

    def _deps(self, e, r, w):
        need = {}

        def add(ev, self_ok):
            if ev is None:
                return
            sid, val = ev
            if sid == ("e", e) and not self_ok:
                return
            if need.get(sid, 0) < val:
                need[sid] = val

        for k in r:
            add(self.last_w.get(k), e != "pe")
        for k in w:
            add(self.last_w.get(k), False)
            for ev in self.readers.get(k, {}).items():
                add(ev, False)
        wt = self.waited[e]
        for sid, val in need.items():
            if wt.get(sid, 0) >= val:
                continue
            self.eng[e].wait_ge(self.semobj[sid], val)
            wt[sid] = val
            self.nwaits += 1

    def _commit(self, ev, r, w):
        sid, val = ev
        for k in w:
            self.last_w[k] = ev
            self.readers[k] = {}
        for k in r:
            d = self.readers.setdefault(k, {})
            if d.get(sid, 0) < val:
                d[sid] = val

    def op(self, e, fn, r=(), w=()):
        self._deps(e, r, w)
        ins = fn(self.eng[e])
        self.cnt[e] += 1
        ins.then_inc(self.sem[e], 1)
        self._commit((("e", e), self.cnt[e]), r, w)
        return ins

    def dma(self, out, in_, r=(), w=(), q=None, **kw):
        if q is None:
            q = "pool" if type(out.tensor).__name__ == "DRamTensorHandle" else "sp"
        i = self.dnext
        self.dnext = (self.dnext + 1) % len(self.dsem)
        wt = self.waited[q]
        if self.dcnt[i] > 0 and wt.get(("d", i), 0) < self.dcnt[i]:
            self.eng[q].wait_ge(self.dsem[i], self.dcnt[i])
            wt[("d", i)] = self.dcnt[i]
        self._deps(q, r, w)
        ins = self.eng[q].dma_start(out=out, in_=in_, **kw)
        self.dcnt[i] += 16
        ins.then_inc(self.dsem[i], 16)
        self._commit((("d", i), self.dcnt[i]), r, w)
        return ins

    def barrier(self):
        for e in self.eng:
            wt = self.waited[e]
            for k in self.eng:
                if k != e and self.cnt[k] > wt.get(("e", k), 0):
                    self.eng[e].wait_ge(self.sem[k], self.cnt[k])
                    wt[("e", k)] = self.cnt[k]
            for i, s in enumerate(self.dsem):
                if self.dcnt[i] > wt.get(("d", i), 0):
                    self.eng[e].wait_ge(s, self.dcnt[i])
                    wt[("d", i)] = self.dcnt[i]

    def finish(self):
        for i, s in enumerate(self.dsem):
            if self.dcnt[i] > 0:
                self.eng["sp"].wait_ge(s, self.dcnt[i])
        for k in ("pe", "dve", "act", "pool"):
            if self.cnt[k] > 0:
                self.eng["sp"].wait_ge(self.sem[k], self.cnt[k])


class Builder:
    def __init__(self, dbg=None, parts="tpfba"):
        self.dbg = dbg or {}
        self.parts = parts
        self.nc = bass.Bass("TRN2", target_bir_lowering=False)
        self.es = ExitStack()
        self.uid = 0

    def dram_in(self, name, shape, dt=F32):
        return self.nc.dram_tensor(name, list(shape), dt, kind="ExternalInput").ap()

    def dram_out(self, name, shape, dt=F32):
        return self.nc.dram_tensor(name, list(shape), dt, kind="ExternalOutput").ap()

    def dram_tmp(self, name, shape, dt=F32):
        return self.nc.dram_tensor(name, list(shape), dt, kind="Internal").ap()

    def sb(self, name, shape, dt=F32):
        return self.es.enter_context(self.nc.sbuf_tensor(name, list(shape), dt))

    def build(self):
        with self.es:
            self._build()
        return self.nc

    def _build(self):
        nc = self.nc
        S = self.S = Sched(nc, self.es)
        op, dma = S.op, S.dma

        xin = self.dram_in("xin", [T, D])
        cond = self.dram_in("cond", [2, D])
        st_re = self.dram_in("st_re", [DEPTH, 2, S5_G, S5_P])
        st_im = self.dram_in("st_im", [DEPTH, 2, S5_G, S5_P])
        W = {}
        for name, shape in WEIGHT_SHAPES.items():
            W[name] = self.dram_in(name, shape)
        yout = self.dram_out("yout", [T, D])
        ns_re = self.dram_out("ns_re", [2, DEPTH, 2, S5_G, S5_P])
        ns_im = self.dram_out("ns_im", [2, DEPTH, 2, S5_G, S5_P])
        xsp = self.dram_tmp("xsp", [128, KT * T])
        xsp3 = xsp.rearrange("p (k t) -> p k t", k=KT)
        dbg_out = {}
        for name, shape in self.dbg.items():
            dbg_out[name] = self.dram_out("dbg_" + name, shape)

        R1 = self.sb("R1", [128, R1N], F32)
        Hh = self.sb("Hh", [128, KT * T], BF16)
        h3 = Hh[:].rearrange("p (k t) -> p k t", k=KT)
        x3 = R1[:, 0:KT * T].rearrange("p (k t) -> p k t", k=KT)
        R1b = R1[:].bitcast(BF16)
        Yv = [R1b[:, i * 4 * T:(i + 1) * 4 * T].rearrange("p (k t) -> p k t", k=4) for i in range(3)]
        SCR0 = 3 * 4 * T // 2
        stage = [self.sb("stage%d" % i, [128, 2048], F32) for i in range(2)]
        self.stage_i = 0
        wbuf = [self.sb("wbuf%d" % i, [128, 5120], BF16) for i in range(2)]
        self.wbuf_i = 0
        self.wbuf = wbuf
        self.R1 = R1
        self.slots = [wbuf[0][:, 0:4096], wbuf[1][:, 0:4096], stage[0][:].bitcast(BF16), stage[1][:].bitcast(BF16)]
        self.slot_keys = [("wbuf", 0), ("wbuf", 1), ("stage", 0), ("stage", 1)]
        self._slot_i = 0
        ident = self.sb("ident", [128, 128], F32)
        identb = self.sb("identb", [128, 128], BF16)
        onesm = self.sb("onesm", [128, 128], F32)
        iot = self.sb("iot", [128, 128], I32)
        modT = self.sb("modT", [128, 24, 2], F32)
        gmul = self.sb("gmul", [128, KT, 2], F32)
        bmod = self.sb("bmod", [128, 24], F32)
        normg = self.sb("normg", [128, KT], F32)
        condT = self.sb("condT", [128, KT, 2], F32)
        rstd = R1[:, 22528:22528 + T]
        smallv = self.sb("smallv", [128, 64], F32)
        xtm = [R1[:, 20480 + i * 1024:20480 + (i + 1) * 1024] for i in range(2)]
        self._sq = [R1[:, 25088 + i * 512:25088 + i * 512 + 256].bitcast(BF16) for i in range(4)]
        PS = [self.es.enter_context(nc.psum_tensor("ps%d" % i, [128, 512], F32)) for i in range(8)]
        self.ps_i = 0

        def psk(i):
            return ("ps", i)

        MODBANK = 7

        def next_ps():
            i = self.ps_i
            self.ps_i = (i + 1) % 7
            return i

        op("pool", lambda e: e.iota(iot[:], pattern=[[1, 128]], base=0, channel_multiplier=-1), w=["iot"])
        op("dve", lambda e: e.tensor_scalar(out=ident[:], in0=iot[:], scalar1=0, scalar2=None, op0=ALU.is_equal),
           r=["iot"], w=["ident"])
        op("dve", lambda e: e.tensor_copy(out=identb[:], in_=ident[:]), r=["ident"], w=["identb"])
        op("pool", lambda e: e.memset(onesm[:], 1.0 / D), w=["onesm"])
        self._onesb = self.sb("onesb", [128, 128], BF16)
        op("dve", lambda e: e.tensor_copy(out=self._onesb[:], in_=onesm[:]), r=["onesm"], w=["onesm"])

        def wload(dst, src, key, eng="act"):
            i = self.stage_i
            self.stage_i = (i + 1) % len(stage)
            n = 1
            for s in dst.shape[1:]:
                n *= s
            assert n <= 2048
            st = stage[i][:, 0:n]
            if len(dst.shape) == 3:
                st = st.rearrange("p (a b) -> p a b", a=dst.shape[1])
            dma(st, src, w=[("stage", i)])
            wk_ = list(key) if isinstance(key, list) else [key]
            if eng == "act":
                op("act", lambda e: e.activation(out=dst, in_=st, func=AF.Copy), r=[("stage", i)], w=wk_)
            else:
                op(eng, lambda e: e.tensor_copy(out=dst, in_=st), r=[("stage", i)], w=wk_)

        def wdirect(dst, src, key):
            dma(dst, src, w=[key])

        with nc.allow_non_contiguous_dma(reason="small transposed vector loads"):
            for j in range(2):
                dma(condT[:, :, j], cond[j].rearrange("(k p) -> p k", p=128), w=["condT"])
        op("act", lambda e: e.activation(out=condT[:], in_=condT[:], func=AF.Silu), r=["condT"], w=["condT"])

        def mod_start(l):
            for mc in range(12):
                i = self.stage_i
                self.stage_i = (i + 1) % len(stage)
                st = stage[i][:].rearrange("p (k c) -> p k c", k=KT)
                dma(st, W["w_mod"][l][:, mc * 256:(mc + 1) * 256].rearrange("(k p) c -> p k c", p=128),
                    w=[("stage", i)])
                for mm in range(2):
                    m = mc * 2 + mm
                    for kt in range(KT):
                        op("pe", lambda e, m=m, mm=mm, kt=kt, st=st: e.matmul(
                            PS[MODBANK][:, m * 2:m * 2 + 2], st[:, kt, mm * 128:(mm + 1) * 128], condT[:, kt, :],
                            start=(kt == 0), stop=(kt == KT - 1)),
                           r=[("stage", i), "condT"], w=[psk(MODBANK)])
        self.mod_start = mod_start
        mod_start(0)

        if "t" in self.parts:
            self._setup_tables()
        S.barrier()
        for tt in range(T // 128):
            b = tt % 2
            dma(xtm[b][:], xin[tt * 128:(tt + 1) * 128, :], w=[("xtm", b)])
            for half in range(2):
                pi = next_ps()
                for j in range(4):
                    kt = half * 4 + j
                    op("pe", lambda e, kt=kt, j=j, pi=pi: e.transpose(PS[pi][:, j * 128:(j + 1) * 128],
                                                                     xtm[b][:, kt * 128:(kt + 1) * 128], ident[:]),
                       r=[("xtm", b), "ident"], w=[psk(pi)])
                eng = "act" if half == 0 else "dve"
                src = PS[pi][:].rearrange("p (j t) -> p j t", j=4)
                dst = x3[:, half * 4:half * 4 + 4, tt * 128:(tt + 1) * 128]
                if eng == "act":
                    op("act", lambda e, dst=dst, src=src: e.activation(out=dst, in_=src, func=AF.Copy),
                       r=[psk(pi)], w=[("x", tt // 4)])
                else:
                    op("dve", lambda e, dst=dst, src=src: e.tensor_copy(out=dst, in_=src),
                       r=[psk(pi)], w=[("x", tt // 4)])

        if "p" in self.parts:
            self._pos_embed(x3)

        for tb in range(NTB):
            dma(xsp3[:, :, tb * 512:(tb + 1) * 512], x3[:, :, tb * 512:(tb + 1) * 512],
                r=[("x", tb)], w=[("xsp", f, tb) for f in range(KT)])


        for l in range(DEPTH):
            with nc.allow_non_contiguous_dma(reason="small transposed vector loads"):
                dma(bmod[:], W["b_mod"][l].rearrange("(m p) -> p m", p=128), w=["bmod"])
                dma(normg[:], W["norm_g"][l].rearrange("(k p) -> p k", p=128), w=["normg"])
            op("dve", lambda e: e.tensor_tensor(out=modT[:], in0=PS[MODBANK][:, 0:48].rearrange("p (m j) -> p m j", j=2),
                                                in1=bmod[:].unsqueeze(2).broadcast_to([128, 24, 2]), op=ALU.add),
               r=[psk(MODBANK), "bmod"], w=["modT"])
            op("dve", lambda e: e.tensor_scalar(out=gmul[:], in0=modT[:, 8:16, :], scalar1=1.0, scalar2=None,
                                                op0=ALU.add), r=["modT"], w=["gmul"])
            op("dve", lambda e: e.tensor_tensor(out=gmul[:], in0=gmul[:],
                                                in1=normg[:].unsqueeze(2).broadcast_to([128, KT, 2]), op=ALU.mult),
               r=["gmul", "normg"], w=["gmul"])

            if l > 0:
                for tb in range(NTB):
                    dma(x3[:, :, tb * 512:(tb + 1) * 512], xsp3[:, :, tb * 512:(tb + 1) * 512],
                        r=[("xsp", f, tb) for f in range(KT)], w=[("x", tb)])
            self._norm_to(x3, rstd, onesm, PS, next_ps, psk)
            for kt in range(KT):
                for (t0, t1, j) in ((0, LS, 0), (LS, T, 1)):
                    keys = [("x", tb) for tb in range(t0 // 512, (t1 + 511) // 512)]
                    hkeys = [("h", kt, tb) for tb in range(t0 // 512, (t1 + 511) // 512)]
                    op("dve", lambda e, kt=kt, t0=t0, t1=t1, j=j: e.scalar_tensor_tensor(
                        out=x3[:, kt, t0:t1], in0=x3[:, kt, t0:t1], scalar=gmul[:, kt, j:j + 1], op0=ALU.mult,
                        in1=rstd[:, t0:t1], op1=ALU.mult), r=keys + ["gmul", "rstd"], w=keys)
                    op("act", lambda e, kt=kt, t0=t0, t1=t1, j=j: e.activation(
                        out=h3[:, kt, t0:t1], in_=x3[:, kt, t0:t1], func=AF.Identity,
                        bias=modT[:, kt, j:j + 1], scale=1.0), r=keys + ["modT"], w=hkeys)
            if "h%d" % l in dbg_out:
                hf = self.sb("dbg_hf%d" % l, [128, T], F32)
                for kt in range(KT):
                    op("dve", lambda e, kt=kt: e.tensor_copy(out=hf[:], in_=h3[:, kt, :]),
                       r=[("h", kt, tb) for tb in range(NTB)], w=["dbg_hf"])
                    dma(dbg_out["h%d" % l][kt * 128:(kt + 1) * 128, :], hf[:], r=["dbg_hf"])

            def proj(col0, ntiles, evac):
                for m0 in range(0, ntiles, 2):
                    nm = min(2, ntiles - m0)
                    bi = self.wbuf_i
                    self.wbuf_i = (bi + 1) % 2
                    wv = wbuf[bi][:, 0:KT * nm * 128].rearrange("p (k c) -> p k c", k=KT)
                    c0 = col0 + m0 * 128
                    wload(wv, W["w_in"][l][:, c0:c0 + nm * 128].rearrange("(k p) c -> p k c", p=128), ("wbuf", bi))
                    for mm in range(nm):
                        for tb in range(NTB):
                            pi = next_ps()
                            for kt in range(KT):
                                op("pe", lambda e, pi=pi, kt=kt, mm=mm, tb=tb, wv=wv: e.matmul(
                                    PS[pi][:], wv[:, kt, mm * 128:(mm + 1) * 128], h3[:, kt, tb * 512:(tb + 1) * 512],
                                    start=(kt == 0), stop=(kt == KT - 1)),
                                   r=[("wbuf", bi), ("h", kt, tb)], w=[psk(pi)])
                            evac(m0 + mm, tb, pi)

            self.ctx = dict(l=l, W=W, op=op, dma=dma, PS=PS, next_ps=next_ps, psk=psk, proj=proj, wload=wload,
                            Yv=Yv, R1=R1, R1b=R1b, SCR0=SCR0, h3=h3, wbuf=wbuf, stage=stage, smallv=smallv,
                            ident=ident, identb=identb, dbg_out=dbg_out, modT=modT, x3=x3, xsp3=xsp3,
                            st_re=st_re, st_im=st_im, ns_re=ns_re, ns_im=ns_im)
            S.barrier()
            self._branch_c()
            S.barrier()
            self._branch_b()
            S.barrier()
            self._branch_a()
            S.barrier()
            self._phase2()
            S.barrier()

        S.barrier()
        self._final(x3, xsp3, rstd, onesm, PS, next_ps, psk, W, yout, xtm, ident)
        S.finish()

    def _norm_to(self, x3, rstd, onesm, PS, next_ps, psk):
        op = self.S.op
        sq = self._sq
        if not hasattr(self, "_epsb"):
            self._epsb = self.sb("epsb", [128, 1], F32)
            op("pool", lambda e: e.memset(self._epsb[:], EPS), w=["epsb"])
        si = 0
        for tb in range(NTB):
            pi = next_ps()
            for kt in range(KT):
                b = si % 4
                si += 1
                op("act", lambda e, b=b, kt=kt, tb=tb: e.activation(out=sq[b][:], in_=x3[:, kt, tb * 512:(tb + 1) * 512],
                                                                    func=AF.Square), r=[("x", tb)], w=[("sq", b)])
                op("pe", lambda e, b=b, kt=kt, pi=pi: e.matmul(PS[pi][:], self._onesb[:], sq[b][:], start=(kt == 0),
                                                               stop=(kt == KT - 1)),
                   r=[("sq", b), "onesm"], w=[psk(pi)])
            op("act", lambda e, pi=pi, tb=tb: e.activation(out=rstd[:, tb * 512:(tb + 1) * 512], in_=PS[pi][:],
                                                           func=AF.Sqrt, bias=self._epsb[:], scale=1.0),
               r=[psk(pi), "epsb"], w=["rstd"])
        op("dve", lambda e: e.reciprocal(out=rstd[:], in_=rstd[:]), r=["rstd"], w=["rstd"])

    def _branch_c(self):
        c = self.ctx
        op, dma, PS, next_ps, psk, W, l = c["op"], c["dma"], c["PS"], c["next_ps"], c["psk"], c["W"], c["l"]
        R1, SCR0, Yv = c["R1"], c["SCR0"], c["Yv"]
        nc = self.nc
        PAD = 8
        TP = T + 6 * PAD
        seqs = [(0, LS), (LS, LP), (LS + LP, LP)]
        offs = []
        o = 0
        for (t0, ln) in seqs:
            offs.append(o + PAD)
            o += ln + 2 * PAD
        ucp = R1[:, SCR0:SCR0 + 4 * TP].rearrange("p (k t) -> p k t", k=4)
        o2 = SCR0 + 4 * TP
        tmpw = R1[:, o2:o2 + TP]
        o2 += TP
        dmean = R1[:, o2:o2 + T // 2].bitcast(BF16)
        o2 += T // 2
        sg = Yv[2]
        assert o2 <= R1N
        if not hasattr(self, "_rcb"):
            self._rcb = self.sb("rcb", [128, 4, 2, 8], F32)
            i8 = self.sb("i8", [128, 8], F32)
            op("pool", lambda e: e.iota(i8[:], pattern=[[1, 8]], base=0, channel_multiplier=0,
                                        allow_small_or_imprecise_dtypes=True), w=["i8"])
            for k in range(4):
                win = 2 << k
                op("dve", lambda e, k=k, win=win: e.tensor_scalar(out=self._rcb[:, k, 0, :], in0=i8[:], scalar1=float(win // 2),
                                                                scalar2=float(win), op0=ALU.add, op1=ALU.min),
                   r=["i8"], w=["rcb"])
                op("dve", lambda e, k=k, win=win: e.tensor_scalar(out=self._rcb[:, k, 1, :], in0=i8[:], scalar1=-1.0,
                                                                scalar2=float(8 + win // 2), op0=ALU.mult, op1=ALU.add),
                   r=["i8"], w=["rcb"])
                op("dve", lambda e, k=k, win=win: e.tensor_scalar(out=self._rcb[:, k, 1, :], in0=self._rcb[:, k, 1, :],
                                                                scalar1=float(win), scalar2=None, op0=ALU.min),
                   r=["rcb"], w=["rcb"])
            op("dve", lambda e: e.reciprocal(out=self._rcb[:], in_=self._rcb[:]), r=["rcb"], w=["rcb"])
        rcb = self._rcb
        smallv = c["smallv"]
        with nc.allow_non_contiguous_dma(reason="small transposed vector loads"):
            dma(smallv[:, 0:4], W["pool_scale"][l].rearrange("(k p) -> p k", p=128), w=["pool_scale"])
        op("dve", lambda e: e.memset(ucp[:], 0.0), w=["ucp"])

        def evac_u(mi, tb, pi):
            if tb < 4:
                dst = ucp[:, mi, offs[0] + tb * 512: offs[0] + (tb + 1) * 512]
                op("act", lambda e: e.activation(out=dst, in_=PS[pi][:], func=AF.Copy), r=[psk(pi)], w=["ucp"])
            else:
                for s in range(2):
                    dst = ucp[:, mi, offs[1 + s]: offs[1 + s] + LP]
                    op("act", lambda e, s=s, dst=dst: e.activation(out=dst, in_=PS[pi][:, s * LP:(s + 1) * LP], func=AF.Copy),
                       r=[psk(pi)], w=["ucp"])

        def evac_g(mi, tb, pi):
            op("act", lambda e: e.activation(out=sg[:, mi, tb * 512:(tb + 1) * 512], in_=PS[pi][:], func=AF.Silu),
               r=[psk(pi)], w=[("y", 2, mi, tb)])

        c["proj"](C_UC, 4, evac_u)
        c["proj"](C_GC, 4, evac_g)
        pw = c["wbuf"][self.wbuf_i]
        bi = self.wbuf_i
        self.wbuf_i = (bi + 1) % 2
        pwv = pw[:, 0:512].rearrange("p (g c) -> p g c", g=4)
        c["wload"](pwv, W["pool_w"][l].rearrange("g p c -> p g c"), ("wbuf", bi))
        for gi in range(4):
            win = 2 << gi
            u = ucp[:, gi, :]
            cur = u
            n = TP
            sh = 1
            for lev in range(gi + 1):
                n2 = n - sh
                eng = "dve"
                src = cur
                op(eng, lambda e, src=src, sh=sh, n2=n2: e.tensor_tensor(out=tmpw[:, 0:n2], in0=src[:, 0:n2],
                                                                         in1=src[:, sh:sh + n2], op=ALU.add),
                   r=["ucp", "tmpw"], w=["tmpw"])
                cur = tmpw
                n = n2
                sh *= 2
            for si, (t0, ln) in enumerate(seqs):
                a = offs[si] - win // 2
                op("dve", lambda e, a=a, t0=t0, ln=ln, si=si: e.scalar_tensor_tensor(
                    out=dmean[:, t0:t0 + ln], in0=tmpw[:, a:a + ln], scalar=1.0 / win, op0=ALU.mult,
                    in1=u[:, offs[si]:offs[si] + ln], op1=ALU.subtract), r=["tmpw", "ucp"], w=["dmean"])
                for side, p0 in ((0, 0), (1, ln - 8)):
                    tt = self._tmp8 if hasattr(self, "_tmp8") else None
                    if tt is None:
                        tt = self._tmp8 = self.sb("tmp8", [128, 8], F32)
                    op("dve", lambda e, a=a, p0=p0, side=side: e.tensor_tensor(
                        out=tt[:], in0=tmpw[:, a + p0:a + p0 + 8], in1=rcb[:, gi, side, :], op=ALU.mult),
                       r=["tmpw", "rcb", "tmp8"], w=["tmp8"])
                    op("dve", lambda e, p0=p0, t0=t0, si=si: e.tensor_tensor(
                        out=dmean[:, t0 + p0:t0 + p0 + 8], in0=tt[:], in1=u[:, offs[si] + p0:offs[si] + p0 + 8],
                        op=ALU.subtract), r=["tmp8", "ucp", "dmean"], w=["dmean"])
            for tb in range(NTB):
                pi = next_ps()
                op("pe", lambda e, pi=pi, tb=tb: e.matmul(PS[pi][:], pwv[:, gi, :], dmean[:, tb * 512:(tb + 1) * 512],
                                                          start=True, stop=True),
                   r=[("wbuf", bi), "dmean"], w=[psk(pi)])
                op("dve", lambda e, pi=pi, tb=tb: e.scalar_tensor_tensor(
                    out=Yv[2][:, gi, tb * 512:(tb + 1) * 512], in0=PS[pi][:], scalar=smallv[:, gi:gi + 1], op0=ALU.mult,
                    in1=sg[:, gi, tb * 512:(tb + 1) * 512], op1=ALU.mult),
                   r=[psk(pi), "pool_scale", ("y", 2, gi, tb)], w=[("y", 2, gi, tb)])
        if "yc%d" % l in c["dbg_out"]:
            self._dump_bf16(Yv[2], c["dbg_out"]["yc%d" % l], [("y", 2, gi, tb) for gi in range(4) for tb in range(NTB)])

    def _dump_bf16(self, src3, dst, keys):
        op, dma = self.S.op, self.S.dma
        if not hasattr(self, "_dbgf"):
            self._dbgf = self.sb("dbgf", [128, T], F32)
        for k in range(src3.shape[1]):
            op("dve", lambda e, k=k: e.tensor_copy(out=self._dbgf[:], in_=src3[:, k, :]), r=keys + ["dbgf"], w=["dbgf"])
            dma(dst[k * 128:(k + 1) * 128, :], self._dbgf[:], r=["dbgf"], w=["dbgo"])

    def _zero_y(self, i):
        op = self.S.op
        Yv = self.ctx["Yv"]
        op("pool", lambda e: e.memset(Yv[i][:], 0.0), w=[("y", i, gi, tb) for gi in range(4) for tb in range(NTB)])

    def _branch_b(self):
        self._zero_y(1)

    def _branch_a(self):
        try:
            self._branch_a_impl()
        except StopBuild:
            self.S.barrier()
            self._zero_y(0)

    def _ka(self, n):
        import os
        if int(os.environ.get("KA_STOP", "99")) == n:
            raise StopBuild()

    def _branch_a_impl(self):
        c = self.ctx
        if "a" not in self.parts:
            self._zero_y(0)
            return
        op, dma, PS, next_ps, psk, W, l = c["op"], c["dma"], c["PS"], c["next_ps"], c["psk"], c["W"], c["l"]
        R1, SCR0, Yv, ident, identb = c["R1"], c["SCR0"], c["Yv"], c["ident"], c["identb"]
        nc, S = self.nc, self.S
        NCH = T // 8
        CP = NCH
        SEQC = [(1, 256, 0), (259, 32, 256), (293, 32, 288)]
        if not hasattr(self, "_uaD"):
            self._uaD = self.dram_tmp("uaD", [4, 128, 8 * NCH], BF16)
            self._y2D = self.dram_tmp("y2D", [2, 128, 16 * CP], BF16)
            self._s5m = self.sb("s5m", [128, 256], BF16)
            self._s5n = self.sb("s5n", [128, 16], F32)
            msk = self._s5m
            ii = R1[:, SCR0:SCR0 + 130].bitcast(I32)
            thr = R1[:, SCR0 + 256:SCR0 + 258]
            colf = R1[:, SCR0 + 512:SCR0 + 640]
            op("pool", lambda e: e.iota(ii[:, 0:1], pattern=[[0, 1]], base=0, channel_multiplier=1), w=["s5i"])
            op("dve", lambda e: e.tensor_scalar(out=ii[:, 0:1], in0=ii[:, 0:1], scalar1=4, scalar2=None, op0=ALU.arith_shift_right),
               r=["s5i"], w=["s5i"])
            op("dve", lambda e: e.tensor_scalar(out=ii[:, 0:1], in0=ii[:, 0:1], scalar1=4, scalar2=None, op0=ALU.logical_shift_left),
               r=["s5i"], w=["s5i"])
            op("pool", lambda e: e.iota(ii[:, 2:130], pattern=[[1, 128]], base=0, channel_multiplier=0), r=["s5i"], w=["s5i"])
            op("dve", lambda e: e.tensor_copy(out=thr[:, 0:1], in_=ii[:, 0:1]), r=["s5i"], w=["s5thr"])
            op("dve", lambda e: e.tensor_scalar(out=thr[:, 1:2], in0=thr[:, 0:1], scalar1=15.0, scalar2=None, op0=ALU.add),
               r=["s5thr"], w=["s5thr"])
            op("dve", lambda e: e.tensor_copy(out=colf, in_=ii[:, 2:130]), r=["s5i"], w=["s5col"])
            op("dve", lambda e: e.tensor_scalar(out=msk[:, 0:128], in0=colf, scalar1=thr[:, 0:1], scalar2=None, op0=ALU.is_ge),
               r=["s5col", "s5thr"], w=["s5mask"])
            op("dve", lambda e: e.tensor_scalar(out=msk[:, 128:256], in0=colf, scalar1=thr[:, 1:2], scalar2=None, op0=ALU.is_le),
               r=["s5col", "s5thr"], w=["s5mask"])
            op("pool", lambda e: e.iota(self._s5n[:], pattern=[[1, 16]], base=-7, channel_multiplier=0,
                                        allow_small_or_imprecise_dtypes=True), w=["s5nvec"])
            S.barrier()
        maskf, maskb, nvec = self._s5m[:, 0:128], self._s5m[:, 128:256], self._s5n[:]
        uaD, y2D = self._uaD, self._y2D

        o = 0
        uaj = [R1[:, o + i * 1280:o + (i + 1) * 1280].bitcast(BF16).rearrange("p (s c) -> p s c", s=8) for i in range(2)]
        o += 2560

        def evac_u(mi, tb, pi):
            dst = uaj[mi % 2][:, :, tb * 64:(tb + 1) * 64]
            src = PS[pi][:].rearrange("p (c s) -> p s c", s=8)
            if tb % 2:
                op("act", lambda e: e.activation(out=dst, in_=src, func=AF.Copy), r=[psk(pi)], w=[("uaj", mi % 2)])
            else:
                op("dve", lambda e: e.tensor_copy(out=dst, in_=src), r=[psk(pi)], w=[("uaj", mi % 2)])
            if tb == NTB - 1:
                dma(uaD[mi].rearrange("p (s c) -> p s c", s=8), uaj[mi % 2], r=[("uaj", mi % 2)], w=[("uaD", mi)])
        c["proj"](C_UA, 4, evac_u)

        self._ka(1)
        o = 2560
        Ap = R1[:, o:o + 512].rearrange("p (r x n) -> p r x n", r=2, x=16); o += 512
        BB = R1[:, o:o + 512].rearrange("p (r x h) -> p r x h", r=2, x=16); o += 512
        CT = R1[:, o:o + 512].rearrange("p (r x h) -> p r x h", r=2, x=16); o += 512
        sm = R1[:, o:o + 512]; o += 512
        Braw = R1[:, o:o + 512].rearrange("p (r x h) -> p r x h", r=2, x=16); o += 512
        assert o <= 5120
        o = 0
        t1 = R1[:, o:o + 1024]; o += 1024
        t2 = R1[:, o:o + 1024]; o += 1024
        CN = R1[:, o:o + 128]; o += 128
        cur = R1[:, o:o + 32].rearrange("p (r d g) -> p r d g", r=2, d=2); o += 32
        curP = R1[:, o:o + 32].rearrange("p (r d g) -> p r d g", r=2, d=2); o += 32
        T1 = R1[:, o:o + 32].rearrange("p (r d g) -> p r d g", r=2, d=2); o += 32
        T2 = R1[:, o:o + 32].rearrange("p (r d g) -> p r d g", r=2, d=2); o += 32
        ARt = R1[:, o:o + 32].rearrange("p (r d g) -> p r d g", r=2, d=2); o += 32
        AIt = R1[:, o:o + 32].rearrange("p (r d g) -> p r d g", r=2, d=2); o += 32
        h0 = R1[:, o:o + 32].rearrange("p (r d g) -> p r d g", r=2, d=2); o += 32
        assert o <= 2560
        o = SCR0
        U2 = R1[:, o:o + 8 * CP].bitcast(BF16).rearrange("p (g c) -> p g c", g=16); o += 8 * CP
        Sx = R1[:, o:o + 16 * CP].bitcast(BF16).rearrange("p (r d g c) -> p r d g c", r=2, d=2, g=8); o += 16 * CP
        BS = R1[:, o:o + 2048].bitcast(BF16).rearrange("p (r x k) -> p r x k", r=2, x=16); o += 2048
        MI = R1[:, o:o + 1024].bitcast(BF16).rearrange("p (g k) -> p g k", g=16); o += 1024
        oXS = o
        xs_ = [R1[:, o + i * 320:o + (i + 1) * 320] for i in range(2)]; o += 640
        oTG = o
        tg_ = [R1[:, o + i * 320:o + (i + 1) * 320] for i in range(2)]; o += 640
        oRB = o
        RR = R1[:, o:o + 2048].bitcast(BF16).rearrange("p (r x k) -> p r x k", r=2, x=16); o += 2048
        BST = R1[:, o:o + 2048].bitcast(BF16).rearrange("p (r x q) -> p r x q", r=2, x=32); o += 2048
        assert o <= R1N, o
        Y2 = R1[:, oRB:oRB + 8 * CP].bitcast(BF16).rearrange("p (g c) -> p g c", g=16)
        yj = Yv[0].rearrange("p k (t c) -> p k t c", t=8)
        smv = lambda i: sm[:, i * 16:(i + 1) * 16]

        def swapped(ap4):
            a = ap4.ap
            return bass.AP(ap4.tensor, ap4.offset + a[1][0], [list(a[0]), [-a[1][0], 2]] + [list(x) for x in a[2:]])

        def cmul(out_re, out_im, a_re, a_im, b_re, b_im, shape, rk, wk, neg_im=False):
            n = 1
            for v in shape[1:]:
                n *= v
            names = "abcd"[:len(shape) - 1]
            pat = "p (%s) -> p %s" % (" ".join(names), " ".join(names))
            kw = {names[i]: shape[i + 1] for i in range(len(names) - 1)}
            v1 = t1[:, 0:n].rearrange(pat, **kw) if len(shape) > 2 else t1[:, 0:n]
            v2 = t2[:, 0:n].rearrange(pat, **kw) if len(shape) > 2 else t2[:, 0:n]
            op("dve", lambda e: e.tensor_tensor(out=v1, in0=a_re, in1=b_re, op=ALU.mult), r=rk + ["t1"], w=["t1"])
            op("dve", lambda e: e.tensor_tensor(out=v2, in0=a_im, in1=b_im, op=ALU.mult), r=rk + ["t2"], w=["t2"])
            op("dve", lambda e: e.tensor_tensor(out=out_re, in0=v1, in1=v2, op=ALU.subtract), r=["t1", "t2"], w=wk)
            op("dve", lambda e: e.tensor_tensor(out=v1, in0=a_re, in1=b_im, op=ALU.mult), r=rk + ["t1"], w=["t1"])
            op("dve", lambda e: e.tensor_tensor(out=v2, in0=a_im, in1=b_re, op=ALU.mult), r=rk + ["t2"], w=["t2"])
            if neg_im:
                op("dve", lambda e: e.scalar_tensor_tensor(out=out_im, in0=v1, scalar=-1.0, op0=ALU.mult, in1=v2, op1=ALU.subtract),
                   r=["t1", "t2"], w=wk)
            else:
                op("dve", lambda e: e.tensor_tensor(out=out_im, in0=v1, in1=v2, op=ALU.add), r=["t1", "t2"], w=wk)

        with nc.allow_non_contiguous_dma(reason="small transposed vector loads"):
            dma(c["smallv"][:, 4:8], W["s5_d"][l].rearrange("(k p) -> p k", p=128), w=["s5dv"])
            dma(c["smallv"][:, 60:64], W["s5_b_glu"][l].rearrange("(k p) -> p k", p=128), w=["s5bg"])
        Dcol = self._dcol if hasattr(self, "_dcol") else None
        if Dcol is None:
            Dcol = self._dcol = self.sb("dcol", [128, 32], F32)
        with nc.allow_non_contiguous_dma(reason="small transposed vector loads"):
            for s_ in range(8):
                dma(Dcol[s_ * 16:(s_ + 1) * 16, :], W["s5_d"][l].rearrange("(g h) -> h g", h=16), w=["dcol"])

        for hf in range(2):
            G0 = 16 * hf
            S.barrier()
            with nc.allow_non_contiguous_dma(reason="small transposed parameter loads"):
                for d in range(2):
                    xs = slice(d * 8, (d + 1) * 8)
                    for gp in range(2):
                        pq = slice(gp * 64, (gp + 1) * 64)
                        dma(smv(0)[pq, xs], W["s5_lam_re"][l][d, G0:G0 + 16, :].rearrange("(g8 gp) p -> gp p g8", gp=2)[gp], w=["s5p"])
                        dma(smv(1)[pq, xs], W["s5_lam_im"][l][d, G0:G0 + 16, :].rearrange("(g8 gp) p -> gp p g8", gp=2)[gp], w=["s5p"])
                        dma(smv(2)[pq, xs], W["s5_log_dt"][l][d, G0:G0 + 16].rearrange("(g8 gp) -> gp g8", gp=2)[gp].partition_broadcast(64),
                            w=["s5p"])
                        for ri, nm in enumerate(("s5_b_re", "s5_b_im")):
                            dma(Braw[pq, ri, xs, :], W[nm][l][d, G0:G0 + 16].rearrange("(g8 gp) p h -> gp p g8 h", gp=2)[gp], w=["s5B"])
                        for ri, st in enumerate((c["st_re"], c["st_im"])):
                            dma(h0[pq, ri, d, :], st[l, d, G0:G0 + 16, :].rearrange("(g8 gp) p -> gp p g8", gp=2)[gp], w=["s5h0"])
            for d in range(2):
                for ri, nm in enumerate(("s5_c_re", "s5_c_im")):
                    for k2 in range(2):
                        srcC = W[nm][l][d, G0 + 8 * k2:G0 + 8 * k2 + 8].rearrange("g h p -> (g h) p")
                        dma(CN[:, 0:64], srcC, w=["CN"])
                        dma(CN[:, 64:128], srcC, w=["CN"])
                        pi = next_ps()
                        op("pe", lambda e: e.transpose(PS[pi][:, 0:128], CN, ident[:]), r=["CN", "ident"], w=[psk(pi)])
                        pv = PS[pi][:, 0:128].rearrange("p (j gp h) -> p j gp h", gp=2, h=16)
                        for gp in range(2):
                            op("dve", lambda e: e.tensor_copy(out=CT[gp * 64:(gp + 1) * 64, ri, d * 8 + 4 * k2:d * 8 + 4 * k2 + 4, :],
                                                              in_=pv[gp * 64:(gp + 1) * 64, :, gp, :]), r=[psk(pi)], w=["CT"])
            self._ka(2)
            op("act", lambda e: e.activation(out=smv(2), in_=smv(2), func=AF.Exp), r=["s5p"], w=["s5dt"])
            op("dve", lambda e: e.tensor_tensor(out=smv(3), in0=smv(0), in1=smv(2), op=ALU.mult), r=["s5p", "s5dt"], w=["s5lrd"])
            op("dve", lambda e: e.scalar_tensor_tensor(out=smv(4), in0=smv(1), scalar=1.0 / (2 * math.pi), op0=ALU.mult,
                                                       in1=smv(2), op1=ALU.mult), r=["s5p", "s5dt"], w=["s5th"])
            EA = t1[:, 0:256].rearrange("p (x n) -> p x n", x=16)
            UA = t2[:, 0:256].rearrange("p (x n) -> p x n", x=16)
            UC = t1[:, 256:512].rearrange("p (x n) -> p x n", x=16)
            TT = t2[:, 256:512].rearrange("p (x n) -> p x n", x=16)
            nb_ = nvec.unsqueeze(1).broadcast_to([128, 16, 16])
            op("dve", lambda e: e.tensor_tensor(out=EA, in0=smv(3).unsqueeze(2).broadcast_to([128, 16, 16]), in1=nb_, op=ALU.mult),
               r=["s5lrd", "s5nvec", "t1"], w=["t1"])
            op("dve", lambda e: e.tensor_tensor(out=UA, in0=smv(4).unsqueeze(2).broadcast_to([128, 16, 16]), in1=nb_, op=ALU.mult),
               r=["s5th", "s5nvec", "t2"], w=["t2"])
            op("act", lambda e: e.activation(out=EA, in_=EA, func=AF.Exp), r=["t1"], w=["t1"])
            op("dve", lambda e: e.tensor_scalar(out=UC, in0=UA, scalar1=0.25, scalar2=None, op0=ALU.add), r=["t2", "t1"], w=["t1b"])
            self.sin_turns(Ap[:, 1], UA, TT, ["t2"], ["ApS"], "t2b")
            self.sin_turns(Ap[:, 0], UC, TT, ["t1b"], ["ApC"], "t2b")
            op("dve", lambda e: e.tensor_tensor(out=Ap[:, 0], in0=Ap[:, 0], in1=EA, op=ALU.mult), r=["ApC", "t1"], w=["Ap"])
            op("dve", lambda e: e.tensor_tensor(out=Ap[:, 1], in0=Ap[:, 1], in1=EA, op=ALU.mult), r=["ApS", "t1", "Ap"], w=["Ap"])
            self._ka(3)
            a_re, a_im = Ap[:, 0, :, 8], Ap[:, 1, :, 8]
            op("dve", lambda e: e.tensor_scalar(out=smv(5), in0=a_re, scalar1=-1.0, scalar2=None, op0=ALU.add), r=["Ap"], w=["s5k"])
            op("dve", lambda e: e.tensor_tensor(out=smv(6), in0=smv(0), in1=smv(0), op=ALU.mult), r=["s5p"], w=["s5k6"])
            op("dve", lambda e: e.tensor_tensor(out=smv(7), in0=smv(1), in1=smv(1), op=ALU.mult), r=["s5p"], w=["s5k7"])
            op("dve", lambda e: e.tensor_tensor(out=smv(6), in0=smv(6), in1=smv(7), op=ALU.add), r=["s5k6", "s5k7"], w=["s5k6"])
            op("dve", lambda e: e.reciprocal(out=smv(6), in_=smv(6)), r=["s5k6"], w=["s5k6"])
            op("dve", lambda e: e.tensor_tensor(out=smv(7), in0=smv(5), in1=smv(0), op=ALU.mult), r=["s5k", "s5p", "s5k6"], w=["s5k7"])
            op("dve", lambda e: e.tensor_tensor(out=smv(8), in0=a_im, in1=smv(1), op=ALU.mult), r=["Ap", "s5p"], w=["s5k8"])
            op("dve", lambda e: e.tensor_tensor(out=smv(7), in0=smv(7), in1=smv(8), op=ALU.add), r=["s5k7", "s5k8"], w=["s5k7"])
            op("dve", lambda e: e.tensor_tensor(out=smv(9), in0=smv(7), in1=smv(6), op=ALU.mult), r=["s5k7", "s5k6"], w=["s5kre"])
            op("dve", lambda e: e.tensor_tensor(out=smv(7), in0=a_im, in1=smv(0), op=ALU.mult), r=["Ap", "s5p", "s5kre"], w=["s5k7"])
            op("dve", lambda e: e.tensor_tensor(out=smv(8), in0=smv(5), in1=smv(1), op=ALU.mult), r=["s5k", "s5p", "s5k7"], w=["s5k8"])
            op("dve", lambda e: e.tensor_tensor(out=smv(7), in0=smv(7), in1=smv(8), op=ALU.subtract), r=["s5k7", "s5k8"], w=["s5k7"])
            op("dve", lambda e: e.tensor_tensor(out=smv(10), in0=smv(7), in1=smv(6), op=ALU.mult), r=["s5k7", "s5k6"], w=["s5kim"])
            bc = lambda v: v.unsqueeze(2).broadcast_to([128, 16, 16])
            cmul(BB[:, 0], BB[:, 1], bc(smv(9)), bc(smv(10)), Braw[:, 0], Braw[:, 1], [128, 16, 16], ["s5kre", "s5kim", "s5B"], ["BB"])
            self._ka(4)
            def pw(ri, d, start, step):
                a = Ap[:, ri, d * 8:(d + 1) * 8, :]
                aa = a.ap
                return bass.AP(a.tensor, a.offset + start * aa[2][0], [list(aa[0]), list(aa[1]), [step * aa[2][0], 8], [0, 16]])
            for d in range(2):
                xs = slice(d * 8, (d + 1) * 8)
                bsv = lambda ri: BS[:, ri, xs, :].rearrange("p g (s h) -> p g s h", s=8)
                rrv = lambda ri: RR[:, ri, xs, :].rearrange("p g (s h) -> p g s h", s=8)
                bbv = lambda ri: BB[:, ri, xs, :].unsqueeze(2).broadcast_to([128, 8, 8, 16])
                ctv = lambda ri: CT[:, ri, xs, :].unsqueeze(2).broadcast_to([128, 8, 8, 16])
                st_b, sp_b = (14, -1) if d == 0 else (7, 1)
                st_r, sp_r = (0, 1) if d == 0 else (7, -1)
                cmul(bsv(0), bsv(1), pw(0, d, st_b, sp_b), pw(1, d, st_b, sp_b), bbv(0), bbv(1), [128, 8, 8, 16],
                     ["Ap", "BB"], [("BS", d)])
                cmul(rrv(0), rrv(1), pw(0, d, st_r, sp_r), pw(1, d, st_r, sp_r), ctv(0), ctv(1), [128, 8, 8, 16],
                     ["Ap", "CT"], [("RR", d)], neg_im=True)
            self._ka(5)
            Dd = t1[:, 0:512]
            for gp in range(2):
                ps_ = slice(gp * 64, (gp + 1) * 64)
                for b4 in range(2):
                    pf, pb = next_ps(), next_ps()
                    for gi in range(4):
                        g8 = b4 * 4 + gi
                        for d, pp in ((0, pf), (1, pb)):
                            for ri in range(2):
                                op("pe", lambda e: e.matmul(PS[pp][:, gi * 128:(gi + 1) * 128], BS[ps_, ri, d * 8 + g8, :],
                                                            RR[ps_, ri, d * 8 + g8, :], start=(ri == 0), stop=(ri == 1)),
                                   r=[("BS", d), ("RR", d)], w=[psk(pp)])
                    mk = lambda m: m.unsqueeze(1).broadcast_to([128, 4, 128])
                    pv = lambda pp: PS[pp][:].rearrange("p (g k) -> p g k", g=4)
                    tv = lambda t: t[:, 0:512].rearrange("p (g k) -> p g k", g=4)
                    op("dve", lambda e: e.tensor_tensor(out=tv(t2), in0=pv(pf), in1=mk(maskf), op=ALU.mult), r=[psk(pf), "s5mask", "t2", "t2b"], w=["t2"])
                    op("dve", lambda e: e.tensor_tensor(out=tv(t1), in0=pv(pb), in1=mk(maskb), op=ALU.mult), r=[psk(pb), "s5mask", "t1", "t1b"], w=["t1"])
                    op("dve", lambda e: e.tensor_tensor(out=tv(t2), in0=tv(t2), in1=tv(t1), op=ALU.add), r=["t1", "t2"], w=["t2"])
                    for gi in range(4):
                        g = 2 * (b4 * 4 + gi) + gp
                        op("dve", lambda e: e.scalar_tensor_tensor(out=MI[:, g, :], in0=ident[:], scalar=Dcol[:, G0 + g:G0 + g + 1],
                                                                   op0=ALU.mult, in1=t2[:, gi * 128:(gi + 1) * 128], op1=ALU.add),
                           r=["t2", "dcol", "ident"], w=[("MI", g)])
            self._ka(6)
            ei = 0
            for ri in range(2):
                for d in range(2):
                    for gp in range(2):
                        ps_ = slice(gp * 64, (gp + 1) * 64)
                        pi = next_ps()
                        pvb = PS[pi][:].bitcast(BF16)
                        for g8 in range(8):
                            op("pe", lambda e: e.transpose(pvb[:, g8 * 64:(g8 + 1) * 64], BS[ps_, ri, d * 8 + g8, :], identb[ps_, ps_]),
                               r=[("BS", d), "identb"], w=[psk(pi)])
                        bdst = BST[:, ri, d * 16:(d + 1) * 16, :].rearrange("p (g8 gp) q -> p g8 gp q", gp=2)[:, :, gp, :]
                        bsrc = pvb[:, 0:512].rearrange("p (g q) -> p g q", g=8)
                        ei += 1
                        if ei % 2:
                            op("act", lambda e: e.activation(out=bdst, in_=bsrc, func=AF.Copy), r=[psk(pi)], w=["BST"])
                        else:
                            op("dve", lambda e: e.tensor_copy(out=bdst, in_=bsrc), r=[psk(pi)], w=["BST"])
            self._ka(7)
            a8r = lambda d: Ap[:, 0, d * 8:(d + 1) * 8, 15].unsqueeze(2).broadcast_to([128, 8, 128])
            a8i = lambda d: Ap[:, 1, d * 8:(d + 1) * 8, 15].unsqueeze(2).broadcast_to([128, 8, 128])
            for d in range(2):
                xs = slice(d * 8, (d + 1) * 8)
                v1 = t1[:, 0:1024].rearrange("p (g k) -> p g k", g=8)
                v2 = t2[:, 0:1024].rearrange("p (g k) -> p g k", g=8)
                op("dve", lambda e: e.tensor_tensor(out=v1, in0=RR[:, 0, xs, :], in1=a8r(d), op=ALU.mult), r=[("RR", d), "Ap", "t1", "t1b"], w=["t1"])
                op("dve", lambda e: e.tensor_tensor(out=v2, in0=RR[:, 1, xs, :], in1=a8i(d), op=ALU.mult), r=[("RR", d), "Ap", "t2", "t2b"], w=["t2"])
                op("dve", lambda e: e.tensor_tensor(out=BS[:, 0, xs, :], in0=v1, in1=v2, op=ALU.add), r=["t1", "t2", "BST"], w=[("BS", d)])
                op("dve", lambda e: e.tensor_tensor(out=v1, in0=RR[:, 1, xs, :], in1=a8r(d), op=ALU.mult), r=[("RR", d), "Ap", "t1"], w=["t1"])
                op("dve", lambda e: e.tensor_tensor(out=v2, in0=RR[:, 0, xs, :], in1=a8i(d), op=ALU.mult), r=[("RR", d), "Ap", "t2"], w=["t2"])
                op("dve", lambda e: e.tensor_tensor(out=BS[:, 1, xs, :], in0=v1, in1=v2, op=ALU.subtract), r=["t1", "t2"], w=[("BS", d)])
            self._ka(8)
            for s_ in range(8):
                for k2 in range(2):
                    srcU = uaD[2 * hf + k2].rearrange("(g h) (s c) -> h g s c", h=16, s=8)[:, :, s_, :]
                    dma(U2[s_ * 16:(s_ + 1) * 16, 8 * k2:8 * k2 + 8, :], srcU, r=[("uaD", 2 * hf + k2), "U2"], w=["U2"])
            self._ka(9)
            for d in range(2):
                for ri in range(2):
                    for g8 in range(8):
                        pi = next_ps()
                        for gp in range(2):
                            op("pe", lambda e: e.matmul(PS[pi][gp * 64:(gp + 1) * 64, 0:CP], BST[:, ri, d * 16 + 2 * g8 + gp, :],
                                                        U2[:, 2 * g8 + gp, :], start=True, stop=True),
                               r=["BST", "U2"], w=[psk(pi)])
                        if g8 % 2:
                            op("act", lambda e: e.activation(out=Sx[:, ri, d, g8, :], in_=PS[pi][:, 0:CP], func=AF.Copy), r=[psk(pi)], w=["Sx"])
                        else:
                            op("dve", lambda e: e.tensor_copy(out=Sx[:, ri, d, g8, :], in_=PS[pi][:, 0:CP]), r=[psk(pi)], w=["Sx"])
            self._ka(10)
            S.barrier()
            if hf == 0 and l + 1 < DEPTH:
                self.mod_start(l + 1)
            NB, BL = 20, 16
            cur5 = t1[:, 0:640].rearrange("p (r d g b) -> p r d g b", r=2, d=2, g=8)
            Ta = t2[:, 0:640]
            Tb = R1[:, oXS:oXS + 640]
            CF = R1[:, oTG:oTG + 640].rearrange("p (r d g b) -> p r d g b", r=2, d=2, g=8)
            v4 = lambda a: a.rearrange("p (r x b) -> p r x b", r=2, x=16)
            v3 = lambda a: a.rearrange("p (r d m) -> p r d m", r=2, d=2)
            curF = t1[:, 0:640]
            CFf = R1[:, oTG:oTG + 640]
            oz = oRB
            PRt = R1[:, oz:oz + 512].rearrange("p (r x k) -> p r x k", r=2, x=16); oz += 512
            PIt = R1[:, oz:oz + 512].rearrange("p (r x k) -> p r x k", r=2, x=16); oz += 512
            E8 = R1[:, oz:oz + 256].rearrange("p (x k) -> p x k", x=16); oz += 256
            U8 = R1[:, oz:oz + 256].rearrange("p (x k) -> p x k", x=16); oz += 256
            U8c = R1[:, oz:oz + 256].rearrange("p (x k) -> p x k", x=16); oz += 256
            W8 = R1[:, oz:oz + 256].rearrange("p (x k) -> p x k", x=16); oz += 256
            n8 = R1[:, oz:oz + 16]; oz += 16
            h0b = R1[:, oRB + 4000:oRB + 4016].bitcast(BF16).rearrange("p (r d g) -> p r d g", r=2, d=2)
            Ts1 = R1[:, oz:oz + 32].rearrange("p (r d g) -> p r d g", r=2, d=2); oz += 32
            Ts2 = R1[:, oz:oz + 32].rearrange("p (r d g) -> p r d g", r=2, d=2); oz += 32
            op("pool", lambda e: e.iota(n8, pattern=[[8, 16]], base=8, channel_multiplier=0, allow_small_or_imprecise_dtypes=True), w=["n8"])
            n8b = n8.unsqueeze(1).broadcast_to([128, 16, 16])
            op("dve", lambda e: e.tensor_tensor(out=E8, in0=smv(3).unsqueeze(2).broadcast_to([128, 16, 16]), in1=n8b, op=ALU.mult),
               r=["s5lrd", "n8"], w=["E8"])
            op("dve", lambda e: e.tensor_tensor(out=U8, in0=smv(4).unsqueeze(2).broadcast_to([128, 16, 16]), in1=n8b, op=ALU.mult),
               r=["s5th", "n8"], w=["U8"])
            op("act", lambda e: e.activation(out=E8, in_=E8, func=AF.Exp), r=["E8"], w=["E8"])
            op("dve", lambda e: e.tensor_scalar(out=U8c, in0=U8, scalar1=0.25, scalar2=None, op0=ALU.add), r=["U8"], w=["U8c"])
            self.sin_turns(PIt[:, 1], U8, W8, ["U8"], ["PIs"], "W8")
            self.sin_turns(PRt[:, 0], U8c, W8, ["U8c"], ["PRc"], "W8")
            op("dve", lambda e: e.tensor_tensor(out=PRt[:, 0], in0=PRt[:, 0], in1=E8, op=ALU.mult), r=["PRc", "E8"], w=["PRt"])
            op("dve", lambda e: e.tensor_copy(out=PRt[:, 1], in_=PRt[:, 0]), r=["PRt"], w=["PRt"])
            op("dve", lambda e: e.tensor_tensor(out=PIt[:, 1], in0=PIt[:, 1], in1=E8, op=ALU.mult), r=["PIs", "E8"], w=["PIt"])
            op("dve", lambda e: e.tensor_scalar(out=PIt[:, 0], in0=PIt[:, 1], scalar1=-1.0, scalar2=None, op0=ALU.mult), r=["PIt"], w=["PIt"])
            op("dve", lambda e: e.tensor_copy(out=h0b, in_=h0), r=["s5h0"], w=["h0b"])
            bb = lambda tab, k: tab[:, :, :, k].unsqueeze(3).broadcast_to([128, 2, 16, NB])
            sm3 = lambda tab, k: tab[:, :, :, k].rearrange("p r (d g) -> p r d g", d=2)

            def sx_at(kf, kb):
                a = Sx.ap
                return bass.AP(Sx.tensor, Sx.offset + kf * a[4][0],
                               [list(a[0]), list(a[1]), [a[2][0] + (kb - kf) * a[4][0], 2], [BL * a[4][0], 8 * NB]])

            def blk_at(t5, bf, bb_):
                a = t5.ap
                return bass.AP(t5.tensor, t5.offset + bf * a[4][0], [list(a[0]), list(a[1]), [a[2][0] + (bb_ - bf) * a[4][0], 2], list(a[3])])

            def cstep(dst, src_c, src_add, PR, PI, big):
                ta = v4(Ta) if big else Ts1
                tb_ = v4(Tb) if big else Ts2
                kk = ["Ta", "Tb"] if big else ["Ts1", "Ts2"]
                op("dve", lambda e: e.tensor_tensor(out=ta, in0=src_c, in1=PR, op=ALU.mult), r=["scan", "PRt", "PIt"], w=[kk[0]])
                op("dve", lambda e: e.tensor_tensor(out=tb_, in0=swapped(src_c), in1=PI, op=ALU.mult), r=["scan", "PRt", "PIt"], w=[kk[1]])
                op("dve", lambda e: e.tensor_tensor(out=ta, in0=ta, in1=tb_, op=ALU.add), r=kk, w=[kk[0]])
                return ta
            op("dve", lambda e: e.memset(curF, 0.0), r=["t1", "t1b"], w=["scan"])
            for k in range(BL):
                ta = cstep(None, v4(curF), None, bb(PRt, 0), bb(PIt, 0), True)
                sxk = sx_at(k, BL - 1 - k)
                op("dve", lambda e: e.tensor_tensor(out=v3(curF), in0=v3(Ta), in1=sxk, op=ALU.add), r=["Ta", "Sx"], w=["scan"])
                op("act", lambda e: e.activation(out=sxk, in_=v3(curF), func=AF.Copy), r=["scan"], w=["Sx"])
            op("dve", lambda e: e.memset(CFf, 0.0), w=["scan"])
            op("dve", lambda e: e.tensor_copy(out=blk_at(CF, 0, 15), in_=h0), r=["s5h0", "scan"], w=["scan"])
            for (bf_dst, bb_dst, bf_src, bb_src) in ((17, 16, 16, 17), (19, 18, 18, 19)):
                op("dve", lambda e: e.tensor_copy(out=blk_at(CF, bf_dst, bb_dst), in_=blk_at(cur5, bf_src, bb_src)), r=["scan"], w=["scan"])
            for j in range(1, 16):
                prev = blk_at(CF, j - 1, 16 - j)
                ta = cstep(None, prev, None, sm3(PRt, 15), sm3(PIt, 15), False)
                op("dve", lambda e: e.tensor_tensor(out=blk_at(CF, j, 15 - j), in0=ta, in1=blk_at(cur5, j - 1, 16 - j), op=ALU.add),
                   r=["Ts1", "scan"], w=["scan"])
            for si, (bf_, bb_) in ((1, (17, 16)), (2, (19, 18))):
                ta = cstep(None, blk_at(CF, bf_, bb_), None, sm3(PRt, 15), sm3(PIt, 15), False)
                op("dve", lambda e: e.tensor_tensor(out=curP, in0=ta, in1=blk_at(cur5, bf_, bb_), op=ALU.add), r=["Ts1", "scan", "curP"], w=["curP"])
                with nc.allow_non_contiguous_dma(reason="small transposed state stores"):
                    for d in range(2):
                        for ri, dst in enumerate((c["ns_re"], c["ns_im"])):
                            for gp in range(2):
                                dma(dst[si - 1, l, d, G0:G0 + 16, :].rearrange("(g8 gp) p -> gp p g8", gp=2)[gp],
                                    curP[gp * 64:(gp + 1) * 64, ri, d, :], r=["curP"], w=[("ns", si, l, d, ri, hf, gp)])
            Ta2 = t1[:, 0:640]
            Tb2 = R1[:, oRB + 2400:oRB + 3040]
            for k in range(BL):
                ta_, tb2_, kk = (v4(Ta), v4(Tb), ["Ta", "Tb"]) if k % 2 == 0 else (v4(Ta2), v4(Tb2), ["Ta2", "Tb2"])
                rk3 = ["scan", "PRt", "PIt"] + (["Ts1"] if k < 2 else [])
                op("dve", lambda e: e.tensor_tensor(out=ta_, in0=v4(CFf), in1=bb(PRt, k), op=ALU.mult), r=rk3, w=[kk[0]])
                op("dve", lambda e: e.tensor_tensor(out=tb2_, in0=swapped(v4(CFf)), in1=bb(PIt, k), op=ALU.mult), r=rk3, w=[kk[1]])
                op("dve", lambda e: e.tensor_tensor(out=ta_, in0=ta_, in1=tb2_, op=ALU.add), r=kk, w=[kk[0]])
                sxk = sx_at(k, BL - 1 - k)
                tsrc = Ta if k % 2 == 0 else Ta2
                op("dve", lambda e: e.tensor_tensor(out=sxk, in0=v3(tsrc), in1=sxk, op=ALU.add), r=[kk[0], "Sx"], w=[("Sxk", k)])
            self._ka(11)
            S.barrier()
            SEQB = [(0, 256), (256, 32), (288, 32)]
            for g in range(16):
                g8, gp = g // 2, g % 2
                ps_ = slice(gp * 64, (gp + 1) * 64)
                pi = next_ps()
                mms = [(PS[pi][:, 0:CP], MI[:, g, :], U2[:, g, :], [("MI", g), "U2"])]
                for ri in range(2):
                    Of, Ob = BS[ps_, ri, g8, :], BS[ps_, ri, 8 + g8, :]
                    for (c0, n) in SEQB:
                        mms.append((PS[pi][:, c0 + 1:c0 + n], Of, Sx[ps_, ri, 0, g8, c0:c0 + n - 1], [("BS", 0), "Sx"]))
                        mms.append((PS[pi][:, c0:c0 + n - 1], Ob, Sx[ps_, ri, 1, g8, c0 + 1:c0 + n], [("BS", 1), "Sx"]))
                    mms.append((PS[pi][:, 0:1], Of, h0b[ps_, ri, 0, g8:g8 + 1], [("BS", 0), "h0b"]))
                    mms.append((PS[pi][:, 255:256], Ob, h0b[ps_, ri, 1, g8:g8 + 1], [("BS", 1), "h0b"]))
                for mi_, (o_, l_, r_, k_) in enumerate(mms):
                    op("pe", lambda e: e.matmul(o_, l_, r_, start=(mi_ == 0), stop=(mi_ == len(mms) - 1)), r=k_, w=[psk(pi)])
                xv, tv_ = xs_[g % 2], tg_[g % 2]
                op("act", lambda e: e.activation(out=xv, in_=PS[pi][:, 0:CP], func=AF.Copy), r=[psk(pi), "scan", "Tb"], w=[("xs", g % 2)])
                op("act", lambda e: e.activation(out=tv_, in_=PS[pi][:, 0:CP], func=AF.Square), r=[psk(pi), "scan"], w=[("tg", g % 2)])
                op("dve", lambda e: e.tensor_scalar(out=tv_, in0=tv_, scalar1=0.044715, scalar2=1.0, op0=ALU.mult, op1=ALU.add),
                   r=[("tg", g % 2)], w=[("tg", g % 2)])
                op("dve", lambda e: e.tensor_tensor(out=tv_, in0=tv_, in1=xv, op=ALU.mult), r=[("tg", g % 2), ("xs", g % 2)], w=[("tg", g % 2)])
                op("act", lambda e: e.activation(out=tv_, in_=tv_, func=AF.Sigmoid, scale=1.5957691216057308), r=[("tg", g % 2)], w=[("tg", g % 2)])
                op("dve", lambda e: e.tensor_tensor(out=Y2[:, g, :], in0=xv, in1=tv_, op=ALU.mult), r=[("tg", g % 2), ("xs", g % 2)], w=["Y2"])
            dma(y2D[hf].rearrange("p (g c) -> p g c", g=16), Y2, r=["Y2"], w=[("y2D", hf)])
        bi = self.wbuf_i
        self.wbuf_i = (bi + 1) % 2
        wg = self.wbuf[bi][:, 0:2048].rearrange("p (k c) -> p k c", k=4)
        c["wload"](wg, W["s5_w_glu"][l].rearrange("(k p) c -> p k c", p=128), ("wbuf", bi))
        S.barrier()
        self._ka(12)
        o = SCR0
        ygl = R1[:, o:o + 2 * T].bitcast(BF16).rearrange("p (k t c) -> p k t c", k=4, t=8); o += 2 * T
        sgt = [R1[:, o + i * 512:o + (i + 1) * 512] for i in range(2)]; o += 1024
        for k4 in range(4):
            hf, k2 = k4 // 2, k4 % 2
            for gl in range(8):
                srcY = y2D[hf].rearrange("(t h) (g c) -> h g t c", h=16, g=16)[:, 8 * k2 + gl, :, :]
                dma(ygl[gl * 16:(gl + 1) * 16, k4, :, :], srcY, r=[("y2D", hf)], w=[("ygl", k4)])
        self._ka(13)
        yglf = ygl.rearrange("p k t c -> p k (t c)")
        yjf = Yv[0]
        for m in range(4):
            for tb in range(NTB):
                ts = slice(tb * 512, (tb + 1) * 512)
                pi = next_ps()
                for k4 in range(4):
                    op("pe", lambda e: e.matmul(PS[pi][:], wg[:, k4, m * 128:(m + 1) * 128], yglf[:, k4, ts], start=(k4 == 0), stop=(k4 == 3)),
                       r=[("wbuf", bi), ("ygl", k4)], w=[psk(pi)])
                st_ = sgt[tb % 2]
                op("act", lambda e: e.activation(out=st_, in_=PS[pi][:], func=AF.Sigmoid, bias=c["smallv"][:, 60 + m:61 + m], scale=1.0),
                   r=[psk(pi), "s5bg"], w=[("sgt", tb % 2)])
                op("dve", lambda e: e.tensor_tensor(out=yjf[:, m, ts], in0=st_, in1=yglf[:, m, ts], op=ALU.mult),
                   r=[("sgt", tb % 2), ("ygl", m)], w=[("y", 0, m, tb2) for tb2 in range(NTB)])
        def evac_g(mi, tb, pi):
            st_ = sgt[tb % 2]
            op("act", lambda e: e.activation(out=st_, in_=PS[pi][:], func=AF.Silu), r=[psk(pi)], w=[("sgt", tb % 2)])
            dstv = yj[:, mi, :, tb * 64:(tb + 1) * 64]
            op("dve", lambda e: e.tensor_tensor(out=dstv, in0=st_.rearrange("p (c s) -> p s c", s=8), in1=dstv, op=ALU.mult),
               r=[("sgt", tb % 2)] + [("y", 0, mi, tb2) for tb2 in range(NTB)], w=[("y", 0, mi, tb2) for tb2 in range(NTB)])
        c["proj"](C_GA, 4, evac_g)

    def _phase2(self):
        c = self.ctx
        op, dma, PS, next_ps, psk, W, l = c["op"], c["dma"], c["PS"], c["next_ps"], c["psk"], c["W"], c["l"]
        R1, R1b, SCR0, Yv, h3, wbuf, modT, xsp3 = (c["R1"], c["R1b"], c["SCR0"], c["Yv"], c["h3"], c["wbuf"], c["modT"],
                                                    c["xsp3"])
        wload = c["wload"]
        mg = R1[:, SCR0:SCR0 + 4 * T].bitcast(BF16).rearrange("p (k t) -> p k t", k=KT)
        o2 = SCR0 + 4 * T
        sig = [R1[:, o2 + i * 512:o2 + (i + 1) * 512] for i in range(3)]
        o2 += 3 * 512
        acc = [R1[:, o2 + i * 512:o2 + (i + 1) * 512] for i in range(2)]
        o2 += 2 * 512
        xo = [R1[:, o2 + i * 512:o2 + (i + 1) * 512] for i in range(3)]
        o2 += 3 * 512
        assert o2 <= R1N
        br_names = ["w_br_a", "w_br_b", "w_br_c"]
        for f in range(KT):
            bi = self.wbuf_i
            self.wbuf_i = (bi + 1) % 2
            wm = wbuf[bi][:, 0:3 * KT * 128].rearrange("p (b k c) -> p b k c", b=3, k=KT)
            wb = wbuf[bi][:, 3072:3072 + 3 * 4 * 128].rearrange("p (b k c) -> p b k c", b=3, k=4)
            engs = ["act", "dve", "act"]
            for b in range(3):
                c0 = C_M + b * D + f * 128
                wload(wm[:, b], W["w_in"][l][:, c0:c0 + 128].rearrange("(k p) c -> p k c", p=128), ("wbuf", bi, "m", b),
                      eng=engs[b])
                wload(wb[:, b], W[br_names[b]][l][:, f * 128:(f + 1) * 128].rearrange("(k p) c -> p k c", p=128),
                      ("wbuf", bi, "b", b), eng=engs[b])
            for tb in range(NTB):
                ts = slice(tb * 512, (tb + 1) * 512)
                a = acc[tb % 2]
                for b in range(3):
                    pm = next_ps()
                    for kt in range(KT):
                        op("pe", lambda e, pm=pm, kt=kt, b=b: e.matmul(PS[pm][:], wm[:, b, kt, :], h3[:, kt, ts],
                                                                       start=(kt == 0), stop=(kt == KT - 1)),
                           r=[("wbuf", bi, "m", b), ("h", kt, tb)], w=[psk(pm)])
                    pp = next_ps()
                    for k4 in range(4):
                        yrhs = (Yv[b][:, k4, ts] if b else
                                Yv[0][:, k4, :].rearrange("p (t c) -> p c t", t=8)[:, tb * 64:(tb + 1) * 64, :])
                        op("pe", lambda e, pp=pp, k4=k4, b=b, yrhs=yrhs: e.matmul(PS[pp][:], wb[:, b, k4, :], yrhs,
                                                                       start=(k4 == 0), stop=(k4 == 3)),
                           r=[("wbuf", bi, "b", b), ("y", b, k4, tb)], w=[psk(pp)])
                    op("act", lambda e, pm=pm, b=b: e.activation(out=sig[b][:], in_=PS[pm][:], func=AF.Sigmoid),
                       r=[psk(pm)], w=[("sig", b)])
                    if b == 0:
                        op("dve", lambda e, pp=pp, b=b, a=a: e.tensor_tensor(out=a, in0=PS[pp][:], in1=sig[b][:], op=ALU.mult),
                           r=[psk(pp), ("sig", b)], w=[("acc", tb % 2)])
                    else:
                        op("dve", lambda e, pp=pp, b=b: e.tensor_tensor(out=sig[b][:], in0=PS[pp][:], in1=sig[b][:], op=ALU.mult),
                           r=[psk(pp), ("sig", b)], w=[("sig", b)])
                        last = (b == 2)
                        dst = mg[:, f, ts] if last else a
                        wk = [("mg", f, tb)] if last else [("acc", tb % 2)]
                        op("dve", lambda e, b=b, dst=dst, a=a: e.tensor_tensor(out=dst, in0=a, in1=sig[b][:], op=ALU.add),
                           r=[("acc", tb % 2), ("sig", b)], w=wk)
        if "mg%d" % l in c["dbg_out"]:
            self._dump_bf16(mg, c["dbg_out"]["mg%d" % l], [("mg", f, tb) for f in range(KT) for tb in range(NTB)])
        for f2 in range(0, KT, 2):
            bi = self.wbuf_i
            self.wbuf_i = (bi + 1) % 2
            wo = wbuf[bi][:, 0:KT * 256].rearrange("p (k c) -> p k c", k=KT)
            wload(wo, W["w_out"][l][:, f2 * 128:f2 * 128 + 256].rearrange("(k p) c -> p k c", p=128),
                  [("wbuf", bi)] + [("wbuf", bi, t_, b_) for t_ in ("m", "b") for b_ in range(3)])
            for ff in range(2):
                f = f2 + ff
                for tb in range(NTB):
                    ts = slice(tb * 512, (tb + 1) * 512)
                    j = 0 if tb < 4 else 1
                    xi = (f * NTB + tb) % 3
                    dma(xo[xi], xsp3[:, f, ts], r=[("xsp", f, tb)], w=[("xo", xi)])
                    pi = next_ps()
                    for kt in range(KT):
                        op("pe", lambda e, pi=pi, kt=kt, ff=ff: e.matmul(PS[pi][:], wo[:, kt, ff * 128:(ff + 1) * 128],
                                                                         mg[:, kt, ts], start=(kt == 0), stop=(kt == KT - 1)),
                           r=[("wbuf", bi), ("mg", kt, tb)], w=[psk(pi)])
                    op("dve", lambda e, pi=pi, xi=xi, f=f, j=j: e.scalar_tensor_tensor(
                        out=xo[xi], in0=PS[pi][:], scalar=modT[:, 16 + f, j:j + 1], op0=ALU.mult, in1=xo[xi], op1=ALU.add),
                       r=[psk(pi), ("xo", xi), "modT"], w=[("xo", xi)])
                    dma(xsp3[:, f, ts], xo[xi], r=[("xo", xi)], w=[("xsp", f, tb)])

    def _final(self, x3, xsp3, rstd, onesm, PS, next_ps, psk, W, yout, xtm, ident):
        op, dma = self.S.op, self.S.dma
        nc = self.nc
        fg = self.sb("fg", [128, KT], F32)
        with nc.allow_non_contiguous_dma(reason="small transposed vector loads"):
            dma(fg[:], W["final_g"].rearrange("(k p) -> p k", p=128), w=["fg"])
        for tb in range(NTB):
            dma(x3[:, :, tb * 512:(tb + 1) * 512], xsp3[:, :, tb * 512:(tb + 1) * 512],
                r=[("xsp", f, tb) for f in range(KT)], w=[("x", tb)])
        self._norm_to(x3, rstd, onesm, PS, next_ps, psk)
        for kt in range(KT):
            for tb in range(NTB):
                ts = slice(tb * 512, (tb + 1) * 512)
                op("dve", lambda e, kt=kt, ts=ts: e.scalar_tensor_tensor(
                    out=x3[:, kt, ts], in0=x3[:, kt, ts], scalar=fg[:, kt:kt + 1], op0=ALU.mult, in1=rstd[:, ts], op1=ALU.mult),
                   r=[("x", tb), "rstd", "fg"], w=[("x", tb)])
        for tt in range(T // 128):
            b = tt % 2
            tsl = slice(tt * 128, (tt + 1) * 128)
            for half in range(2):
                pi = next_ps()
                for j in range(4):
                    kt = half * 4 + j
                    op("pe", lambda e, kt=kt, j=j, pi=pi: e.transpose(PS[pi][:, j * 128:(j + 1) * 128], x3[:, kt, tsl], ident[:]),
                       r=[("x", tt // 4), "ident"], w=[psk(pi)])
                dst = xtm[b][:, half * 512:(half + 1) * 512]
                if half == 0:
                    op("act", lambda e, dst=dst, pi=pi: e.activation(out=dst, in_=PS[pi][:], func=AF.Copy),
                       r=[psk(pi)], w=[("xtm", b)])
                else:
                    op("dve", lambda e, dst=dst, pi=pi: e.tensor_copy(out=dst, in_=PS[pi][:]), r=[psk(pi)], w=[("xtm", b)])
            dma(yout[tsl, :], xtm[b][:], r=[("xtm", b)], w=[("yout", tt)])


    def sin_turns(self, out, u, tmp, rk, wk, tk, eng="dve"):
        op = self.S.op
        op(eng, lambda e: e.tensor_scalar(out=tmp, in0=u, scalar1=MAGIC, scalar2=MAGIC, op0=ALU.add, op1=ALU.subtract),
           r=rk, w=[tk])
        op(eng, lambda e: e.tensor_tensor(out=tmp, in0=u, in1=tmp, op=ALU.subtract), r=list(rk) + [tk], w=[tk])
        op("act", lambda e: e.activation(out=out, in_=tmp, func=AF.Sin, scale=TWO_PI_S), r=[tk], w=wk)

    def _pos_embed(self, x3):
        op = self.S.op
        R1 = self.R1
        o = 26112
        omg = R1[:, o:o + 2]; o += 2
        posv = R1[:, o:o + 64]; o += 64
        pu = R1[:, o:o + 384]; o += 384
        pt = R1[:, o:o + 384]; o += 384
        pe = R1[:, o:o + 384]; o += 384
        op("pool", lambda e: e.iota(omg, pattern=[[128, 2]], base=0, channel_multiplier=1,
                                    allow_small_or_imprecise_dtypes=True), w=["omg"])
        op("pool", lambda e: e.iota(posv, pattern=[[1, 64]], base=0, channel_multiplier=0,
                                    allow_small_or_imprecise_dtypes=True), w=["posv"])
        op("act", lambda e: e.activation(out=omg, in_=omg, func=AF.Exp, scale=-math.log(10000.0) / 256.0),
           r=["omg"], w=["omg"])
        op("dve", lambda e: e.tensor_scalar(out=omg, in0=omg, scalar1=1.0 / (2 * math.pi), scalar2=None, op0=ALU.mult),
           r=["omg"], w=["omg"])
        for kt in range(8):
            n = 32 if kt < 4 else 64
            off = kt * 32 if kt < 4 else 128 + (kt - 4) * 64
            ph = 0.25 if (kt % 4) >= 2 else 0.0
            c = kt % 2
            op("dve", lambda e, n=n, off=off, ph=ph, c=c: e.tensor_scalar(
                out=pu[:, off:off + n], in0=posv[:, 0:n], scalar1=omg[:, c:c + 1], scalar2=ph, op0=ALU.mult, op1=ALU.add),
               r=["posv", "omg"], w=["pu"])
        self.sin_turns(pe, pu, pt, ["pu"], ["pe"], "pt")
        for kt in range(8):
            xs = x3[:, kt, 0:LS].rearrange("p (r c) -> p r c", c=64)
            if kt < 4:
                tb_ = pe[:, kt * 32:(kt + 1) * 32].unsqueeze(2).broadcast_to([128, 32, 64])
            else:
                tb_ = pe[:, 128 + (kt - 4) * 64:128 + (kt - 3) * 64].unsqueeze(1).broadcast_to([128, 32, 64])
            op("dve", lambda e, xs=xs, tb_=tb_: e.tensor_tensor(out=xs, in0=xs, in1=tb_, op=ALU.add),
               r=["pe"] + [("x", b) for b in range(4)], w=[("x", b) for b in range(4)])

    def _setup_tables(self):
        op, dma = self.S.op, self.S.dma
        R1 = self.R1
        self.tabs = {}
        o = 0
        kk2 = R1[:, o:o + 1536]; o += 1536
        tpos = R1[:, o:o + 2048]; o += 2048
        mm = [R1[:, o + i * 2048:o + (i + 1) * 2048] for i in range(2)]; o += 4096
        qq = [R1[:, o + i * 2048:o + (i + 1) * 2048] for i in range(2)]; o += 4096
        ob = [[R1[:, o + (2 * i + c) * 1024:o + (2 * i + c + 1) * 1024].bitcast(BF16) for c in range(2)] for i in range(2)]
        o += 4096
        pcol = R1[:, o:o + 16]; o += 16
        kcol = R1[:, o:o + 16]; o += 16
        op("pool", lambda e: e.iota(pcol, pattern=[[128, 16]], base=0, channel_multiplier=1,
                                    allow_small_or_imprecise_dtypes=True), w=["pcol"])
        op("pool", lambda e: e.iota(kcol, pattern=[[256, 16]], base=1, channel_multiplier=2,
                                    allow_small_or_imprecise_dtypes=True), w=["kcol"])
        op("pool", lambda e: e.iota(kk2, pattern=[[2, 1536]], base=1, channel_multiplier=0,
                                    allow_small_or_imprecise_dtypes=True), w=["kk2"])
        cnt = [0]

        def gen(freev, pscal, Wd, N, sink):
            i = cnt[0] % 2
            cnt[0] += 1
            m, q = mm[i][:, 0:Wd], qq[i][:, 0:Wd]
            oc, os_ = ob[i][0][:, 0:Wd], ob[i][1][:, 0:Wd]
            op("dve", lambda e: e.tensor_scalar(out=m, in0=freev, scalar1=pscal, scalar2=None, op0=ALU.mult),
               r=["kk2", "tpos", "pcol", "kcol"], w=[("m", i)])
            op("dve", lambda e: e.tensor_scalar(out=q, in0=m, scalar1=1.0 / (2 * N), scalar2=MAGIC, op0=ALU.mult, op1=ALU.add),
               r=[("m", i)], w=[("q", i)])
            op("dve", lambda e: e.tensor_scalar(out=q, in0=q, scalar1=MAGIC, scalar2=-2.0 * N, op0=ALU.subtract, op1=ALU.mult),
               r=[("q", i)], w=[("q", i)])
            op("dve", lambda e: e.tensor_tensor(out=m, in0=m, in1=q, op=ALU.add), r=[("m", i), ("q", i)], w=[("m", i)])
            op("act", lambda e: e.activation(out=os_, in_=m, func=AF.Sin, scale=-math.pi / N * (1 - 1e-6)),
               r=[("m", i)], w=[("os", i)])
            op("dve", lambda e: e.scalar_tensor_tensor(out=q, in0=m, scalar=-1.0, op0=ALU.mult, in1=m, op1=ALU.max),
               r=[("m", i)], w=[("q", i)])
            op("act", lambda e: e.activation(out=oc, in_=q, func=AF.Sin, scale=-math.pi / N * (1 - 1e-6), bias=self._halfpi[:]),
               r=[("q", i), "halfpi"], w=[("oc", i)])
            sink(oc, os_, [("oc", i), ("os", i)])

        self._halfpi = self.sb("halfpi", [128, 1], F32)
        op("pool", lambda e: e.memset(self._halfpi[:], math.pi / 2 * (1 - 1e-6)), w=["halfpi"])
        for name, L in (("S", LS), ("P", LP)):
            N = 3 * L // 2
            NK = N // 2
            nI = L // 128
            nJ = (NK + 127) // 128
            kws = [min(128, NK - j * 128) for j in range(nJ)]
            Fc = self.dram_tmp("Fc" + name, [nJ, 128, nI, 128], BF16)
            Fs = self.dram_tmp("Fs" + name, [nJ, 128, nI, 128], BF16)
            Gc = self.dram_tmp("Gc" + name, [nJ, 128, L], BF16)
            Gs = self.dram_tmp("Gs" + name, [nJ, 128, L], BF16)
            self.tabs[name] = dict(L=L, N=N, NK=NK, nI=nI, nJ=nJ, kws=kws, Fc=Fc, Fs=Fs, Gc=Gc, Gs=Gs)
            op("pool", lambda e: e.iota(tpos[:, 0:L], pattern=[[1, L]], base=L // 2, channel_multiplier=0,
                                        allow_small_or_imprecise_dtypes=True), r=["tpos"], w=["tpos"])
            for I in range(nI):
                def sinkF(oc, os_, keys, I=I):
                    for j in range(nJ):
                        kw = kws[j]
                        dma(Fc[j, :, I, 0:kw], oc[:, j * 128:j * 128 + kw], r=keys, w=[("Fc" + name, j)], q="sp")
                        dma(Fs[j, :, I, 0:kw], os_[:, j * 128:j * 128 + kw], r=keys, w=[("Fs" + name, j)], q="sp")
                gen(kk2[:, 0:NK], pcol[:, I:I + 1], NK, N, sinkF)
            for j in range(nJ):
                def sinkG(oc, os_, keys, j=j):
                    dma(Gc[j], oc, r=keys, w=[("Gc" + name, j)], q="sp")
                    dma(Gs[j], os_, r=keys, w=[("Gs" + name, j)], q="sp")
                gen(tpos[:, 0:L], kcol[:, j:j + 1], L, N, sinkG)

    def _filters(self, name):
        c = self.ctx
        op, dma, PS, next_ps, psk, W, l = c["op"], c["dma"], c["PS"], c["next_ps"], c["psk"], c["W"], c["l"]
        R1, SCR0 = c["R1"], c["SCR0"]
        nc = self.nc
        tb = self.tabs[name]
        L, N, NK, nI, nJ, kws = tb["L"], tb["N"], tb["NK"], tb["nI"], tb["nJ"], tb["kws"]
        if "Hf" not in tb:
            tb["Hf"] = self.dram_tmp("Hf" + name, [nJ, 128, 2, 2, 512], BF16)
        Hf = tb["Hf"]
        o = SCR0
        htm = R1[:, o:o + nI * 512].bitcast(BF16).rearrange("p (i c) -> p i c", i=nI); o += 8192
        z = R1[0:65, o:o + L]; o += 2048
        h1 = R1[0:64, o:o + L]; o += 2048
        h2 = R1[0:64, o:o + L]; o += 2048
        tmp = R1[0:65, o:o + L]; o += 2048
        assert o <= R1N
        o = 0
        tio = R1[:, o:o + L]; o += 2048
        dl = R1[:, o:o + 512]; o += 512
        wt = R1[:, o:o + 512]; o += 512
        hst = [R1[:, o + i * 1024:o + (i + 1) * 1024].bitcast(BF16).rearrange("p (r q c) -> p r q c", r=2, q=2) for i in range(2)]
        o += 2048
        sv = self._fsv if hasattr(self, "_fsv") else None
        if sv is None:
            sv = self._fsv = self.sb("fsv", [128, 48], F32)
            self._w1p = self.sb("w1p", [65, 64], F32)
            self._w2 = self.sb("w2f", [64, 64], F32)
        w1p, w2 = self._w1p, self._w2
        w3 = R1[0:64, 5120:6144]
        op("pool", lambda e: e.iota(tio, pattern=[[1, L]], base=0, channel_multiplier=0,
                                    allow_small_or_imprecise_dtypes=True), w=["tio"])
        op("pool", lambda e: e.iota(dl, pattern=[[1, 512]], base=0, channel_multiplier=0,
                                    allow_small_or_imprecise_dtypes=True), w=["dl"])
        dmin = abs(math.log(1e-2) / 1.5)
        dmax = abs(math.log(1e-2) / 0.3)
        op("dve", lambda e: e.tensor_scalar(out=dl, in0=dl, scalar1=(dmax - dmin) / 511.0, scalar2=dmin, op0=ALU.mult, op1=ALU.add),
           r=["dl"], w=["dl"])
        op("pool", lambda e: e.memset(sv[:, 0:2], 0.0), w=["sv01"])
        for p0 in (0, 32):
            op("pool", lambda e, p0=p0: e.iota(sv[p0:p0 + 16, 0:1], pattern=[[0, 1]], base=0, channel_multiplier=1,
                                               allow_small_or_imprecise_dtypes=True), r=["sv01"], w=["sv01"])
            fstep = (15.0 - 1e-4) / 15.0
            op("dve", lambda e, p0=p0: e.tensor_scalar(out=sv[p0:p0 + 16, 0:1], in0=sv[p0:p0 + 16, 0:1], scalar1=fstep / L,
                                                       scalar2=1e-4 / L, op0=ALU.mult, op1=ALU.add), r=["sv01"], w=["sv01"])
            op("pool", lambda e, p0=p0: e.memset(sv[p0:p0 + 16, 1:2], 0.25 if p0 == 0 else 0.5), r=["sv01"], w=["sv01"])
        op("pool", lambda e: e.iota(sv[:, 8:8 + nI], pattern=[[128, nI]], base=-(L // 2), channel_multiplier=1,
                                    allow_small_or_imprecise_dtypes=True), w=["negoff"])
        op("dve", lambda e: e.scalar_tensor_tensor(out=sv[:, 24:24 + nI], in0=sv[:, 8:8 + nI], scalar=-1.0, op0=ALU.mult,
                                                   in1=sv[:, 8:8 + nI], op1=ALU.max), r=["negoff"], w=["negoff2"])
        op("dve", lambda e: e.tensor_scalar(out=sv[:, 8:8 + nI], in0=sv[:, 24:24 + nI], scalar1=-2.0 / L, scalar2=None,
                                            op0=ALU.mult), r=["negoff2"], w=["negoff"])
        op("pool", lambda e: e.memset(w1p[:], 0.0), w=["w1p"])
        with nc.allow_non_contiguous_dma(reason="small transposed vector loads"):
            dma(w1p[0:16, :], W["hy_f_w1"][l][1:17, :], w=["w1p"])
            dma(w1p[32:48, :], W["hy_f_w1"][l][17:33, :], w=["w1p"])
            dma(w1p[64:65, :], W["hy_f_w1"][l][0:1, :], w=["w1p"])
            dma(w2[:], W["hy_f_w2"][l], w=["w2f"])
            dma(w3[:], W["hy_f_w3"][l], w=["w3f"])
            dma(sv[0:64, 2:3], W["hy_f_b1"][l].rearrange("(p o) -> p o", o=1), w=["svw"])
            dma(sv[0:64, 3:4], W["hy_f_freq"][l].rearrange("(p o) -> p o", o=1), w=["svw"])
            dma(sv[0:64, 4:5], W["hy_f_b2"][l].rearrange("(p o) -> p o", o=1), w=["svw"])
        op("dve", lambda e: e.tensor_scalar(out=sv[0:64, 5:6], in0=sv[0:64, 3:4], scalar1=1.0 / (2 * math.pi), scalar2=None,
                                            op0=ALU.mult), r=["svw"], w=["svs"])
        op("dve", lambda e: e.tensor_tensor(out=sv[0:64, 6:7], in0=sv[0:64, 5:6], in1=sv[0:64, 2:3], op=ALU.mult),
           r=["svs", "svw"], w=["svc1"])
        op("dve", lambda e: e.tensor_tensor(out=sv[0:64, 7:8], in0=sv[0:64, 5:6], in1=sv[0:64, 4:5], op=ALU.mult),
           r=["svs", "svw"], w=["svc2"])
        op("dve", lambda e: e.tensor_scalar(out=tmp[0:64, :], in0=tio[0:64, :], scalar1=sv[0:64, 0:1], scalar2=sv[0:64, 1:2],
                                            op0=ALU.mult, op1=ALU.add), r=["tio", "sv01"], w=["fu"])
        self.sin_turns(z[0:64, :], tmp[0:64, :], h1[0:64, :], ["fu"], ["fz"], "ft")
        op("dve", lambda e: e.tensor_scalar(out=z[64:65, :], in0=tio[64:65, :], scalar1=1.0 / (L - 1), scalar2=None, op0=ALU.mult),
           r=["tio"], w=["fz64"])
        nb = (L + 511) // 512
        for (src, ks, wgt, wkey, ccol, dst, rkeys, dkey) in ((z, 65, w1p, "w1p", 6, h1, ["fz", "fz64"], "fh1"),
                                                             (h1, 64, w2, "w2f", 7, h2, ["fh1"], "fh2")):
            for b in range(nb):
                cs = slice(b * 512, min(L, (b + 1) * 512))
                pi = next_ps()
                wd = cs.stop - cs.start
                op("pe", lambda e: e.matmul(PS[pi][0:64, 0:wd], wgt[0:ks, :], src[0:ks, cs], start=True, stop=True),
                   r=rkeys + [wkey], w=[psk(pi)])
                op("dve", lambda e: e.tensor_scalar(out=tmp[0:64, cs], in0=PS[pi][0:64, 0:wd], scalar1=sv[0:64, 5:6],
                                                    scalar2=sv[0:64, ccol:ccol + 1], op0=ALU.mult, op1=ALU.add),
                   r=[psk(pi), "svs", "svc1", "svc2"], w=["fu"])
            self.sin_turns(dst[0:64, :], tmp[0:64, :], z[0:64, :] if dst is h1 else h1[0:64, :], ["fu"], [dkey],
                           "fz" if dst is h1 else "fh1")
        for I in range(nI):
            op("act", lambda e: e.activation(out=wt, in_=dl, func=AF.Exp, scale=sv[:, 8 + I:9 + I]), r=["dl", "negoff"], w=["wt"])
            for oo in range(2):
                pi = next_ps()
                op("pe", lambda e: e.matmul(PS[pi][:], h2[0:64, I * 128:(I + 1) * 128], w3[:, oo * 512:(oo + 1) * 512],
                                            start=True, stop=True), r=["fh2", "w3f"], w=[psk(pi)])
                op("dve", lambda e: e.scalar_tensor_tensor(out=htm[:, I, oo * 512:(oo + 1) * 512], in0=wt, scalar=HY_SHIFT,
                                                           op0=ALU.add, in1=PS[pi][:], op1=ALU.mult),
                   r=["wt", psk(pi)], w=[("htm", I)])
        for j in range(nJ):
            kw = kws[j]
            fc, fs, fkey = self._load_F(name, j)
            hs = hst[j % 2]
            for ri, ft in enumerate((fc, fs)):
                for oo in range(2):
                    pi = next_ps()
                    for I in range(nI):
                        op("pe", lambda e: e.matmul(PS[pi][0:kw, :], ft[:, I, 0:kw], htm[:, I, oo * 512:(oo + 1) * 512],
                                                    start=(I == 0), stop=(I == nI - 1)), r=[fkey, ("htm", I)], w=[psk(pi)])
                    if (ri + oo) % 2 == 0:
                        op("act", lambda e: e.activation(out=hs[0:kw, ri, oo, :], in_=PS[pi][0:kw, :], func=AF.Copy, scale=2.0 / N),
                           r=[psk(pi)], w=[("hst", j % 2)])
                    else:
                        op("dve", lambda e: e.tensor_scalar(out=hs[0:kw, ri, oo, :], in0=PS[pi][0:kw, :], scalar1=2.0 / N,
                                                            scalar2=None, op0=ALU.mult), r=[psk(pi)], w=[("hst", j % 2)])
            dma(Hf[j, 0:kw], hs[0:kw], r=[("hst", j % 2)], w=[("Hf" + name, j)])

    def _load_F(self, name, j):
        tb = self.tabs[name]
        nI = tb["nI"]
        si = self._slot_i
        self._slot_i = (si + 1) % 4
        key = self.slot_keys[si]
        fc = self.slots[si][:, 0:nI * 128].rearrange("p (i k) -> p i k", i=nI)
        fs = self.slots[si][:, 2048:2048 + nI * 128].rearrange("p (i k) -> p i k", i=nI)
        self.S.dma(fc, tb["Fc"][j], r=[("Fc" + name, j)], w=[key])
        self.S.dma(fs, tb["Fs"][j], r=[("Fs" + name, j)], w=[key])
        return fc, fs, key

    def _branch_b(self):
        c = self.ctx
        op, dma, PS, next_ps, psk, W, l = c["op"], c["dma"], c["PS"], c["next_ps"], c["psk"], c["W"], c["l"]
        R1, SCR0, Yv, identb = c["R1"], c["SCR0"], c["Yv"], c["identb"]
        nc = self.nc
        S = self.S
        if "f" in self.parts:
            self._filters("S")
            self._filters("P")
            S.barrier()
        if "b" not in self.parts:
            self._zero_y(1)
            return
        o = SCR0
        bufA = R1[:, o:o + 2 * T].bitcast(BF16).rearrange("p (k t) -> p k t", k=4); o += 2 * T
        bufBf = R1[:, o:o + 2 * T].bitcast(BF16); o += 2 * T
        tmv = bufBf.rearrange("p (i c) -> p i c", c=512)
        bufP = R1[:, o:o + 6144].bitcast(BF16).rearrange("p (j r c) -> p j r c", j=12, r=2); o += 6144
        assert o <= R1N
        TPAD = T + 12
        o = 0
        Cc = R1[:, o:o + TPAD]; o += TPAD
        Rr = R1[:, o:o + TPAD // 2].bitcast(BF16); o += TPAD // 2
        t12 = [R1[:, o + i * 512:o + (i + 1) * 512] for i in range(2)]; o += 1024
        assert o <= 5120
        hft = [self.wbuf[i][:, 4096:5120].rearrange("p (r c) -> p r c", r=2) for i in range(2)]
        seqs = [("S", 0, LS, 2), ("P", LS, LP, LS + 6), ("P", LS + LP, LP, LS + LP + 10)]
        sv = c["smallv"]
        with nc.allow_non_contiguous_dma(reason="small transposed vector loads"):
            for k in range(3):
                dma(sv[:, 8 + k * 12:8 + (k + 1) * 12], W["hy_conv_w"][l][k].rearrange("(m p) -> p m", p=128), w=["hyw"])
            dma(sv[:, 44:56], W["hy_conv_b"][l].rearrange("(m p) -> p m", p=128), w=["hyw"])
            for oo in range(2):
                dma(sv[:, 56 + oo * 4:60 + oo * 4], W["hy_bias"][l][oo].rearrange("(m p) -> p m", p=128), w=["hyw"])
        op("dve", lambda e: e.memset(Rr, 0.0), w=["Rr"])

        def proj_conv(part, dst, dkeyf):
            import os
            pc = int(os.environ.get("KB_PC", "9"))
            def evac(mi, tb, pi):
                m = part * 4 + mi
                segs = [(0, 512, 2 + tb * 512)] if tb < 4 else [(0, LP, LS + 6), (LP, 2 * LP, LS + LP + 10)]
                for (a, b, po) in segs:
                    op("dve", lambda e: e.tensor_copy(out=Rr[:, po:po + b - a], in_=PS[pi][:, a:b]), r=[psk(pi)], w=["Rr", ("rrseq", pi)])
                    op("act", lambda e: e.activation(out=Cc[:, po:po + b - a], in_=PS[pi][:, a:b], func=AF.Identity,
                                                     scale=sv[:, 8 + 12 + m:8 + 12 + m + 1], bias=sv[:, 44 + m:45 + m]),
                       r=[psk(pi), "hyw", ("rrseq", pi)], w=["Cc"])
                if tb == NTB - 1 and pc >= 3:
                    for (tn, t0, ln, po) in seqs:
                        op("dve", lambda e: e.scalar_tensor_tensor(out=Cc[:, po:po + ln], in0=Rr[:, po - 1:po - 1 + ln],
                                                                   scalar=sv[:, 8 + m:9 + m], op0=ALU.mult,
                                                                   in1=Cc[:, po:po + ln], op1=ALU.add),
                           r=["Rr", "Cc", "hyw"], w=["Cc"])
                        if pc >= 4:
                            op("dve", lambda e: e.scalar_tensor_tensor(out=dst[:, mi, t0:t0 + ln], in0=Rr[:, po + 1:po + 1 + ln],
                                                                   scalar=sv[:, 8 + 24 + m:8 + 24 + m + 1], op0=ALU.mult,
                                                                   in1=Cc[:, po:po + ln], op1=ALU.add),
                               r=["Rr", "Cc", "hyw"], w=[dkeyf(mi, tbb) for tbb in range(t0 // 512, (t0 + ln + 511) // 512)])
            c["proj"](C_UB + part * 512, 4, evac)

        def to_tm(src, skeyf):
            for tt in range(T // 128):
                pi = next_ps()
                pv = PS[pi][:].bitcast(BF16)
                for k in range(4):
                    op("pe", lambda e: e.transpose(pv[:, k * 128:(k + 1) * 128], src[:, k, tt * 128:(tt + 1) * 128], identb[:]),
                       r=[skeyf(k, tt // 4), "identb"], w=[psk(pi)])
                if tt % 2:
                    op("act", lambda e: e.activation(out=tmv[:, tt, :], in_=pv[:, 0:512], func=AF.Copy), r=[psk(pi)], w=[("tm", tt)])
                else:
                    op("dve", lambda e: e.tensor_copy(out=tmv[:, tt, :], in_=pv[:, 0:512]), r=[psk(pi)], w=[("tm", tt)])

        def forward(tn, t0, oo):
            tb = self.tabs[tn]
            nI, nJ, kws, Hf = tb["nI"], tb["nJ"], tb["kws"], tb["Hf"]
            I0 = t0 // 128
            for j in range(nJ):
                kw = kws[j]
                fc, fs, fkey = self._load_F(tn, j)
                hb = hft[j % 2]
                dma(hb[0:kw], Hf[j, 0:kw, :, oo, :], r=[("Hf" + tn, j)], w=[("hft", j % 2)])
                pr = next_ps()
                for I in range(nI):
                    op("pe", lambda e: e.matmul(PS[pr][0:kw, :], fc[:, I, 0:kw], tmv[:, I0 + I, :], start=(I == 0), stop=(I == nI - 1)),
                       r=[fkey, ("tm", I0 + I)], w=[psk(pr)])
                pim = next_ps()
                for I in range(nI):
                    op("pe", lambda e: e.matmul(PS[pim][0:kw, :], fs[:, I, 0:kw], tmv[:, I0 + I, :], start=(I == 0), stop=(I == nI - 1)),
                       r=[fkey, ("tm", I0 + I)], w=[psk(pim)])
                hk = [("hft", j % 2)]
                op("dve", lambda e: e.tensor_tensor(out=t12[0][0:kw], in0=PS[pr][0:kw, :], in1=hb[0:kw, 0, :], op=ALU.mult),
                   r=[psk(pr)] + hk, w=[("t12", 0)])
                op("dve", lambda e: e.tensor_tensor(out=t12[1][0:kw], in0=PS[pim][0:kw, :], in1=hb[0:kw, 1, :], op=ALU.mult),
                   r=[psk(pim)] + hk, w=[("t12", 1)])
                op("dve", lambda e: e.tensor_tensor(out=bufP[0:kw, j, 0, :], in0=t12[0][0:kw], in1=t12[1][0:kw], op=ALU.subtract),
                   r=[("t12", 0), ("t12", 1)], w=[("P", j)])
                op("dve", lambda e: e.tensor_tensor(out=t12[0][0:kw], in0=PS[pr][0:kw, :], in1=hb[0:kw, 1, :], op=ALU.mult),
                   r=[psk(pr)] + hk, w=[("t12", 0)])
                op("dve", lambda e: e.tensor_tensor(out=t12[1][0:kw], in0=PS[pim][0:kw, :], in1=hb[0:kw, 0, :], op=ALU.mult),
                   r=[psk(pim)] + hk, w=[("t12", 1)])
                op("dve", lambda e: e.tensor_tensor(out=bufP[0:kw, j, 1, :], in0=t12[0][0:kw], in1=t12[1][0:kw], op=ALU.add),
                   r=[("t12", 0), ("t12", 1)], w=[("P", j)])

        def inverse(tn, t0, evac):
            tb = self.tabs[tn]
            L, nJ, kws = tb["L"], tb["nJ"], tb["kws"]
            for b0 in range(0, L, 512):
                wd = min(512, L - b0)
                accs = [next_ps() for _ in range(4)]
                for j0 in range(0, nJ, 2):
                    si = self._slot_i
                    self._slot_i = (si + 1) % 4
                    gv = self.slots[si].rearrange("p (r j c) -> p r j c", r=2, j=2)
                    nj = min(2, nJ - j0)
                    for ri, tabn in enumerate(("Gc", "Gs")):
                        dma(gv[:, ri, 0:nj, 0:wd], tb[tabn][j0:j0 + nj, :, b0:b0 + wd].rearrange("j p c -> p j c"),
                            r=[(tabn + tn, j0 + jj) for jj in range(nj)], w=[self.slot_keys[si]])
                    for jj in range(nj):
                        j = j0 + jj
                        kw = kws[j]
                        for cc in range(4):
                            for ri in range(2):
                                op("pe", lambda e: e.matmul(PS[accs[cc]][:, 0:wd], bufP[0:kw, j, ri, cc * 128:(cc + 1) * 128],
                                                            gv[0:kw, ri, jj, 0:wd], start=(j == 0 and ri == 0),
                                                            stop=(j == nJ - 1 and ri == 1)),
                                   r=[("P", j), self.slot_keys[si]], w=[psk(accs[cc])])
                for cc in range(4):
                    evac(cc, t0 + b0, wd, accs[cc])

        kA = lambda k, tb_: ("bufA", k, tb_)
        kY = lambda k, tb_: ("y", 1, k, tb_)
        import os
        stop = int(os.environ.get("KB_STOP", "99"))
        proj_conv(0, bufA, kA)
        if stop == 1:
            S.barrier(); self._zero_y(1); return
        to_tm(bufA, kA)
        if stop == 2:
            S.barrier(); self._zero_y(1); return
        proj_conv(1, Yv[1], kY)
        if stop == 3:
            S.barrier(); self._zero_y(1); return
        if stop == 4:
            forward("S", 0, 0)
            S.barrier(); self._zero_y(1); return
        if stop == 5:
            forward("S", 0, 0)
            inverse("S", 0, lambda cc, ta, wd, pi: None)
            S.barrier(); self._zero_y(1); return

        def evac1(cc, ta, wd, pi):
            tbk = ta // 512
            ti = cc % 2
            op("dve", lambda e: e.scalar_tensor_tensor(out=t12[ti][:, 0:wd], in0=bufA[:, cc, ta:ta + wd], scalar=sv[:, 56 + cc:57 + cc],
                                                       op0=ALU.mult, in1=PS[pi][:, 0:wd], op1=ALU.add),
               r=[psk(pi), kA(cc, tbk), "hyw"], w=[("t12", ti)])
            op("dve", lambda e: e.tensor_tensor(out=Yv[1][:, cc, ta:ta + wd], in0=t12[ti][:, 0:wd], in1=Yv[1][:, cc, ta:ta + wd],
                                                 op=ALU.mult), r=[("t12", ti), kY(cc, tbk)], w=[kY(cc, tbk)])
        for (tn, t0, ln, po) in seqs:
            forward(tn, t0, 0)
            inverse(tn, t0, evac1)
        to_tm(Yv[1], kY)
        proj_conv(2, bufA, kA)

        def evac2(cc, ta, wd, pi):
            tbk = ta // 512
            ti = cc % 2
            op("dve", lambda e: e.scalar_tensor_tensor(out=t12[ti][:, 0:wd], in0=Yv[1][:, cc, ta:ta + wd], scalar=sv[:, 60 + cc:61 + cc],
                                                       op0=ALU.mult, in1=PS[pi][:, 0:wd], op1=ALU.add),
               r=[psk(pi), kY(cc, tbk), "hyw"], w=[("t12", ti)])
            op("dve", lambda e: e.tensor_tensor(out=bufA[:, cc, ta:ta + wd], in0=t12[ti][:, 0:wd], in1=bufA[:, cc, ta:ta + wd],
                                                 op=ALU.mult), r=[("t12", ti), kA(cc, tbk)], w=[kA(cc, tbk)])
        for (tn, t0, ln, po) in seqs:
            forward(tn, t0, 1)
            inverse(tn, t0, evac2)

        def evac_g(mi, tb, pi):
            ti = tb % 2
            op("act", lambda e: e.activation(out=t12[ti][:], in_=PS[pi][:], func=AF.Silu), r=[psk(pi)], w=[("t12", ti)])
            op("dve", lambda e: e.tensor_tensor(out=Yv[1][:, mi, tb * 512:(tb + 1) * 512], in0=t12[ti][:],
                                                 in1=bufA[:, mi, tb * 512:(tb + 1) * 512], op=ALU.mult),
               r=[("t12", ti), kA(mi, tb)], w=[kY(mi, tb)])
        c["proj"](C_GB, 4, evac_g)
        if "yb%d" % l in c["dbg_out"]:
            self._dump_bf16(Yv[1], c["dbg_out"]["yb%d" % l], [kY(k, tbb) for k in range(4) for tbb in range(NTB)])

    def _tmview(self, buf):
        return buf.rearrange("p k t -> p (k t)").rearrange("p (i c) -> p i c", c=512)


WEIGHT_SHAPES = {
    "norm_g": (2, 1024), "w_mod": (2, 1024, 3072), "b_mod": (2, 3072), "w_in": (2, 1024, 7168),
    "s5_lam_re": (2, 2, 32, 64), "s5_lam_im": (2, 2, 32, 64), "s5_log_dt": (2, 2, 32),
    "s5_b_re": (2, 2, 32, 64, 16), "s5_b_im": (2, 2, 32, 64, 16), "s5_c_re": (2, 2, 32, 16, 64),
    "s5_c_im": (2, 2, 32, 16, 64), "s5_d": (2, 512), "s5_w_glu": (2, 512, 512), "s5_b_glu": (2, 512),
    "hy_conv_w": (2, 3, 1536), "hy_conv_b": (2, 1536), "hy_f_w1": (2, 33, 64), "hy_f_b1": (2, 64),
    "hy_f_w2": (2, 64, 64), "hy_f_b2": (2, 64), "hy_f_freq": (2, 64), "hy_f_w3": (2, 64, 1024),
    "hy_bias": (2, 2, 512), "pool_w": (2, 4, 128, 128), "pool_scale": (2, 512),
    "w_br_a": (2, 512, 1024), "w_br_b": (2, 512, 1024), "w_br_c": (2, 512, 1024), "w_out": (2, 1024, 1024),
    "final_g": (1024,),
}

_NC_CACHE = {}


def make_in_maps(inputs):
    maps = []
    f = lambda a: np.ascontiguousarray(np.asarray(a, dtype=np.float32))
    xs, xp = f(inputs["x_sample"]), f(inputs["x_prompt"])
    cc, cctx = f(inputs["c"]), f(inputs["c_ctx"])
    sre, sim = f(inputs["state_s5_re"]), f(inputs["state_s5_im"])
    wts = {k: f(inputs[k]) for k in WEIGHT_SHAPES}
    for i in range(NCORES):
        m = dict(wts)
        m["xin"] = np.ascontiguousarray(np.concatenate([xs[i], xp[2 * i], xp[2 * i + 1]], axis=0))
        m["cond"] = np.ascontiguousarray(np.stack([cc[i], cctx], axis=0))
        m["st_re"] = sre[i]
        m["st_im"] = sim[i]
        maps.append(m)
    return maps


def run(inputs, dbg=None, parts="tpfba"):
    key = (tuple(sorted((dbg or {}).items())), parts)
    if key not in _NC_CACHE:
        _NC_CACHE[key] = Builder(dbg, parts).build()
    nc = _NC_CACHE[key]
    res = run_bass_kernel_spmd(nc, make_in_maps(inputs), core_ids=list(range(NCORES)))
    return res.results


def kernel(_parts="tpfba", **inputs):
    r = run(inputs, parts=_parts)
    y_prompt = np.empty((16, LP, D), np.float32)
    y_sample = np.empty((8, LS, D), np.float32)
    n_re = np.empty((16, DEPTH, 2, S5_G, S5_P), np.float32)
    n_im = np.empty((16, DEPTH, 2, S5_G, S5_P), np.float32)
    for i in range(NCORES):
        y = r[i]["yout"]
        y_sample[i] = y[0:LS]
        y_prompt[2 * i] = y[LS:LS + LP]
        y_prompt[2 * i + 1] = y[LS + LP:T]
        n_re[2 * i:2 * i + 2] = r[i]["ns_re"]
        n_im[2 * i:2 * i + 2] = r[i]["ns_im"]
    return (y_prompt, y_sample, n_re, n_im)
```

```python
import math
from contextlib import ExitStack

import numpy as np
import concourse.bass as bass
import concourse.mybir as mybir
from concourse.bass_utils import run_bass_kernel_spmd

F32 = mybir.dt.float32
BF16 = mybir.dt.bfloat16
I32 = mybir.dt.int32
AF = mybir.ActivationFunctionType
ALU = mybir.AluOpType

NCORES = 8
D = 1024
KT = D // 128
DEPTH = 2
LS = 2048
LP = 256
T = LS + 2 * LP
NTB = T // 512
IN_W = 7168
EPS = 1e-6
S5_G = 32
S5_P = 64
S5_H = 16
R1N = 32256
MAGIC = 12582912.0
TWO_PI_S = 2 * math.pi * (1 - 1e-6)
HY_SHIFT = 0.05

C_UA, C_GA, C_UB, C_GB, C_UC, C_GC, C_M = 0, 512, 1024, 2560, 3072, 3584, 4096


class StopBuild(Exception):
    pass


class Sched:
    def __init__(self, nc, es, n_dma_sems=24):
        self.nc = nc
        self.eng = {"pe": nc.tensor, "dve": nc.vector, "act": nc.scalar, "pool": nc.gpsimd, "sp": nc.sync}
        self.sem = {k: es.enter_context(nc.semaphore("s_" + k)) for k in self.eng}
        self.cnt = {k: 0 for k in self.eng}
        self.dsem = [es.enter_context(nc.semaphore("d%d" % i)) for i in range(n_dma_sems)]
        self.dcnt = [0] * n_dma_sems
        self.dnext = 0
        self.semobj = {}
        for k in self.eng:
            self.semobj[("e", k)] = self.sem[k]
        for i, s in enumerate(self.dsem):
            self.semobj[("d", i)] = s
        self.waited = {k: {} for k in self.eng}
        self.last_w = {}
        self.readers = {}
        self.nwaits = 0

    def _deps(self, e, r, w):
        need = {}

        def add(ev, self_ok):
            if ev is None:
                return
            sid, val = ev
            if sid == ("e", e) and not self_ok:
                return
            if need.get(sid, 0) < val:
                need[sid] = val

        for k in r:
            add(self.last_w.get(k), e != "pe")
        for k in w:
            add(self.last_w.get(k), False)
            for ev in self.readers.get(k, {}).items():
                add(ev, False)
        wt = self.waited[e]
        for sid, val in need.items():
            if wt.get(sid, 0) >= val:
                continue
            self.eng[e].wait_ge(self.semobj[sid], val)
            wt[sid] = val
            self.nwaits += 1

    def _commit(self, ev, r, w):
        sid, val = ev
        for k in w:
            self.last_w[k] = ev
            self.readers[k] = {}
        for k in r:
            d = self.readers.setdefault(k, {})
            if d.get(sid, 0) < val:
                d[sid] = val

    def op(self, e, fn, r=(), w=()):
        self._deps(e, r, w)
        ins = fn(self.eng[e])
        self.cnt[e] += 1
        ins.then_inc(self.sem[e], 1)
        self._commit((("e", e), self.cnt[e]), r, w)
        return ins

    def dma(self, out, in_, r=(), w=(), q=None, **kw):
        if q is None:
            q = "pool" if type(out.tensor).__name__ == "DRamTensorHandle" else "sp"
        i = self.dnext
        self.dnext = (self.dnext + 1) % len(self.dsem)
        wt = self.waited[q]
        if self.dcnt[i] > 0 and wt.get(("d", i), 0) < self.dcnt[i]:
            self.eng[q].wait_ge(self.dsem[i], self.dcnt[i])
            wt[("d", i)] = self.dcnt[i]
        self._deps(q, r, w)
        ins = self.eng[q].dma_start(out=out, in_=in_, **kw)
        self.dcnt[i] += 16
        ins.then_inc(self.dsem[i], 16)
        self._commit((("d", i), self.dcnt[i]), r, w)
        return ins

    def barrier(self):
        for e in self.eng:
            wt = self.waited[e]
            for k in self.eng:
                if k != e and self.cnt[k] > wt.get(("e", k), 0):
                    self.eng[e].wait_ge(self.sem[k], self.cnt[k])
                    wt[("e", k)] = self.cnt[k]
            for i, s in enumerate(self.dsem):
                if self.dcnt[i] > wt.get(("d", i), 0):
                    self.eng[e].wait_ge(s, self.dcnt[i])
                    wt[("d", i)] = self.dcnt[i]

    def finish(self):
        for i, s in enumerate(self.dsem):
            if self.dcnt[i] > 0:
                self.eng["sp"].wait_ge(s, self.dcnt[i])
        for k in ("pe", "dve", "act", "pool"):
            if self.cnt[k] > 0:
                self.eng["sp"].wait_ge(self.sem[k], self.cnt[k])


class Builder:
    def __init__(self, dbg=None, parts="tpfba"):
        self.dbg = dbg or {}
        self.parts = parts
        self.nc = bass.Bass("TRN2", target_bir_lowering=False)
        self.es = ExitStack()
        self.uid = 0

    def dram_in(self, name, shape, dt=F32):
        return self.nc.dram_tensor(name, list(shape), dt, kind="ExternalInput").ap()

    def dram_out(self, name, shape, dt=F32):
        return self.nc.dram_tensor(name, list(shape), dt, kind="ExternalOutput").ap()

    def dram_tmp(self, name, shape, dt=F32):
        return self.nc.dram_tensor(name, list(shape), dt, kind="Internal").ap()

    def sb(self, name, shape, dt=F32):
        return self.es.enter_context(self.nc.sbuf_tensor(name, list(shape), dt))

    def build(self):
        with self.es:
            self._build()
        return self.nc

    def _build(self):
        nc = self.nc
        S = self.S = Sched(nc, self.es)
        op, dma = S.op, S.dma

        xin = self.dram_in("xin", [T, D])
        cond = self.dram_in("cond", [2, D])
        st_re = self.dram_in("st_re", [DEPTH, 2, S5_G, S5_P])
        st_im = self.dram_in("st_im", [DEPTH, 2, S5_G, S5_P])
        W = {}
        for name, shape in WEIGHT_SHAPES.items():
            W[name] = self.dram_in(name, shape)
        yout = self.dram_out("yout", [T, D])
        ns_re = self.dram_out("ns_re", [2, DEPTH, 2, S5_G, S5_P])
        ns_im = self.dram_out("ns_im", [2, DEPTH, 2, S5_G, S5_P])
        xsp = self.dram_tmp("xsp", [128, KT * T])
        xsp3 = xsp.rearrange("p (k t) -> p k t", k=KT)
        dbg_out = {}
        for name, shape in self.dbg.items():
            dbg_out[name] = self.dram_out("dbg_" + name, shape)

        R1 = self.sb("R1", [128, R1N], F32)
        Hh = self.sb("Hh", [128, KT * T], BF16)
        h3 = Hh[:].rearrange("p (k t) -> p k t", k=KT)
        x3 = R1[:, 0:KT * T].rearrange("p (k t) -> p k t", k=KT)
        R1b = R1[:].bitcast(BF16)
        Yv = [R1b[:, i * 4 * T:(i + 1) * 4 * T].rearrange("p (k t) -> p k t", k=4) for i in range(3)]
        SCR0 = 3 * 4 * T // 2
        stage = [self.sb("stage%d" % i, [128, 2048], F32) for i in range(2)]
        self.stage_i = 0
        wbuf = [self.sb("wbuf%d" % i, [128, 5120], BF16) for i in range(2)]
        self.wbuf_i = 0
        self.wbuf = wbuf
        self.R1 = R1
        self.slots = [wbuf[0][:, 0:4096], wbuf[1][:, 0:4096], stage[0][:].bitcast(BF16), stage[1][:].bitcast(BF16)]
        self.slot_keys = [("wbuf", 0), ("wbuf", 1), ("stage", 0), ("stage", 1)]
        self._slot_i = 0
        ident = self.sb("ident", [128, 128], F32)
        identb = self.sb("identb", [128, 128], BF16)
        onesm = self.sb("onesm", [128, 128], F32)
        iot = self.sb("iot", [128, 128], I32)
        modT = self.sb("modT", [128, 24, 2], F32)
        gmul = self.sb("gmul", [128, KT, 2], F32)
        bmod = self.sb("bmod", [128, 24], F32)
        normg = self.sb("normg", [128, KT], F32)
        condT = self.sb("condT", [128, KT, 2], F32)
        rstd = R1[:, 22528:22528 + T]
        smallv = self.sb("smallv", [128, 64], F32)
        xtm = [R1[:, 20480 + i * 1024:20480 + (i + 1) * 1024] for i in range(2)]
        self._sq = [R1[:, 25088 + i * 512:25088 + i * 512 + 256].bitcast(BF16) for i in range(4)]
        PS = [self.es.enter_context(nc.psum_tensor("ps%d" % i, [128, 512], F32)) for i in range(8)]
        self.ps_i = 0

        def psk(i):
            return ("ps", i)

        MODBANK = 7

        def next_ps():
            i = self.ps_i
            self.ps_i = (i + 1) % 7
            return i

        op("pool", lambda e: e.iota(iot[:], pattern=[[1, 128]], base=0, channel_multiplier=-1), w=["iot"])
        op("dve", lambda e: e.tensor_scalar(out=ident[:], in0=iot[:], scalar1=0, scalar2=None, op0=ALU.is_equal),
           r=["iot"], w=["ident"])
        op("dve", lambda e: e.tensor_copy(out=identb[:], in_=ident[:]), r=["ident"], w=["identb"])
        op("pool", lambda e: e.memset(onesm[:], 1.0 / D), w=["onesm"])
        self._onesb = self.sb("onesb", [128, 128], BF16)
        op("dve", lambda e: e.tensor_copy(out=self._onesb[:], in_=onesm[:]), r=["onesm"], w=["onesm"])

        def wload(dst, src, key, eng="act"):
            i = self.stage_i
            self.stage_i = (i + 1) % len(stage)
            n = 1
            for s in dst.shape[1:]:
                n *= s
            assert n <= 2048
            st = stage[i][:, 0:n]
            if len(dst.shape) == 3:
                st = st.rearrange("p (a b) -> p a b", a=dst.shape[1])
            dma(st, src, w=[("stage", i)])
            wk_ = list(key) if isinstance(key, list) else [key]
            if eng == "act":
                op("act", lambda e: e.activation(out=dst, in_=st, func=AF.Copy), r=[("stage", i)], w=wk_)
            else:
                op(eng, lambda e: e.tensor_copy(out=dst, in_=st), r=[("stage", i)], w=wk_)

        def wdirect(dst, src, key):
            dma(dst, src, w=[key])

        with nc.allow_non_contiguous_dma(reason="small transposed vector loads"):
            for j in range(2):
                dma(condT[:, :, j], cond[j].rearrange("(k p) -> p k", p=128), w=["condT"])
        op("act", lambda e: e.activation(out=condT[:], in_=condT[:], func=AF.Silu), r=["condT"], w=["condT"])

        def mod_start(l):
            for mc in range(12):
                i = self.stage_i
                self.stage_i = (i + 1) % len(stage)
                st = stage[i][:].rearrange("p (k c) -> p k c", k=KT)
                dma(st, W["w_mod"][l][:, mc * 256:(mc + 1) * 256].rearrange("(k p) c -> p k c", p=128),
                    w=[("stage", i)])
                for mm in range(2):
                    m = mc * 2 + mm
                    for kt in range(KT):
                        op("pe", lambda e, m=m, mm=mm, kt=kt, st=st: e.matmul(
                            PS[MODBANK][:, m * 2:m * 2 + 2], st[:, kt, mm * 128:(mm + 1) * 128], condT[:, kt, :],
                            start=(kt == 0), stop=(kt == KT - 1)),
                           r=[("stage", i), "condT"], w=[psk(MODBANK)])
        self.mod_start = mod_start
        mod_start(0)

        if "t" in self.parts:
            self._setup_tables()
        S.barrier()
        for tt in range(T // 128):
            b = tt % 2
            dma(xtm[b][:], xin[tt * 128:(tt + 1) * 128, :], w=[("xtm", b)])
            for half in range(2):
                pi = next_ps()
                for j in range(4):
                    kt = half * 4 + j
                    op("pe", lambda e, kt=kt, j=j, pi=pi: e.transpose(PS[pi][:, j * 128:(j + 1) * 128],
                                                                     xtm[b][:, kt * 128:(kt + 1) * 128], ident[:]),
                       r=[("xtm", b), "ident"], w=[psk(pi)])
                eng = "act" if half == 0 else "dve"
                src = PS[pi][:].rearrange("p (j t) -> p j t", j=4)
                dst = x3[:, half * 4:half * 4 + 4, tt * 128:(tt + 1) * 128]
                if eng == "act":
                    op("act", lambda e, dst=dst, src=src: e.activation(out=dst, in_=src, func=AF.Copy),
                       r=[psk(pi)], w=[("x", tt // 4)])
                else:
                    op("dve", lambda e, dst=dst, src=src: e.tensor_copy(out=dst, in_=src),
                       r=[psk(pi)], w=[("x", tt // 4)])

        if "p" in self.parts:
            self._pos_embed(x3)

        for tb in range(NTB):
            dma(xsp3[:, :, tb * 512:(tb + 1) * 512], x3[:, :, tb * 512:(tb + 1) * 512],
                r=[("x", tb)], w=[("xsp", f, tb) for f in range(KT)])


        for l in range(DEPTH):
            with nc.allow_non_contiguous_dma(reason="small transposed vector loads"):
                dma(bmod[:], W["b_mod"][l].rearrange("(m p) -> p m", p=128), w=["bmod"])
                dma(normg[:], W["norm_g"][l].rearrange("(k p) -> p k", p=128), w=["normg"])
            op("dve", lambda e: e.tensor_tensor(out=modT[:], in0=PS[MODBANK][:, 0:48].rearrange("p (m j) -> p m j", j=2),
                                                in1=bmod[:].unsqueeze(2).broadcast_to([128, 24, 2]), op=ALU.add),
               r=[psk(MODBANK), "bmod"], w=["modT"])
            op("dve", lambda e: e.tensor_scalar(out=gmul[:], in0=modT[:, 8:16, :], scalar1=1.0, scalar2=None,
                                                op0=ALU.add), r=["modT"], w=["gmul"])
            op("dve", lambda e: e.tensor_tensor(out=gmul[:], in0=gmul[:],
                                                in1=normg[:].unsqueeze(2).broadcast_to([128, KT, 2]), op=ALU.mult),
               r=["gmul", "normg"], w=["gmul"])

            if l > 0:
                for tb in range(NTB):
                    dma(x3[:, :, tb * 512:(tb + 1) * 512], xsp3[:, :, tb * 512:(tb + 1) * 512],
                        r=[("xsp", f, tb) for f in range(KT)], w=[("x", tb)])
            self._norm_to(x3, rstd, onesm, PS, next_ps, psk)
            for kt in range(KT):
                for (t0, t1, j) in ((0, LS, 0), (LS, T, 1)):
                    keys = [("x", tb) for tb in range(t0 // 512, (t1 + 511) // 512)]
                    hkeys = [("h", kt, tb) for tb in range(t0 // 512, (t1 + 511) // 512)]
                    op("dve", lambda e, kt=kt, t0=t0, t1=t1, j=j: e.scalar_tensor_tensor(
                        out=x3[:, kt, t0:t1], in0=x3[:, kt, t0:t1], scalar=gmul[:, kt, j:j + 1], op0=ALU.mult,
                        in1=rstd[:, t0:t1], op1=ALU.mult), r=keys + ["gmul", "rstd"], w=keys)
                    op("act", lambda e, kt=kt, t0=t0, t1=t1, j=j: e.activation(
                        out=h3[:, kt, t0:t1], in_=x3[:, kt, t0:t1], func=AF.Identity,
                        bias=modT[:, kt, j:j + 1], scale=1.0), r=keys + ["modT"], w=hkeys)
            if "h%d" % l in dbg_out:
                hf = self.sb("dbg_hf%d" % l, [128, T], F32)
                for kt in range(KT):
                    op("dve", lambda e, kt=kt: e.tensor_copy(out=hf[:], in_=h3[:, kt, :]),
                       r=[("h", kt, tb) for tb in range(NTB)], w=["dbg_hf"])
                    dma(dbg_out["h%d" % l][kt * 128:(kt + 1) * 128, :], hf[:], r=["dbg_hf"])

            def proj_load(col0, ntiles, m0):
                nm = min(2, ntiles - m0)
                bi = self.wbuf_i
                self.wbuf_i = (bi + 1) % 2
                wv = wbuf[bi][:, 0:KT * nm * 128].rearrange("p (k c) -> p k c", k=KT)
                c0 = col0 + m0 * 128
                wload(wv, W["w_in"][l][:, c0:c0 + nm * 128].rearrange("(k p) c -> p k c", p=128), ("wbuf", bi))
                return nm, bi, wv

            def prime(col0, ntiles):
                self._primed = (col0, proj_load(col0, ntiles, 0))

            def proj(col0, ntiles, evac):
                for m0 in range(0, ntiles, 2):
                    pr_ = getattr(self, "_primed", None)
                    if m0 == 0 and pr_ is not None and pr_[0] == col0:
                        nm, bi, wv = pr_[1]
                        self._primed = None
                    else:
                        nm, bi, wv = proj_load(col0, ntiles, m0)
                    for mm in range(nm):
                        for tb in range(NTB):
                            pi = next_ps()
                            for kt in range(KT):
                                op("pe", lambda e, pi=pi, kt=kt, mm=mm, tb=tb, wv=wv: e.matmul(
                                    PS[pi][:], wv[:, kt, mm * 128:(mm + 1) * 128], h3[:, kt, tb * 512:(tb + 1) * 512],
                                    start=(kt == 0), stop=(kt == KT - 1)),
                                   r=[("wbuf", bi), ("h", kt, tb)], w=[psk(pi)])
                            evac(m0 + mm, tb, pi)

            self.ctx = dict(l=l, W=W, op=op, dma=dma, PS=PS, next_ps=next_ps, psk=psk, proj=proj, wload=wload,
                            Yv=Yv, R1=R1, R1b=R1b, SCR0=SCR0, h3=h3, wbuf=wbuf, stage=stage, smallv=smallv,
                            ident=ident, identb=identb, dbg_out=dbg_out, modT=modT, x3=x3, xsp3=xsp3,
                            st_re=st_re, st_im=st_im, ns_re=ns_re, ns_im=ns_im)
            prime(C_UC, 4)
            S.barrier()
            self._branch_c()
            S.barrier()
            self._branch_b()
            if "a" in self.parts:
                prime(C_UA, 4)
            S.barrier()
            self._branch_a()
            self._phase2()
            S.barrier()

        S.barrier()
        self._final(x3, xsp3, rstd, onesm, PS, next_ps, psk, W, yout, xtm, ident)
        S.finish()

    def _norm_to(self, x3, rstd, onesm, PS, next_ps, psk):
        op = self.S.op
        sq = self._sq
        if not hasattr(self, "_epsb"):
            self._epsb = self.sb("epsb", [128, 1], F32)
            op("pool", lambda e: e.memset(self._epsb[:], EPS), w=["epsb"])
        si = 0
        for tb in range(NTB):
            pi = next_ps()
            for kt in range(KT):
                b = si % 4
                si += 1
                op("act", lambda e, b=b, kt=kt, tb=tb: e.activation(out=sq[b][:], in_=x3[:, kt, tb * 512:(tb + 1) * 512],
                                                                    func=AF.Square), r=[("x", tb)], w=[("sq", b)])
                op("pe", lambda e, b=b, kt=kt, pi=pi: e.matmul(PS[pi][:], self._onesb[:], sq[b][:], start=(kt == 0),
                                                               stop=(kt == KT - 1)),
                   r=[("sq", b), "onesm"], w=[psk(pi)])
            op("act", lambda e, pi=pi, tb=tb: e.activation(out=rstd[:, tb * 512:(tb + 1) * 512], in_=PS[pi][:],
                                                           func=AF.Sqrt, bias=self._epsb[:], scale=1.0),
               r=[psk(pi), "epsb"], w=["rstd"])
        op("dve", lambda e: e.reciprocal(out=rstd[:], in_=rstd[:]), r=["rstd"], w=["rstd"])

    def _branch_c(self):
        c = self.ctx
        op, dma, PS, next_ps, psk, W, l = c["op"], c["dma"], c["PS"], c["next_ps"], c["psk"], c["W"], c["l"]
        R1, SCR0, Yv = c["R1"], c["SCR0"], c["Yv"]
        nc = self.nc
        PAD = 8
        TP = T + 6 * PAD
        seqs = [(0, LS), (LS, LP), (LS + LP, LP)]
        offs = []
        o = 0
        for (t0, ln) in seqs:
            offs.append(o + PAD)
            o += ln + 2 * PAD
        ucp = R1[:, SCR0:SCR0 + 4 * TP].rearrange("p (k t) -> p k t", k=4)
        o2 = SCR0 + 4 * TP
        tmpw = R1[:, o2:o2 + TP]
        o2 += TP
        dmean = R1[:, o2:o2 + T // 2].bitcast(BF16)
        o2 += T // 2
        sg = Yv[2]
        assert o2 <= R1N
        if not hasattr(self, "_rcb"):
            self._rcb = self.sb("rcb", [128, 4, 2, 8], F32)
            i8 = self.sb("i8", [128, 8], F32)
            op("pool", lambda e: e.iota(i8[:], pattern=[[1, 8]], base=0, channel_multiplier=0,
                                        allow_small_or_imprecise_dtypes=True), w=["i8"])
            for k in range(4):
                win = 2 << k
                op("dve", lambda e, k=k, win=win: e.tensor_scalar(out=self._rcb[:, k, 0, :], in0=i8[:], scalar1=float(win // 2),
                                                                scalar2=float(win), op0=ALU.add, op1=ALU.min),
                   r=["i8"], w=["rcb"])
                op("dve", lambda e, k=k, win=win: e.tensor_scalar(out=self._rcb[:, k, 1, :], in0=i8[:], scalar1=-1.0,
                                                                scalar2=float(8 + win // 2), op0=ALU.mult, op1=ALU.add),
                   r=["i8"], w=["rcb"])
                op("dve", lambda e, k=k, win=win: e.tensor_scalar(out=self._rcb[:, k, 1, :], in0=self._rcb[:, k, 1, :],
                                                                scalar1=float(win), scalar2=None, op0=ALU.min),
                   r=["rcb"], w=["rcb"])
            op("dve", lambda e: e.reciprocal(out=self._rcb[:], in_=self._rcb[:]), r=["rcb"], w=["rcb"])
        rcb = self._rcb
        smallv = c["smallv"]
        with nc.allow_non_contiguous_dma(reason="small transposed vector loads"):
            dma(smallv[:, 0:4], W["pool_scale"][l].rearrange("(k p) -> p k", p=128), w=["pool_scale"])
        op("dve", lambda e: e.memset(ucp[:], 0.0), w=["ucp"])

        def evac_u(mi, tb, pi):
            if tb < 4:
                dst = ucp[:, mi, offs[0] + tb * 512: offs[0] + (tb + 1) * 512]
                op("act", lambda e: e.activation(out=dst, in_=PS[pi][:], func=AF.Copy), r=[psk(pi)], w=["ucp"])
            else:
                for s in range(2):
                    dst = ucp[:, mi, offs[1 + s]: offs[1 + s] + LP]
                    op("act", lambda e, s=s, dst=dst: e.activation(out=dst, in_=PS[pi][:, s * LP:(s + 1) * LP], func=AF.Copy),
                       r=[psk(pi)], w=["ucp"])

        def evac_g(mi, tb, pi):
            op("act", lambda e: e.activation(out=sg[:, mi, tb * 512:(tb + 1) * 512], in_=PS[pi][:], func=AF.Silu),
               r=[psk(pi)], w=[("y", 2, mi, tb)])

        c["proj"](C_UC, 4, evac_u)
        c["proj"](C_GC, 4, evac_g)
        pw = c["wbuf"][self.wbuf_i]
        bi = self.wbuf_i
        self.wbuf_i = (bi + 1) % 2
        pwv = pw[:, 0:512].rearrange("p (g c) -> p g c", g=4)
        c["wload"](pwv, W["pool_w"][l].rearrange("g p c -> p g c"), ("wbuf", bi))
        for gi in range(4):
            win = 2 << gi
            u = ucp[:, gi, :]
            cur = u
            n = TP
            sh = 1
            for lev in range(gi + 1):
                n2 = n - sh
                eng = "dve"
                src = cur
                op(eng, lambda e, src=src, sh=sh, n2=n2: e.tensor_tensor(out=tmpw[:, 0:n2], in0=src[:, 0:n2],
                                                                         in1=src[:, sh:sh + n2], op=ALU.add),
                   r=["ucp", "tmpw"], w=["tmpw"])
                cur = tmpw
                n = n2
                sh *= 2
            for si, (t0, ln) in enumerate(seqs):
                a = offs[si] - win // 2
                op("dve", lambda e, a=a, t0=t0, ln=ln, si=si: e.scalar_tensor_tensor(
                    out=dmean[:, t0:t0 + ln], in0=tmpw[:, a:a + ln], scalar=1.0 / win, op0=ALU.mult,
                    in1=u[:, offs[si]:offs[si] + ln], op1=ALU.subtract), r=["tmpw", "ucp"], w=["dmean"])
                for side, p0 in ((0, 0), (1, ln - 8)):
                    tt = self._tmp8 if hasattr(self, "_tmp8") else None
                    if tt is None:
                        tt = self._tmp8 = self.sb("tmp8", [128, 8], F32)
                    op("dve", lambda e, a=a, p0=p0, side=side: e.tensor_tensor(
                        out=tt[:], in0=tmpw[:, a + p0:a + p0 + 8], in1=rcb[:, gi, side, :], op=ALU.mult),
                       r=["tmpw", "rcb", "tmp8"], w=["tmp8"])
                    op("dve", lambda e, p0=p0, t0=t0, si=si: e.tensor_tensor(
                        out=dmean[:, t0 + p0:t0 + p0 + 8], in0=tt[:], in1=u[:, offs[si] + p0:offs[si] + p0 + 8],
                        op=ALU.subtract), r=["tmp8", "ucp", "dmean"], w=["dmean"])
            for tb in range(NTB):
                pi = next_ps()
                op("pe", lambda e, pi=pi, tb=tb: e.matmul(PS[pi][:], pwv[:, gi, :], dmean[:, tb * 512:(tb + 1) * 512],
                                                          start=True, stop=True),
                   r=[("wbuf", bi), "dmean"], w=[psk(pi)])
                op("dve", lambda e, pi=pi, tb=tb: e.scalar_tensor_tensor(
                    out=Yv[2][:, gi, tb * 512:(tb + 1) * 512], in0=PS[pi][:], scalar=smallv[:, gi:gi + 1], op0=ALU.mult,
                    in1=sg[:, gi, tb * 512:(tb + 1) * 512], op1=ALU.mult),
                   r=[psk(pi), "pool_scale", ("y", 2, gi, tb)], w=[("y", 2, gi, tb)])
        if "yc%d" % l in c["dbg_out"]:
            self._dump_bf16(Yv[2], c["dbg_out"]["yc%d" % l], [("y", 2, gi, tb) for gi in range(4) for tb in range(NTB)])

    def _dump_bf16(self, src3, dst, keys):
        op, dma = self.S.op, self.S.dma
        if not hasattr(self, "_dbgf"):
            self._dbgf = self.sb("dbgf", [128, T], F32)
        for k in range(src3.shape[1]):
            op("dve", lambda e, k=k: e.tensor_copy(out=self._dbgf[:], in_=src3[:, k, :]), r=keys + ["dbgf"], w=["dbgf"])
            dma(dst[k * 128:(k + 1) * 128, :], self._dbgf[:], r=["dbgf"], w=["dbgo"])

    def _zero_y(self, i):
        op = self.S.op
        Yv = self.ctx["Yv"]
        op("pool", lambda e: e.memset(Yv[i][:], 0.0), w=[("y", i, gi, tb) for gi in range(4) for tb in range(NTB)])

    def _branch_b(self):
        self._zero_y(1)

    def _branch_a(self):
        try:
            self._branch_a_impl()
        except StopBuild:
            self.S.barrier()
            self._zero_y(0)

    def _ka(self, n):
        import os
        if int(os.environ.get("KA_STOP", "99")) == n:
            raise StopBuild()

    def _branch_a_impl(self):
        c = self.ctx
        if "a" not in self.parts:
            self._zero_y(0)
            return
        op, dma, PS, next_ps, psk, W, l = c["op"], c["dma"], c["PS"], c["next_ps"], c["psk"], c["W"], c["l"]
        R1, SCR0, Yv, ident, identb = c["R1"], c["SCR0"], c["Yv"], c["ident"], c["identb"]
        nc, S = self.nc, self.S
        NCH = T // 8
        CP = NCH
        SEQC = [(1, 256, 0), (259, 32, 256), (293, 32, 288)]
        if not hasattr(self, "_uaD"):
            self._uaD = self.dram_tmp("uaD", [4, 128, 8 * NCH], BF16)
            self._y2D = self.dram_tmp("y2D", [2, 128, 16 * CP], BF16)
            self._s5m = self.sb("s5m", [128, 256], BF16)
            self._s5n = self.sb("s5n", [128, 16], F32)
            msk = self._s5m
            ii = R1[:, SCR0:SCR0 + 130].bitcast(I32)
            thr = R1[:, SCR0 + 256:SCR0 + 258]
            colf = R1[:, SCR0 + 512:SCR0 + 640]
            op("pool", lambda e: e.iota(ii[:, 0:1], pattern=[[0, 1]], base=0, channel_multiplier=1), w=["s5i"])
            op("dve", lambda e: e.tensor_scalar(out=ii[:, 0:1], in0=ii[:, 0:1], scalar1=4, scalar2=None, op0=ALU.arith_shift_right),
               r=["s5i"], w=["s5i"])
            op("dve", lambda e: e.tensor_scalar(out=ii[:, 0:1], in0=ii[:, 0:1], scalar1=4, scalar2=None, op0=ALU.logical_shift_left),
               r=["s5i"], w=["s5i"])
            op("pool", lambda e: e.iota(ii[:, 2:130], pattern=[[1, 128]], base=0, channel_multiplier=0), r=["s5i"], w=["s5i"])
            op("dve", lambda e: e.tensor_copy(out=thr[:, 0:1], in_=ii[:, 0:1]), r=["s5i"], w=["s5thr"])
            op("dve", lambda e: e.tensor_scalar(out=thr[:, 1:2], in0=thr[:, 0:1], scalar1=15.0, scalar2=None, op0=ALU.add),
               r=["s5thr"], w=["s5thr"])
            op("dve", lambda e: e.tensor_copy(out=colf, in_=ii[:, 2:130]), r=["s5i"], w=["s5col"])
            op("dve", lambda e: e.tensor_scalar(out=msk[:, 0:128], in0=colf, scalar1=thr[:, 0:1], scalar2=None, op0=ALU.is_ge),
               r=["s5col", "s5thr"], w=["s5mask"])
            op("dve", lambda e: e.tensor_scalar(out=msk[:, 128:256], in0=colf, scalar1=thr[:, 1:2], scalar2=None, op0=ALU.is_le),
               r=["s5col", "s5thr"], w=["s5mask"])
            op("pool", lambda e: e.iota(self._s5n[:], pattern=[[1, 16]], base=-7, channel_multiplier=0,
                                        allow_small_or_imprecise_dtypes=True), w=["s5nvec"])
            S.barrier()
        maskf, maskb, nvec = self._s5m[:, 0:128], self._s5m[:, 128:256], self._s5n[:]
        uaD, y2D = self._uaD, self._y2D

        o = 0
        uaj = [R1[:, o + i * 1280:o + (i + 1) * 1280].bitcast(BF16).rearrange("p (s c) -> p s c", s=8) for i in range(2)]
        o += 2560

        def evac_u(mi, tb, pi):
            dst = uaj[mi % 2][:, :, tb * 64:(tb + 1) * 64]
            src = PS[pi][:].rearrange("p (c s) -> p s c", s=8)
            if tb % 2:
                op("act", lambda e: e.activation(out=dst, in_=src, func=AF.Copy), r=[psk(pi)], w=[("uaj", mi % 2)])
            else:
                op("dve", lambda e: e.tensor_copy(out=dst, in_=src), r=[psk(pi)], w=[("uaj", mi % 2)])
            if tb == NTB - 1:
                dma(uaD[mi].rearrange("p (s c) -> p s c", s=8), uaj[mi % 2], r=[("uaj", mi % 2)], w=[("uaD", mi)])
        c["proj"](C_UA, 4, evac_u)

        self._ka(1)
        o = 2560
        Ap = R1[:, o:o + 512].rearrange("p (r x n) -> p r x n", r=2, x=16); o += 512
        BB = R1[:, o:o + 512].rearrange("p (r x h) -> p r x h", r=2, x=16); o += 512
        CT = R1[:, o:o + 512].rearrange("p (r x h) -> p r x h", r=2, x=16); o += 512
        sm = R1[:, o:o + 512]; o += 512
        Braw = R1[:, o:o + 512].rearrange("p (r x h) -> p r x h", r=2, x=16); o += 512
        assert o <= 5120
        o = 0
        t1 = R1[:, o:o + 1024]; o += 1024
        t2 = R1[:, o:o + 1024]; o += 1024
        CN = R1[:, o:o + 128]; o += 128
        cur = R1[:, o:o + 32].rearrange("p (r d g) -> p r d g", r=2, d=2); o += 32
        curP = R1[:, o:o + 32].rearrange("p (r d g) -> p r d g", r=2, d=2); o += 32
        T1 = R1[:, o:o + 32].rearrange("p (r d g) -> p r d g", r=2, d=2); o += 32
        T2 = R1[:, o:o + 32].rearrange("p (r d g) -> p r d g", r=2, d=2); o += 32
        ARt = R1[:, o:o + 32].rearrange("p (r d g) -> p r d g", r=2, d=2); o += 32
        AIt = R1[:, o:o + 32].rearrange("p (r d g) -> p r d g", r=2, d=2); o += 32
        h0 = R1[:, o:o + 32].rearrange("p (r d g) -> p r d g", r=2, d=2); o += 32
        assert o <= 2560
        o = SCR0
        U2 = R1[:, o:o + 8 * CP].bitcast(BF16).rearrange("p (g c) -> p g c", g=16); o += 8 * CP
        Sx = R1[:, o:o + 16 * CP].bitcast(BF16).rearrange("p (r d g c) -> p r d g c", r=2, d=2, g=8); o += 16 * CP
        BS = R1[:, o:o + 2048].bitcast(BF16).rearrange("p (r x k) -> p r x k", r=2, x=16); o += 2048
        MI = R1[:, o:o + 1024].bitcast(BF16).rearrange("p (g k) -> p g k", g=16); o += 1024
        oXS = o
        xs_ = [R1[:, o + i * 320:o + (i + 1) * 320] for i in range(2)]; o += 640
        oTG = o
        tg_ = [R1[:, o + i * 320:o + (i + 1) * 320] for i in range(2)]; o += 640
        oRB = o
        RR = R1[:, o:o + 2048].bitcast(BF16).rearrange("p (r x k) -> p r x k", r=2, x=16); o += 2048
        BST = R1[:, o:o + 2048].bitcast(BF16).rearrange("p (r x q) -> p r x q", r=2, x=32); o += 2048
        assert o <= R1N, o
        Y2 = R1[:, oRB:oRB + 8 * CP].bitcast(BF16).rearrange("p (g c) -> p g c", g=16)
        yj = Yv[0].rearrange("p k (t c) -> p k t c", t=8)
        smv = lambda i: sm[:, i * 16:(i + 1) * 16]

        def swapped(ap4):
            a = ap4.ap
            return bass.AP(ap4.tensor, ap4.offset + a[1][0], [list(a[0]), [-a[1][0], 2]] + [list(x) for x in a[2:]])

        def cmul(out_re, out_im, a_re, a_im, b_re, b_im, shape, rk, wk, neg_im=False):
            n = 1
            for v in shape[1:]:
                n *= v
            names = "abcd"[:len(shape) - 1]
            pat = "p (%s) -> p %s" % (" ".join(names), " ".join(names))
            kw = {names[i]: shape[i + 1] for i in range(len(names) - 1)}
            v1 = t1[:, 0:n].rearrange(pat, **kw) if len(shape) > 2 else t1[:, 0:n]
            v2 = t2[:, 0:n].rearrange(pat, **kw) if len(shape) > 2 else t2[:, 0:n]
            op("dve", lambda e: e.tensor_tensor(out=v1, in0=a_re, in1=b_re, op=ALU.mult), r=rk + ["t1"], w=["t1"])
            op("dve", lambda e: e.tensor_tensor(out=v2, in0=a_im, in1=b_im, op=ALU.mult), r=rk + ["t2"], w=["t2"])
            op("dve", lambda e: e.tensor_tensor(out=out_re, in0=v1, in1=v2, op=ALU.subtract), r=["t1", "t2"], w=wk)
            op("dve", lambda e: e.tensor_tensor(out=v1, in0=a_re, in1=b_im, op=ALU.mult), r=rk + ["t1"], w=["t1"])
            op("dve", lambda e: e.tensor_tensor(out=v2, in0=a_im, in1=b_re, op=ALU.mult), r=rk + ["t2"], w=["t2"])
            if neg_im:
                op("dve", lambda e: e.scalar_tensor_tensor(out=out_im, in0=v1, scalar=-1.0, op0=ALU.mult, in1=v2, op1=ALU.subtract),
                   r=["t1", "t2"], w=wk)
            else:
                op("dve", lambda e: e.tensor_tensor(out=out_im, in0=v1, in1=v2, op=ALU.add), r=["t1", "t2"], w=wk)

        with nc.allow_non_contiguous_dma(reason="small transposed vector loads"):
            dma(c["smallv"][:, 4:8], W["s5_d"][l].rearrange("(k p) -> p k", p=128), w=["s5dv"])
            dma(c["smallv"][:, 60:64], W["s5_b_glu"][l].rearrange("(k p) -> p k", p=128), w=["s5bg"])
        Dcol = self._dcol if hasattr(self, "_dcol") else None
        if Dcol is None:
            Dcol = self._dcol = self.sb("dcol", [128, 32], F32)
        with nc.allow_non_contiguous_dma(reason="small transposed vector loads"):
            for s_ in range(8):
                dma(Dcol[s_ * 16:(s_ + 1) * 16, :], W["s5_d"][l].rearrange("(g h) -> h g", h=16), w=["dcol"])

        for hf in range(2):
            G0 = 16 * hf
            S.barrier()
            with nc.allow_non_contiguous_dma(reason="small transposed parameter loads"):
                for d in range(2):
                    xs = slice(d * 8, (d + 1) * 8)
                    for gp in range(2):
                        pq = slice(gp * 64, (gp + 1) * 64)
                        dma(smv(0)[pq, xs], W["s5_lam_re"][l][d, G0:G0 + 16, :].rearrange("(g8 gp) p -> gp p g8", gp=2)[gp], w=["s5p"])
                        dma(smv(1)[pq, xs], W["s5_lam_im"][l][d, G0:G0 + 16, :].rearrange("(g8 gp) p -> gp p g8", gp=2)[gp], w=["s5p"])
                        dma(smv(2)[pq, xs], W["s5_log_dt"][l][d, G0:G0 + 16].rearrange("(g8 gp) -> gp g8", gp=2)[gp].partition_broadcast(64),
                            w=["s5p"])
                        for ri, nm in enumerate(("s5_b_re", "s5_b_im")):
                            dma(Braw[pq, ri, xs, :], W[nm][l][d, G0:G0 + 16].rearrange("(g8 gp) p h -> gp p g8 h", gp=2)[gp], w=["s5B"])
                        for ri, st in enumerate((c["st_re"], c["st_im"])):
                            dma(h0[pq, ri, d, :], st[l, d, G0:G0 + 16, :].rearrange("(g8 gp) p -> gp p g8", gp=2)[gp], w=["s5h0"])
            for d in range(2):
                for ri, nm in enumerate(("s5_c_re", "s5_c_im")):
                    for k2 in range(2):
                        srcC = W[nm][l][d, G0 + 8 * k2:G0 + 8 * k2 + 8].rearrange("g h p -> (g h) p")
                        dma(CN[:, 0:64], srcC, w=["CN"])
                        dma(CN[:, 64:128], srcC, w=["CN"])
                        pi = next_ps()
                        op("pe", lambda e: e.transpose(PS[pi][:, 0:128], CN, ident[:]), r=["CN", "ident"], w=[psk(pi)])
                        pv = PS[pi][:, 0:128].rearrange("p (j gp h) -> p j gp h", gp=2, h=16)
                        for gp in range(2):
                            op("dve", lambda e: e.tensor_copy(out=CT[gp * 64:(gp + 1) * 64, ri, d * 8 + 4 * k2:d * 8 + 4 * k2 + 4, :],
                                                              in_=pv[gp * 64:(gp + 1) * 64, :, gp, :]), r=[psk(pi)], w=["CT"])
            self._ka(2)
            op("act", lambda e: e.activation(out=smv(2), in_=smv(2), func=AF.Exp), r=["s5p"], w=["s5dt"])
            op("dve", lambda e: e.tensor_tensor(out=smv(3), in0=smv(0), in1=smv(2), op=ALU.mult), r=["s5p", "s5dt"], w=["s5lrd"])
            op("dve", lambda e: e.scalar_tensor_tensor(out=smv(4), in0=smv(1), scalar=1.0 / (2 * math.pi), op0=ALU.mult,
                                                       in1=smv(2), op1=ALU.mult), r=["s5p", "s5dt"], w=["s5th"])
            EA = t1[:, 0:256].rearrange("p (x n) -> p x n", x=16)
            UA = t2[:, 0:256].rearrange("p (x n) -> p x n", x=16)
            UC = t1[:, 256:512].rearrange("p (x n) -> p x n", x=16)
            TT = t2[:, 256:512].rearrange("p (x n) -> p x n", x=16)
            nb_ = nvec.unsqueeze(1).broadcast_to([128, 16, 16])
            op("dve", lambda e: e.tensor_tensor(out=EA, in0=smv(3).unsqueeze(2).broadcast_to([128, 16, 16]), in1=nb_, op=ALU.mult),
               r=["s5lrd", "s5nvec", "t1"], w=["t1"])
            op("dve", lambda e: e.tensor_tensor(out=UA, in0=smv(4).unsqueeze(2).broadcast_to([128, 16, 16]), in1=nb_, op=ALU.mult),
               r=["s5th", "s5nvec", "t2"], w=["t2"])
            op("act", lambda e: e.activation(out=EA, in_=EA, func=AF.Exp), r=["t1"], w=["t1"])
            op("dve", lambda e: e.tensor_scalar(out=UC, in0=UA, scalar1=0.25, scalar2=None, op0=ALU.add), r=["t2", "t1"], w=["t1b"])
            self.sin_turns(Ap[:, 1], UA, TT, ["t2"], ["ApS"], "t2b")
            self.sin_turns(Ap[:, 0], UC, TT, ["t1b"], ["ApC"], "t2b")
            op("dve", lambda e: e.tensor_tensor(out=Ap[:, 0], in0=Ap[:, 0], in1=EA, op=ALU.mult), r=["ApC", "t1"], w=["Ap"])
            op("dve", lambda e: e.tensor_tensor(out=Ap[:, 1], in0=Ap[:, 1], in1=EA, op=ALU.mult), r=["ApS", "t1", "Ap"], w=["Ap"])
            self._ka(3)
            a_re, a_im = Ap[:, 0, :, 8], Ap[:, 1, :, 8]
            op("dve", lambda e: e.tensor_scalar(out=smv(5), in0=a_re, scalar1=-1.0, scalar2=None, op0=ALU.add), r=["Ap"], w=["s5k"])
            op("dve", lambda e: e.tensor_tensor(out=smv(6), in0=smv(0), in1=smv(0), op=ALU.mult), r=["s5p"], w=["s5k6"])
            op("dve", lambda e: e.tensor_tensor(out=smv(7), in0=smv(1), in1=smv(1), op=ALU.mult), r=["s5p"], w=["s5k7"])
            op("dve", lambda e: e.tensor_tensor(out=smv(6), in0=smv(6), in1=smv(7), op=ALU.add), r=["s5k6", "s5k7"], w=["s5k6"])
            op("dve", lambda e: e.reciprocal(out=smv(6), in_=smv(6)), r=["s5k6"], w=["s5k6"])
            op("dve", lambda e: e.tensor_tensor(out=smv(7), in0=smv(5), in1=smv(0), op=ALU.mult), r=["s5k", "s5p", "s5k6"], w=["s5k7"])
            op("dve", lambda e: e.tensor_tensor(out=smv(8), in0=a_im, in1=smv(1), op=ALU.mult), r=["Ap", "s5p"], w=["s5k8"])
            op("dve", lambda e: e.tensor_tensor(out=smv(7), in0=smv(7), in1=smv(8), op=ALU.add), r=["s5k7", "s5k8"], w=["s5k7"])
            op("dve", lambda e: e.tensor_tensor(out=smv(9), in0=smv(7), in1=smv(6), op=ALU.mult), r=["s5k7", "s5k6"], w=["s5kre"])
            op("dve", lambda e: e.tensor_tensor(out=smv(7), in0=a_im, in1=smv(0), op=ALU.mult), r=["Ap", "s5p", "s5kre"], w=["s5k7"])
            op("dve", lambda e: e.tensor_tensor(out=smv(8), in0=smv(5), in1=smv(1), op=ALU.mult), r=["s5k", "s5p", "s5k7"], w=["s5k8"])
            op("dve", lambda e: e.tensor_tensor(out=smv(7), in0=smv(7), in1=smv(8), op=ALU.subtract), r=["s5k7", "s5k8"], w=["s5k7"])
            op("dve", lambda e: e.tensor_tensor(out=smv(10), in0=smv(7), in1=smv(6), op=ALU.mult), r=["s5k7", "s5k6"], w=["s5kim"])
            bc = lambda v: v.unsqueeze(2).broadcast_to([128, 16, 16])
            cmul(BB[:, 0], BB[:, 1], bc(smv(9)), bc(smv(10)), Braw[:, 0], Braw[:, 1], [128, 16, 16], ["s5kre", "s5kim", "s5B"], ["BB"])
            self._ka(4)
            def pw(ri, d, start, step):
                a = Ap[:, ri, d * 8:(d + 1) * 8, :]
                aa = a.ap
                return bass.AP(a.tensor, a.offset + start * aa[2][0], [list(aa[0]), list(aa[1]), [step * aa[2][0], 8], [0, 16]])
            for d in range(2):
                xs = slice(d * 8, (d + 1) * 8)
                bsv = lambda ri: BS[:, ri, xs, :].rearrange("p g (s h) -> p g s h", s=8)
                rrv = lambda ri: RR[:, ri, xs, :].rearrange("p g (s h) -> p g s h", s=8)
                bbv = lambda ri: BB[:, ri, xs, :].unsqueeze(2).broadcast_to([128, 8, 8, 16])
                ctv = lambda ri: CT[:, ri, xs, :].unsqueeze(2).broadcast_to([128, 8, 8, 16])
                st_b, sp_b = (14, -1) if d == 0 else (7, 1)
                st_r, sp_r = (0, 1) if d == 0 else (7, -1)
                cmul(bsv(0), bsv(1), pw(0, d, st_b, sp_b), pw(1, d, st_b, sp_b), bbv(0), bbv(1), [128, 8, 8, 16],
                     ["Ap", "BB"], [("BS", d)])
                cmul(rrv(0), rrv(1), pw(0, d, st_r, sp_r), pw(1, d, st_r, sp_r), ctv(0), ctv(1), [128, 8, 8, 16],
                     ["Ap", "CT"], [("RR", d)], neg_im=True)
            self._ka(5)
            Dd = t1[:, 0:512]
            for gp in range(2):
                ps_ = slice(gp * 64, (gp + 1) * 64)
                for b4 in range(2):
                    pf, pb = next_ps(), next_ps()
                    for gi in range(4):
                        g8 = b4 * 4 + gi
                        for d, pp in ((0, pf), (1, pb)):
                            for ri in range(2):
                                op("pe", lambda e: e.matmul(PS[pp][:, gi * 128:(gi + 1) * 128], BS[ps_, ri, d * 8 + g8, :],
                                                            RR[ps_, ri, d * 8 + g8, :], start=(ri == 0), stop=(ri == 1)),
                                   r=[("BS", d), ("RR", d)], w=[psk(pp)])
                    mk = lambda m: m.unsqueeze(1).broadcast_to([128, 4, 128])
                    pv = lambda pp: PS[pp][:].rearrange("p (g k) -> p g k", g=4)
                    tv = lambda t: t[:, 0:512].rearrange("p (g k) -> p g k", g=4)
                    op("dve", lambda e: e.tensor_tensor(out=tv(t2), in0=pv(pf), in1=mk(maskf), op=ALU.mult), r=[psk(pf), "s5mask", "t2", "t2b"], w=["t2"])
                    op("dve", lambda e: e.tensor_tensor(out=tv(t1), in0=pv(pb), in1=mk(maskb), op=ALU.mult), r=[psk(pb), "s5mask", "t1", "t1b"], w=["t1"])
                    op("dve", lambda e: e.tensor_tensor(out=tv(t2), in0=tv(t2), in1=tv(t1), op=ALU.add), r=["t1", "t2"], w=["t2"])
                    for gi in range(4):
                        g = 2 * (b4 * 4 + gi) + gp
                        op("dve", lambda e: e.scalar_tensor_tensor(out=MI[:, g, :], in0=ident[:], scalar=Dcol[:, G0 + g:G0 + g + 1],
                                                                   op0=ALU.mult, in1=t2[:, gi * 128:(gi + 1) * 128], op1=ALU.add),
                           r=["t2", "dcol", "ident"], w=[("MI", g)])
            self._ka(6)
            ei = 0
            for ri in range(2):
                for d in range(2):
                    for gp in range(2):
                        ps_ = slice(gp * 64, (gp + 1) * 64)
                        pi = next_ps()
                        pvb = PS[pi][:].bitcast(BF16)
                        for g8 in range(8):
                            op("pe", lambda e: e.transpose(pvb[:, g8 * 64:(g8 + 1) * 64], BS[ps_, ri, d * 8 + g8, :], identb[ps_, ps_]),
                               r=[("BS", d), "identb"], w=[psk(pi)])
                        bdst = BST[:, ri, d * 16:(d + 1) * 16, :].rearrange("p (g8 gp) q -> p g8 gp q", gp=2)[:, :, gp, :]
                        bsrc = pvb[:, 0:512].rearrange("p (g q) -> p g q", g=8)
                        ei += 1
                        if ei % 2:
                            op("act", lambda e: e.activation(out=bdst, in_=bsrc, func=AF.Copy), r=[psk(pi)], w=["BST"])
                        else:
                            op("dve", lambda e: e.tensor_copy(out=bdst, in_=bsrc), r=[psk(pi)], w=["BST"])
            self._ka(7)
            a8r = lambda d: Ap[:, 0, d * 8:(d + 1) * 8, 15].unsqueeze(2).broadcast_to([128, 8, 128])
            a8i = lambda d: Ap[:, 1, d * 8:(d + 1) * 8, 15].unsqueeze(2).broadcast_to([128, 8, 128])
            for d in range(2):
                xs = slice(d * 8, (d + 1) * 8)
                v1 = t1[:, 0:1024].rearrange("p (g k) -> p g k", g=8)
                v2 = t2[:, 0:1024].rearrange("p (g k) -> p g k", g=8)
                op("dve", lambda e: e.tensor_tensor(out=v1, in0=RR[:, 0, xs, :], in1=a8r(d), op=ALU.mult), r=[("RR", d), "Ap", "t1", "t1b"], w=["t1"])
                op("dve", lambda e: e.tensor_tensor(out=v2, in0=RR[:, 1, xs, :], in1=a8i(d), op=ALU.mult), r=[("RR", d), "Ap", "t2", "t2b"], w=["t2"])
                op("dve", lambda e: e.tensor_tensor(out=BS[:, 0, xs, :], in0=v1, in1=v2, op=ALU.add), r=["t1", "t2", "BST"], w=[("BS", d)])
                op("dve", lambda e: e.tensor_tensor(out=v1, in0=RR[:, 1, xs, :], in1=a8r(d), op=ALU.mult), r=[("RR", d), "Ap", "t1"], w=["t1"])
                op("dve", lambda e: e.tensor_tensor(out=v2, in0=RR[:, 0, xs, :], in1=a8i(d), op=ALU.mult), r=[("RR", d), "Ap", "t2"], w=["t2"])
                op("dve", lambda e: e.tensor_tensor(out=BS[:, 1, xs, :], in0=v1, in1=v2, op=ALU.subtract), r=["t1", "t2"], w=[("BS", d)])
            self._ka(8)
            for s_ in range(8):
                for k2 in range(2):
                    srcU = uaD[2 * hf + k2].rearrange("(g h) (s c) -> h g s c", h=16, s=8)[:, :, s_, :]
                    dma(U2[s_ * 16:(s_ + 1) * 16, 8 * k2:8 * k2 + 8, :], srcU, r=[("uaD", 2 * hf + k2), "U2"], w=["U2"])
            self._ka(9)
            for d in range(2):
                for ri in range(2):
                    for g8 in range(8):
                        pi = next_ps()
                        for gp in range(2):
                            op("pe", lambda e: e.matmul(PS[pi][gp * 64:(gp + 1) * 64, 0:CP], BST[:, ri, d * 16 + 2 * g8 + gp, :],
                                                        U2[:, 2 * g8 + gp, :], start=True, stop=True),
                               r=["BST", "U2"], w=[psk(pi)])
                        if g8 % 2:
                            op("act", lambda e: e.activation(out=Sx[:, ri, d, g8, :], in_=PS[pi][:, 0:CP], func=AF.Copy), r=[psk(pi)], w=["Sx"])
                        else:
                            op("dve", lambda e: e.tensor_copy(out=Sx[:, ri, d, g8, :], in_=PS[pi][:, 0:CP]), r=[psk(pi)], w=["Sx"])
            self._ka(10)
            S.barrier()
            if hf == 0 and l + 1 < DEPTH:
                self.mod_start(l + 1)
            NB, BL = 20, 16
            cur5 = t1[:, 0:640].rearrange("p (r d g b) -> p r d g b", r=2, d=2, g=8)
            Ta = t2[:, 0:640]
            Tb = R1[:, oXS:oXS + 640]
            CF = R1[:, oTG:oTG + 640].rearrange("p (r d g b) -> p r d g b", r=2, d=2, g=8)
            v4 = lambda a: a.rearrange("p (r x b) -> p r x b", r=2, x=16)
            v3 = lambda a: a.rearrange("p (r d m) -> p r d m", r=2, d=2)
            curF = t1[:, 0:640]
            CFf = R1[:, oTG:oTG + 640]
            oz = oRB
            PRt = R1[:, oz:oz + 512].rearrange("p (r x k) -> p r x k", r=2, x=16); oz += 512
            PIt = R1[:, oz:oz + 512].rearrange("p (r x k) -> p r x k", r=2, x=16); oz += 512
            E8 = R1[:, oz:oz + 256].rearrange("p (x k) -> p x k", x=16); oz += 256
            U8 = R1[:, oz:oz + 256].rearrange("p (x k) -> p x k", x=16); oz += 256
            U8c = R1[:, oz:oz + 256].rearrange("p (x k) -> p x k", x=16); oz += 256
            W8 = R1[:, oz:oz + 256].rearrange("p (x k) -> p x k", x=16); oz += 256
            n8 = R1[:, oz:oz + 16]; oz += 16
            h0b = R1[:, oRB + 4000:oRB + 4016].bitcast(BF16).rearrange("p (r d g) -> p r d g", r=2, d=2)
            Ts1 = R1[:, oz:oz + 32].rearrange("p (r d g) -> p r d g", r=2, d=2); oz += 32
            Ts2 = R1[:, oz:oz + 32].rearrange("p (r d g) -> p r d g", r=2, d=2); oz += 32
            op("pool", lambda e: e.iota(n8, pattern=[[8, 16]], base=8, channel_multiplier=0, allow_small_or_imprecise_dtypes=True), w=["n8"])
            n8b = n8.unsqueeze(1).broadcast_to([128, 16, 16])
            op("dve", lambda e: e.tensor_tensor(out=E8, in0=smv(3).unsqueeze(2).broadcast_to([128, 16, 16]), in1=n8b, op=ALU.mult),
               r=["s5lrd", "n8"], w=["E8"])
            op("dve", lambda e: e.tensor_tensor(out=U8, in0=smv(4).unsqueeze(2).broadcast_to([128, 16, 16]), in1=n8b, op=ALU.mult),
               r=["s5th", "n8"], w=["U8"])
            op("act", lambda e: e.activation(out=E8, in_=E8, func=AF.Exp), r=["E8"], w=["E8"])
            op("dve", lambda e: e.tensor_scalar(out=U8c, in0=U8, scalar1=0.25, scalar2=None, op0=ALU.add), r=["U8"], w=["U8c"])
            self.sin_turns(PIt[:, 1], U8, W8, ["U8"], ["PIs"], "W8")
            self.sin_turns(PRt[:, 0], U8c, W8, ["U8c"], ["PRc"], "W8")
            op("dve", lambda e: e.tensor_tensor(out=PRt[:, 0], in0=PRt[:, 0], in1=E8, op=ALU.mult), r=["PRc", "E8"], w=["PRt"])
            op("dve", lambda e: e.tensor_copy(out=PRt[:, 1], in_=PRt[:, 0]), r=["PRt"], w=["PRt"])
            op("dve", lambda e: e.tensor_tensor(out=PIt[:, 1], in0=PIt[:, 1], in1=E8, op=ALU.mult), r=["PIs", "E8"], w=["PIt"])
            op("dve", lambda e: e.tensor_scalar(out=PIt[:, 0], in0=PIt[:, 1], scalar1=-1.0, scalar2=None, op0=ALU.mult), r=["PIt"], w=["PIt"])
            op("dve", lambda e: e.tensor_copy(out=h0b, in_=h0), r=["s5h0"], w=["h0b"])
            bb = lambda tab, k: tab[:, :, :, k].unsqueeze(3).broadcast_to([128, 2, 16, NB])
            sm3 = lambda tab, k: tab[:, :, :, k].rearrange("p r (d g) -> p r d g", d=2)

            def sx_at(kf, kb):
                a = Sx.ap
                return bass.AP(Sx.tensor, Sx.offset + kf * a[4][0],
                               [list(a[0]), list(a[1]), [a[2][0] + (kb - kf) * a[4][0], 2], [BL * a[4][0], 8 * NB]])

            def blk_at(t5, bf, bb_):
                a = t5.ap
                return bass.AP(t5.tensor, t5.offset + bf * a[4][0], [list(a[0]), list(a[1]), [a[2][0] + (bb_ - bf) * a[4][0], 2], list(a[3])])

            def cstep(dst, src_c, src_add, PR, PI, big):
                ta = v4(Ta) if big else Ts1
                tb_ = v4(Tb) if big else Ts2
                kk = ["Ta", "Tb"] if big else ["Ts1", "Ts2"]
                op("dve", lambda e: e.tensor_tensor(out=ta, in0=src_c, in1=PR, op=ALU.mult), r=["scan", "PRt", "PIt"], w=[kk[0]])
                op("dve", lambda e: e.tensor_tensor(out=tb_, in0=swapped(src_c), in1=PI, op=ALU.mult), r=["scan", "PRt", "PIt"], w=[kk[1]])
                op("dve", lambda e: e.tensor_tensor(out=ta, in0=ta, in1=tb_, op=ALU.add), r=kk, w=[kk[0]])
                return ta
            op("dve", lambda e: e.memset(curF, 0.0), r=["t1", "t1b"], w=["scan"])
            for k in range(BL):
                ta = cstep(None, v4(curF), None, bb(PRt, 0), bb(PIt, 0), True)
                sxk = sx_at(k, BL - 1 - k)
                op("dve", lambda e: e.tensor_tensor(out=v3(curF), in0=v3(Ta), in1=sxk, op=ALU.add), r=["Ta", "Sx"], w=["scan"])
                op("act", lambda e: e.activation(out=sxk, in_=v3(curF), func=AF.Copy), r=["scan"], w=["Sx"])
            op("dve", lambda e: e.memset(CFf, 0.0), w=["scan"])
            op("dve", lambda e: e.tensor_copy(out=blk_at(CF, 0, 15), in_=h0), r=["s5h0", "scan"], w=["scan"])
            for (bf_dst, bb_dst, bf_src, bb_src) in ((17, 16, 16, 17), (19, 18, 18, 19)):
                op("dve", lambda e: e.tensor_copy(out=blk_at(CF, bf_dst, bb_dst), in_=blk_at(cur5, bf_src, bb_src)), r=["scan"], w=["scan"])
            for j in range(1, 16):
                prev = blk_at(CF, j - 1, 16 - j)
                ta = cstep(None, prev, None, sm3(PRt, 15), sm3(PIt, 15), False)
                op("dve", lambda e: e.tensor_tensor(out=blk_at(CF, j, 15 - j), in0=ta, in1=blk_at(cur5, j - 1, 16 - j), op=ALU.add),
                   r=["Ts1", "scan"], w=["scan"])
            for si, (bf_, bb_) in ((1, (17, 16)), (2, (19, 18))):
                ta = cstep(None, blk_at(CF, bf_, bb_), None, sm3(PRt, 15), sm3(PIt, 15), False)
                op("dve", lambda e: e.tensor_tensor(out=curP, in0=ta, in1=blk_at(cur5, bf_, bb_), op=ALU.add), r=["Ts1", "scan", "curP"], w=["curP"])
                with nc.allow_non_contiguous_dma(reason="small transposed state stores"):
                    for d in range(2):
                        for ri, dst in enumerate((c["ns_re"], c["ns_im"])):
                            for gp in range(2):
                                dma(dst[si - 1, l, d, G0:G0 + 16, :].rearrange("(g8 gp) p -> gp p g8", gp=2)[gp],
                                    curP[gp * 64:(gp + 1) * 64, ri, d, :], r=["curP"], w=[("ns", si, l, d, ri, hf, gp)])
            Ta2 = t1[:, 0:640]
            Tb2 = R1[:, oRB + 2400:oRB + 3040]
            for k in range(BL):
                ta_, tb2_, kk = (v4(Ta), v4(Tb), ["Ta", "Tb"]) if k % 2 == 0 else (v4(Ta2), v4(Tb2), ["Ta2", "Tb2"])
                rk3 = ["scan", "PRt", "PIt"] + (["Ts1"] if k < 2 else [])
                op("dve", lambda e: e.tensor_tensor(out=ta_, in0=v4(CFf), in1=bb(PRt, k), op=ALU.mult), r=rk3, w=[kk[0]])
                op("dve", lambda e: e.tensor_tensor(out=tb2_, in0=swapped(v4(CFf)), in1=bb(PIt, k), op=ALU.mult), r=rk3, w=[kk[1]])
                op("dve", lambda e: e.tensor_tensor(out=ta_, in0=ta_, in1=tb2_, op=ALU.add), r=kk, w=[kk[0]])
                sxk = sx_at(k, BL - 1 - k)
                tsrc = Ta if k % 2 == 0 else Ta2
                op("dve", lambda e: e.tensor_tensor(out=sxk, in0=v3(tsrc), in1=sxk, op=ALU.add), r=[kk[0], "Sx"], w=[("Sxk", k)])
            self._ka(11)
            S.barrier()
            SEQB = [(0, 256), (256, 32), (288, 32)]
            for g in range(16):
                g8, gp = g // 2, g % 2
                ps_ = slice(gp * 64, (gp + 1) * 64)
                pi = next_ps()
                mms = [(PS[pi][:, 0:CP], MI[:, g, :], U2[:, g, :], [("MI", g), "U2"])]
                for ri in range(2):
                    Of, Ob = BS[ps_, ri, g8, :], BS[ps_, ri, 8 + g8, :]
                    for (c0, n) in SEQB:
                        mms.append((PS[pi][:, c0 + 1:c0 + n], Of, Sx[ps_, ri, 0, g8, c0:c0 + n - 1], [("BS", 0), "Sx"]))
                        mms.append((PS[pi][:, c0:c0 + n - 1], Ob, Sx[ps_, ri, 1, g8, c0 + 1:c0 + n], [("BS", 1), "Sx"]))
                    mms.append((PS[pi][:, 0:1], Of, h0b[ps_, ri, 0, g8:g8 + 1], [("BS", 0), "h0b"]))
                    mms.append((PS[pi][:, 255:256], Ob, h0b[ps_, ri, 1, g8:g8 + 1], [("BS", 1), "h0b"]))
                for mi_, (o_, l_, r_, k_) in enumerate(mms):
                    op("pe", lambda e: e.matmul(o_, l_, r_, start=(mi_ == 0), stop=(mi_ == len(mms) - 1)), r=k_, w=[psk(pi)])
                xv, tv_ = xs_[g % 2], tg_[g % 2]
                op("act", lambda e: e.activation(out=xv, in_=PS[pi][:, 0:CP], func=AF.Copy), r=[psk(pi), "scan", "Tb"], w=[("xs", g % 2)])
                op("act", lambda e: e.activation(out=tv_, in_=PS[pi][:, 0:CP], func=AF.Square), r=[psk(pi), "scan"], w=[("tg", g % 2)])
                op("dve", lambda e: e.tensor_scalar(out=tv_, in0=tv_, scalar1=0.044715, scalar2=1.0, op0=ALU.mult, op1=ALU.add),
                   r=[("tg", g % 2)], w=[("tg", g % 2)])
                op("dve", lambda e: e.tensor_tensor(out=tv_, in0=tv_, in1=xv, op=ALU.mult), r=[("tg", g % 2), ("xs", g % 2)], w=[("tg", g % 2)])
                op("act", lambda e: e.activation(out=tv_, in_=tv_, func=AF.Sigmoid, scale=1.5957691216057308), r=[("tg", g % 2)], w=[("tg", g % 2)])
                op("dve", lambda e: e.tensor_tensor(out=Y2[:, g, :], in0=xv, in1=tv_, op=ALU.mult), r=[("tg", g % 2), ("xs", g % 2)], w=["Y2"])
            dma(y2D[hf].rearrange("p (g c) -> p g c", g=16), Y2, r=["Y2"], w=[("y2D", hf)])
        bi = self.wbuf_i
        self.wbuf_i = (bi + 1) % 2
        wg = self.wbuf[bi][:, 0:2048].rearrange("p (k c) -> p k c", k=4)
        c["wload"](wg, W["s5_w_glu"][l].rearrange("(k p) c -> p k c", p=128), ("wbuf", bi))
        S.barrier()
        self._ka(12)
        o = SCR0
        ygl = R1[:, o:o + 2 * T].bitcast(BF16).rearrange("p (k t c) -> p k t c", k=4, t=8); o += 2 * T
        sgt = [R1[:, o + i * 512:o + (i + 1) * 512] for i in range(2)]; o += 1024
        for k4 in range(4):
            hf, k2 = k4 // 2, k4 % 2
            for gl in range(8):
                srcY = y2D[hf].rearrange("(t h) (g c) -> h g t c", h=16, g=16)[:, 8 * k2 + gl, :, :]
                dma(ygl[gl * 16:(gl + 1) * 16, k4, :, :], srcY, r=[("y2D", hf)], w=[("ygl", k4)])
        self._ka(13)
        yglf = ygl.rearrange("p k t c -> p k (t c)")
        yjf = Yv[0]
        for m in range(4):
            for tb in range(NTB):
                ts = slice(tb * 512, (tb + 1) * 512)
                pi = next_ps()
                for k4 in range(4):
                    op("pe", lambda e: e.matmul(PS[pi][:], wg[:, k4, m * 128:(m + 1) * 128], yglf[:, k4, ts], start=(k4 == 0), stop=(k4 == 3)),
                       r=[("wbuf", bi), ("ygl", k4)], w=[psk(pi)])
                st_ = sgt[tb % 2]
                op("act", lambda e: e.activation(out=st_, in_=PS[pi][:], func=AF.Sigmoid, bias=c["smallv"][:, 60 + m:61 + m], scale=1.0),
                   r=[psk(pi), "s5bg"], w=[("sgt", tb % 2)])
                op("dve", lambda e: e.tensor_tensor(out=yjf[:, m, ts], in0=st_, in1=yglf[:, m, ts], op=ALU.mult),
                   r=[("sgt", tb % 2), ("ygl", m)], w=[("y", 0, m, tb2) for tb2 in range(NTB)])
        def evac_g(mi, tb, pi):
            st_ = sgt[tb % 2]
            op("act", lambda e: e.activation(out=st_, in_=PS[pi][:], func=AF.Silu), r=[psk(pi)], w=[("sgt", tb % 2)])
            dstv = yj[:, mi, :, tb * 64:(tb + 1) * 64]
            op("dve", lambda e: e.tensor_tensor(out=dstv, in0=st_.rearrange("p (c s) -> p s c", s=8), in1=dstv, op=ALU.mult),
               r=[("sgt", tb % 2)] + [("y", 0, mi, tb2) for tb2 in range(NTB)], w=[("y", 0, mi, tb2) for tb2 in range(NTB)])
        c["proj"](C_GA, 4, evac_g)

    def _phase2(self):
        c = self.ctx
        op, dma, PS, next_ps, psk, W, l = c["op"], c["dma"], c["PS"], c["next_ps"], c["psk"], c["W"], c["l"]
        R1, R1b, SCR0, Yv, h3, wbuf, modT, xsp3 = (c["R1"], c["R1b"], c["SCR0"], c["Yv"], c["h3"], c["wbuf"], c["modT"],
                                                    c["xsp3"])
        wload = c["wload"]
        mg = R1[:, SCR0:SCR0 + 4 * T].bitcast(BF16).rearrange("p (k t) -> p k t", k=KT)
        o2 = SCR0 + 4 * T
        sig = [R1[:, o2 + i * 512:o2 + (i + 1) * 512] for i in range(3)]
        o2 += 3 * 512
        acc = [R1[:, o2 + i * 512:o2 + (i + 1) * 512] for i in range(2)]
        o2 += 2 * 512
        xo = [R1[:, o2 + i * 512:o2 + (i + 1) * 512] for i in range(3)]
        o2 += 3 * 512
        assert o2 <= R1N
        br_names = ["w_br_a", "w_br_b", "w_br_c"]

        def load_f(f, first):
            bi = self.wbuf_i
            self.wbuf_i = (bi + 1) % 2
            wm = wbuf[bi][:, 0:3 * KT * 128].rearrange("p (b k c) -> p b k c", b=3, k=KT)
            wb = wbuf[bi][:, 3072:3072 + 3 * 4 * 128].rearrange("p (b k c) -> p b k c", b=3, k=4)
            engs = ["act", "dve", "act"]
            for b in range(3):
                c0 = C_M + b * D + f * 128
                whole = [("wbuf", bi)] if first else []
                wload(wm[:, b], W["w_in"][l][:, c0:c0 + 128].rearrange("(k p) c -> p k c", p=128), [("wbuf", bi, "m", b)] + whole,
                      eng=engs[b])
                wload(wb[:, b], W[br_names[b]][l][:, f * 128:(f + 1) * 128].rearrange("(k p) c -> p k c", p=128),
                      [("wbuf", bi, "b", b)] + whole, eng=engs[b])
            return bi, wm, wb
        pre0 = load_f(0, True)
        self.S.barrier()
        for f in range(KT):
            bi, wm, wb = pre0 if f == 0 else load_f(f, False)
            for tb in range(NTB):
                ts = slice(tb * 512, (tb + 1) * 512)
                a = acc[tb % 2]
                for b in range(3):
                    pm = next_ps()
                    for kt in range(KT):
                        op("pe", lambda e, pm=pm, kt=kt, b=b: e.matmul(PS[pm][:], wm[:, b, kt, :], h3[:, kt, ts],
                                                                       start=(kt == 0), stop=(kt == KT - 1)),
                           r=[("wbuf", bi, "m", b), ("h", kt, tb)], w=[psk(pm)])
                    pp = next_ps()
                    for k4 in range(4):
                        yrhs = (Yv[b][:, k4, ts] if b else
                                Yv[0][:, k4, :].rearrange("p (t c) -> p c t", t=8)[:, tb * 64:(tb + 1) * 64, :])
                        op("pe", lambda e, pp=pp, k4=k4, b=b, yrhs=yrhs: e.matmul(PS[pp][:], wb[:, b, k4, :], yrhs,
                                                                       start=(k4 == 0), stop=(k4 == 3)),
                           r=[("wbuf", bi, "b", b), ("y", b, k4, tb)], w=[psk(pp)])
                    op("act", lambda e, pm=pm, b=b: e.activation(out=sig[b][:], in_=PS[pm][:], func=AF.Sigmoid),
                       r=[psk(pm)], w=[("sig", b)])
                    if b == 0:
                        op("dve", lambda e, pp=pp, b=b, a=a: e.tensor_tensor(out=a, in0=PS[pp][:], in1=sig[b][:], op=ALU.mult),
                           r=[psk(pp), ("sig", b)], w=[("acc", tb % 2)])
                    else:
                        op("dve", lambda e, pp=pp, b=b: e.tensor_tensor(out=sig[b][:], in0=PS[pp][:], in1=sig[b][:], op=ALU.mult),
                           r=[psk(pp), ("sig", b)], w=[("sig", b)])
                        last = (b == 2)
                        dst = mg[:, f, ts] if last else a
                        wk = [("mg", f, tb)] if last else [("acc", tb % 2)]
                        op("dve", lambda e, b=b, dst=dst, a=a: e.tensor_tensor(out=dst, in0=a, in1=sig[b][:], op=ALU.add),
                           r=[("acc", tb % 2), ("sig", b)], w=wk)
        if "mg%d" % l in c["dbg_out"]:
            self._dump_bf16(mg, c["dbg_out"]["mg%d" % l], [("mg", f, tb) for f in range(KT) for tb in range(NTB)])
        for f2 in range(0, KT, 2):
            bi = self.wbuf_i
            self.wbuf_i = (bi + 1) % 2
            wo = wbuf[bi][:, 0:KT * 256].rearrange("p (k c) -> p k c", k=KT)
            wload(wo, W["w_out"][l][:, f2 * 128:f2 * 128 + 256].rearrange("(k p) c -> p k c", p=128),
                  [("wbuf", bi)] + [("wbuf", bi, t_, b_) for t_ in ("m", "b") for b_ in range(3)])
            for ff in range(2):
                f = f2 + ff
                for tb in range(NTB):
                    ts = slice(tb * 512, (tb + 1) * 512)
                    j = 0 if tb < 4 else 1
                    xi = (f * NTB + tb) % 3
                    dma(xo[xi], xsp3[:, f, ts], r=[("xsp", f, tb)], w=[("xo", xi)])
                    pi = next_ps()
                    for kt in range(KT):
                        op("pe", lambda e, pi=pi, kt=kt, ff=ff: e.matmul(PS[pi][:], wo[:, kt, ff * 128:(ff + 1) * 128],
                                                                         mg[:, kt, ts], start=(kt == 0), stop=(kt == KT - 1)),
                           r=[("wbuf", bi), ("mg", kt, tb)], w=[psk(pi)])
                    op("dve", lambda e, pi=pi, xi=xi, f=f, j=j: e.scalar_tensor_tensor(
                        out=xo[xi], in0=PS[pi][:], scalar=modT[:, 16 + f, j:j + 1], op0=ALU.mult, in1=xo[xi], op1=ALU.add),
                       r=[psk(pi), ("xo", xi), "modT"], w=[("xo", xi)])
                    dma(xsp3[:, f, ts], xo[xi], r=[("xo", xi)], w=[("xsp", f, tb)])

    def _final(self, x3, xsp3, rstd, onesm, PS, next_ps, psk, W, yout, xtm, ident):
        op, dma = self.S.op, self.S.dma
        nc = self.nc
        fg = self.sb("fg", [128, KT], F32)
        with nc.allow_non_contiguous_dma(reason="small transposed vector loads"):
            dma(fg[:], W["final_g"].rearrange("(k p) -> p k", p=128), w=["fg"])
        for tb in range(NTB):
            dma(x3[:, :, tb * 512:(tb + 1) * 512], xsp3[:, :, tb * 512:(tb + 1) * 512],
                r=[("xsp", f, tb) for f in range(KT)], w=[("x", tb)])
        self._norm_to(x3, rstd, onesm, PS, next_ps, psk)
        for kt in range(KT):
            for tb in range(NTB):
                ts = slice(tb * 512, (tb + 1) * 512)
                op("dve", lambda e, kt=kt, ts=ts: e.scalar_tensor_tensor(
                    out=x3[:, kt, ts], in0=x3[:, kt, ts], scalar=fg[:, kt:kt + 1], op0=ALU.mult, in1=rstd[:, ts], op1=ALU.mult),
                   r=[("x", tb), "rstd", "fg"], w=[("x", tb)])
        for tt in range(T // 128):
            b = tt % 2
            tsl = slice(tt * 128, (tt + 1) * 128)
            for half in range(2):
                pi = next_ps()
                for j in range(4):
                    kt = half * 4 + j
                    op("pe", lambda e, kt=kt, j=j, pi=pi: e.transpose(PS[pi][:, j * 128:(j + 1) * 128], x3[:, kt, tsl], ident[:]),
                       r=[("x", tt // 4), "ident"], w=[psk(pi)])
                dst = xtm[b][:, half * 512:(half + 1) * 512]
                if half == 0:
                    op("act", lambda e, dst=dst, pi=pi: e.activation(out=dst, in_=PS[pi][:], func=AF.Copy),
                       r=[psk(pi)], w=[("xtm", b)])
                else:
                    op("dve", lambda e, dst=dst, pi=pi: e.tensor_copy(out=dst, in_=PS[pi][:]), r=[psk(pi)], w=[("xtm", b)])
            dma(yout[tsl, :], xtm[b][:], r=[("xtm", b)], w=[("yout", tt)])


    def sin_turns(self, out, u, tmp, rk, wk, tk, eng="dve"):
        op = self.S.op
        op(eng, lambda e: e.tensor_scalar(out=tmp, in0=u, scalar1=MAGIC, scalar2=MAGIC, op0=ALU.add, op1=ALU.subtract),
           r=rk, w=[tk])
        op(eng, lambda e: e.tensor_tensor(out=tmp, in0=u, in1=tmp, op=ALU.subtract), r=list(rk) + [tk], w=[tk])
        op("act", lambda e: e.activation(out=out, in_=tmp, func=AF.Sin, scale=TWO_PI_S), r=[tk], w=wk)

    def _pos_embed(self, x3):
        op = self.S.op
        R1 = self.R1
        o = 26112
        omg = R1[:, o:o + 2]; o += 2
        posv = R1[:, o:o + 64]; o += 64
        pu = R1[:, o:o + 384]; o += 384
        pt = R1[:, o:o + 384]; o += 384
        pe = R1[:, o:o + 384]; o += 384
        op("pool", lambda e: e.iota(omg, pattern=[[128, 2]], base=0, channel_multiplier=1,
                                    allow_small_or_imprecise_dtypes=True), w=["omg"])
        op("pool", lambda e: e.iota(posv, pattern=[[1, 64]], base=0, channel_multiplier=0,
                                    allow_small_or_imprecise_dtypes=True), w=["posv"])
        op("act", lambda e: e.activation(out=omg, in_=omg, func=AF.Exp, scale=-math.log(10000.0) / 256.0),
           r=["omg"], w=["omg"])
        op("dve", lambda e: e.tensor_scalar(out=omg, in0=omg, scalar1=1.0 / (2 * math.pi), scalar2=None, op0=ALU.mult),
           r=["omg"], w=["omg"])
        for kt in range(8):
            n = 32 if kt < 4 else 64
            off = kt * 32 if kt < 4 else 128 + (kt - 4) * 64
            ph = 0.25 if (kt % 4) >= 2 else 0.0
            c = kt % 2
            op("dve", lambda e, n=n, off=off, ph=ph, c=c: e.tensor_scalar(
                out=pu[:, off:off + n], in0=posv[:, 0:n], scalar1=omg[:, c:c + 1], scalar2=ph, op0=ALU.mult, op1=ALU.add),
               r=["posv", "omg"], w=["pu"])
        self.sin_turns(pe, pu, pt, ["pu"], ["pe"], "pt")
        for kt in range(8):
            xs = x3[:, kt, 0:LS].rearrange("p (r c) -> p r c", c=64)
            if kt < 4:
                tb_ = pe[:, kt * 32:(kt + 1) * 32].unsqueeze(2).broadcast_to([128, 32, 64])
            else:
                tb_ = pe[:, 128 + (kt - 4) * 64:128 + (kt - 3) * 64].unsqueeze(1).broadcast_to([128, 32, 64])
            op("dve", lambda e, xs=xs, tb_=tb_: e.tensor_tensor(out=xs, in0=xs, in1=tb_, op=ALU.add),
               r=["pe"] + [("x", b) for b in range(4)], w=[("x", b) for b in range(4)])

    def _setup_tables(self):
        op, dma = self.S.op, self.S.dma
        R1 = self.R1
        self.tabs = {}
        o = 0
        kk2 = R1[:, o:o + 1536]; o += 1536
        tpos = R1[:, o:o + 2048]; o += 2048
        mm = [R1[:, o + i * 2048:o + (i + 1) * 2048] for i in range(2)]; o += 4096
        qq = [R1[:, o + i * 2048:o + (i + 1) * 2048] for i in range(2)]; o += 4096
        ob = [[R1[:, o + (2 * i + c) * 1024:o + (2 * i + c + 1) * 1024].bitcast(BF16) for c in range(2)] for i in range(2)]
        o += 4096
        pcol = R1[:, o:o + 16]; o += 16
        kcol = R1[:, o:o + 16]; o += 16
        op("pool", lambda e: e.iota(pcol, pattern=[[128, 16]], base=0, channel_multiplier=1,
                                    allow_small_or_imprecise_dtypes=True), w=["pcol"])
        op("pool", lambda e: e.iota(kcol, pattern=[[256, 16]], base=1, channel_multiplier=2,
                                    allow_small_or_imprecise_dtypes=True), w=["kcol"])
        op("pool", lambda e: e.iota(kk2, pattern=[[2, 1536]], base=1, channel_multiplier=0,
                                    allow_small_or_imprecise_dtypes=True), w=["kk2"])
        cnt = [0]

        def gen(freev, pscal, Wd, N, sink):
            i = cnt[0] % 2
            cnt[0] += 1
            m, q = mm[i][:, 0:Wd], qq[i][:, 0:Wd]
            oc, os_ = ob[i][0][:, 0:Wd], ob[i][1][:, 0:Wd]
            op("dve", lambda e: e.tensor_scalar(out=m, in0=freev, scalar1=pscal, scalar2=None, op0=ALU.mult),
               r=["kk2", "tpos", "pcol", "kcol"], w=[("m", i)])
            op("dve", lambda e: e.tensor_scalar(out=q, in0=m, scalar1=1.0 / (2 * N), scalar2=MAGIC, op0=ALU.mult, op1=ALU.add),
               r=[("m", i)], w=[("q", i)])
            op("dve", lambda e: e.tensor_scalar(out=q, in0=q, scalar1=MAGIC, scalar2=-2.0 * N, op0=ALU.subtract, op1=ALU.mult),
               r=[("q", i)], w=[("q", i)])
            op("dve", lambda e: e.tensor_tensor(out=m, in0=m, in1=q, op=ALU.add), r=[("m", i), ("q", i)], w=[("m", i)])
            op("act", lambda e: e.activation(out=os_, in_=m, func=AF.Sin, scale=-math.pi / N * (1 - 1e-6)),
               r=[("m", i)], w=[("os", i)])
            op("dve", lambda e: e.scalar_tensor_tensor(out=q, in0=m, scalar=-1.0, op0=ALU.mult, in1=m, op1=ALU.max),
               r=[("m", i)], w=[("q", i)])
            op("act", lambda e: e.activation(out=oc, in_=q, func=AF.Sin, scale=-math.pi / N * (1 - 1e-6), bias=self._halfpi[:]),
               r=[("q", i), "halfpi"], w=[("oc", i)])
            sink(oc, os_, [("oc", i), ("os", i)])

        self._halfpi = self.sb("halfpi", [128, 1], F32)
        op("pool", lambda e: e.memset(self._halfpi[:], math.pi / 2 * (1 - 1e-6)), w=["halfpi"])
        for name, L in (("S", LS), ("P", LP)):
            N = 3 * L // 2
            NK = N // 2
            nI = L // 128
            nJ = (NK + 127) // 128
            kws = [min(128, NK - j * 128) for j in range(nJ)]
            Fc = self.dram_tmp("Fc" + name, [nJ, 128, nI, 128], BF16)
            Fs = self.dram_tmp("Fs" + name, [nJ, 128, nI, 128], BF16)
            Gc = self.dram_tmp("Gc" + name, [nJ, 128, L], BF16)
            Gs = self.dram_tmp("Gs" + name, [nJ, 128, L], BF16)
            self.tabs[name] = dict(L=L, N=N, NK=NK, nI=nI, nJ=nJ, kws=kws, Fc=Fc, Fs=Fs, Gc=Gc, Gs=Gs)
            op("pool", lambda e: e.iota(tpos[:, 0:L], pattern=[[1, L]], base=L // 2, channel_multiplier=0,
                                        allow_small_or_imprecise_dtypes=True), r=["tpos"], w=["tpos"])
            for I in range(nI):
                def sinkF(oc, os_, keys, I=I):
                    for j in range(nJ):
                        kw = kws[j]
                        dma(Fc[j, :, I, 0:kw], oc[:, j * 128:j * 128 + kw], r=keys, w=[("Fc" + name, j)], q="sp")
                        dma(Fs[j, :, I, 0:kw], os_[:, j * 128:j * 128 + kw], r=keys, w=[("Fs" + name, j)], q="sp")
                gen(kk2[:, 0:NK], pcol[:, I:I + 1], NK, N, sinkF)
            for j in range(nJ):
                def sinkG(oc, os_, keys, j=j):
                    dma(Gc[j], oc, r=keys, w=[("Gc" + name, j)], q="sp")
                    dma(Gs[j], os_, r=keys, w=[("Gs" + name, j)], q="sp")
                gen(tpos[:, 0:L], kcol[:, j:j + 1], L, N, sinkG)

    def _filters(self, name):
        c = self.ctx
        op, dma, PS, next_ps, psk, W, l = c["op"], c["dma"], c["PS"], c["next_ps"], c["psk"], c["W"], c["l"]
        R1, SCR0 = c["R1"], c["SCR0"]
        nc = self.nc
        tb = self.tabs[name]
        L, N, NK, nI, nJ, kws = tb["L"], tb["N"], tb["NK"], tb["nI"], tb["nJ"], tb["kws"]
        if "Hf" not in tb:
            tb["Hf"] = self.dram_tmp("Hf" + name, [nJ, 128, 2, 2, 512], BF16)
        Hf = tb["Hf"]
        o = SCR0
        htm = R1[:, o:o + nI * 512].bitcast(BF16).rearrange("p (i c) -> p i c", i=nI); o += 8192
        z = R1[0:65, o:o + L]; o += 2048
        h1 = R1[0:64, o:o + L]; o += 2048
        h2 = R1[0:64, o:o + L]; o += 2048
        tmp = R1[0:65, o:o + L]; o += 2048
        assert o <= R1N
        o = 0
        tio = R1[:, o:o + L]; o += 2048
        dl = R1[:, o:o + 512]; o += 512
        wt = R1[:, o:o + 512]; o += 512
        hst = [R1[:, o + i * 1024:o + (i + 1) * 1024].bitcast(BF16).rearrange("p (r q c) -> p r q c", r=2, q=2) for i in range(2)]
        o += 2048
        sv = self._fsv if hasattr(self, "_fsv") else None
        if sv is None:
            sv = self._fsv = self.sb("fsv", [128, 48], F32)
            self._w1p = self.sb("w1p", [65, 64], F32)
            self._w2 = self.sb("w2f", [64, 64], F32)
        w1p, w2 = self._w1p, self._w2
        w3 = R1[0:64, 5120:6144]
        op("pool", lambda e: e.iota(tio, pattern=[[1, L]], base=0, channel_multiplier=0,
                                    allow_small_or_imprecise_dtypes=True), w=["tio"])
        op("pool", lambda e: e.iota(dl, pattern=[[1, 512]], base=0, channel_multiplier=0,
                                    allow_small_or_imprecise_dtypes=True), w=["dl"])
        dmin = abs(math.log(1e-2) / 1.5)
        dmax = abs(math.log(1e-2) / 0.3)
        op("dve", lambda e: e.tensor_scalar(out=dl, in0=dl, scalar1=(dmax - dmin) / 511.0, scalar2=dmin, op0=ALU.mult, op1=ALU.add),
           r=["dl"], w=["dl"])
        op("pool", lambda e: e.memset(sv[:, 0:2], 0.0), w=["sv01"])
        for p0 in (0, 32):
            op("pool", lambda e, p0=p0: e.iota(sv[p0:p0 + 16, 0:1], pattern=[[0, 1]], base=0, channel_multiplier=1,
                                               allow_small_or_imprecise_dtypes=True), r=["sv01"], w=["sv01"])
            fstep = (15.0 - 1e-4) / 15.0
            op("dve", lambda e, p0=p0: e.tensor_scalar(out=sv[p0:p0 + 16, 0:1], in0=sv[p0:p0 + 16, 0:1], scalar1=fstep / L,
                                                       scalar2=1e-4 / L, op0=ALU.mult, op1=ALU.add), r=["sv01"], w=["sv01"])
            op("pool", lambda e, p0=p0: e.memset(sv[p0:p0 + 16, 1:2], 0.25 if p0 == 0 else 0.5), r=["sv01"], w=["sv01"])
        op("pool", lambda e: e.iota(sv[:, 8:8 + nI], pattern=[[128, nI]], base=-(L // 2), channel_multiplier=1,
                                    allow_small_or_imprecise_dtypes=True), w=["negoff"])
        op("dve", lambda e: e.scalar_tensor_tensor(out=sv[:, 24:24 + nI], in0=sv[:, 8:8 + nI], scalar=-1.0, op0=ALU.mult,
                                                   in1=sv[:, 8:8 + nI], op1=ALU.max), r=["negoff"], w=["negoff2"])
        op("dve", lambda e: e.tensor_scalar(out=sv[:, 8:8 + nI], in0=sv[:, 24:24 + nI], scalar1=-2.0 / L, scalar2=None,
                                            op0=ALU.mult), r=["negoff2"], w=["negoff"])
        op("pool", lambda e: e.memset(w1p[:], 0.0), w=["w1p"])
        with nc.allow_non_contiguous_dma(reason="small transposed vector loads"):
            dma(w1p[0:16, :], W["hy_f_w1"][l][1:17, :], w=["w1p"])
            dma(w1p[32:48, :], W["hy_f_w1"][l][17:33, :], w=["w1p"])
            dma(w1p[64:65, :], W["hy_f_w1"][l][0:1, :], w=["w1p"])
            dma(w2[:], W["hy_f_w2"][l], w=["w2f"])
            dma(w3[:], W["hy_f_w3"][l], w=["w3f"])
            dma(sv[0:64, 2:3], W["hy_f_b1"][l].rearrange("(p o) -> p o", o=1), w=["svw"])
            dma(sv[0:64, 3:4], W["hy_f_freq"][l].rearrange("(p o) -> p o", o=1), w=["svw"])
            dma(sv[0:64, 4:5], W["hy_f_b2"][l].rearrange("(p o) -> p o", o=1), w=["svw"])
        op("dve", lambda e: e.tensor_scalar(out=sv[0:64, 5:6], in0=sv[0:64, 3:4], scalar1=1.0 / (2 * math.pi), scalar2=None,
                                            op0=ALU.mult), r=["svw"], w=["svs"])
        op("dve", lambda e: e.tensor_tensor(out=sv[0:64, 6:7], in0=sv[0:64, 5:6], in1=sv[0:64, 2:3], op=ALU.mult),
           r=["svs", "svw"], w=["svc1"])
        op("dve", lambda e: e.tensor_tensor(out=sv[0:64, 7:8], in0=sv[0:64, 5:6], in1=sv[0:64, 4:5], op=ALU.mult),
           r=["svs", "svw"], w=["svc2"])
        op("dve", lambda e: e.tensor_scalar(out=tmp[0:64, :], in0=tio[0:64, :], scalar1=sv[0:64, 0:1], scalar2=sv[0:64, 1:2],
                                            op0=ALU.mult, op1=ALU.add), r=["tio", "sv01"], w=["fu"])
        self.sin_turns(z[0:64, :], tmp[0:64, :], h1[0:64, :], ["fu"], ["fz"], "ft")
        op("dve", lambda e: e.tensor_scalar(out=z[64:65, :], in0=tio[64:65, :], scalar1=1.0 / (L - 1), scalar2=None, op0=ALU.mult),
           r=["tio"], w=["fz64"])
        nb = (L + 511) // 512
        for (src, ks, wgt, wkey, ccol, dst, rkeys, dkey) in ((z, 65, w1p, "w1p", 6, h1, ["fz", "fz64"], "fh1"),
                                                             (h1, 64, w2, "w2f", 7, h2, ["fh1"], "fh2")):
            for b in range(nb):
                cs = slice(b * 512, min(L, (b + 1) * 512))
                pi = next_ps()
                wd = cs.stop - cs.start
                op("pe", lambda e: e.matmul(PS[pi][0:64, 0:wd], wgt[0:ks, :], src[0:ks, cs], start=True, stop=True),
                   r=rkeys + [wkey], w=[psk(pi)])
                op("dve", lambda e: e.tensor_scalar(out=tmp[0:64, cs], in0=PS[pi][0:64, 0:wd], scalar1=sv[0:64, 5:6],
                                                    scalar2=sv[0:64, ccol:ccol + 1], op0=ALU.mult, op1=ALU.add),
                   r=[psk(pi), "svs", "svc1", "svc2"], w=["fu"])
            self.sin_turns(dst[0:64, :], tmp[0:64, :], z[0:64, :] if dst is h1 else h1[0:64, :], ["fu"], [dkey],
                           "fz" if dst is h1 else "fh1")
        for I in range(nI):
            op("act", lambda e: e.activation(out=wt, in_=dl, func=AF.Exp, scale=sv[:, 8 + I:9 + I]), r=["dl", "negoff"], w=["wt"])
            for oo in range(2):
                pi = next_ps()
                op("pe", lambda e: e.matmul(PS[pi][:], h2[0:64, I * 128:(I + 1) * 128], w3[:, oo * 512:(oo + 1) * 512],
                                            start=True, stop=True), r=["fh2", "w3f"], w=[psk(pi)])
                op("dve", lambda e: e.scalar_tensor_tensor(out=htm[:, I, oo * 512:(oo + 1) * 512], in0=wt, scalar=HY_SHIFT,
                                                           op0=ALU.add, in1=PS[pi][:], op1=ALU.mult),
                   r=["wt", psk(pi)], w=[("htm", I)])
        for j in range(nJ):
            kw = kws[j]
            fc, fs, fkey = self._load_F(name, j)
            hs = hst[j % 2]
            for ri, ft in enumerate((fc, fs)):
                for oo in range(2):
                    pi = next_ps()
                    for I in range(nI):
                        op("pe", lambda e: e.matmul(PS[pi][0:kw, :], ft[:, I, 0:kw], htm[:, I, oo * 512:(oo + 1) * 512],
                                                    start=(I == 0), stop=(I == nI - 1)), r=[fkey, ("htm", I)], w=[psk(pi)])
                    if (ri + oo) % 2 == 0:
                        op("act", lambda e: e.activation(out=hs[0:kw, ri, oo, :], in_=PS[pi][0:kw, :], func=AF.Copy, scale=2.0 / N),
                           r=[psk(pi)], w=[("hst", j % 2)])
                    else:
                        op("dve", lambda e: e.tensor_scalar(out=hs[0:kw, ri, oo, :], in0=PS[pi][0:kw, :], scalar1=2.0 / N,
                                                            scalar2=None, op0=ALU.mult), r=[psk(pi)], w=[("hst", j % 2)])
            dma(Hf[j, 0:kw], hs[0:kw], r=[("hst", j % 2)], w=[("Hf" + name, j)])

    def _load_F(self, name, j):
        tb = self.tabs[name]
        nI = tb["nI"]
        si = self._slot_i
        self._slot_i = (si + 1) % 4
        key = self.slot_keys[si]
        fc = self.slots[si][:, 0:nI * 128].rearrange("p (i k) -> p i k", i=nI)
        fs = self.slots[si][:, 2048:2048 + nI * 128].rearrange("p (i k) -> p i k", i=nI)
        self.S.dma(fc, tb["Fc"][j], r=[("Fc" + name, j)], w=[key])
        self.S.dma(fs, tb["Fs"][j], r=[("Fs" + name, j)], w=[key])
        return fc, fs, key

    def _branch_b(self):
        c = self.ctx
        op, dma, PS, next_ps, psk, W, l = c["op"], c["dma"], c["PS"], c["next_ps"], c["psk"], c["W"], c["l"]
        R1, SCR0, Yv, identb = c["R1"], c["SCR0"], c["Yv"], c["identb"]
        nc = self.nc
        S = self.S
        if "f" in self.parts:
            self._filters("S")
            self._filters("P")
            S.barrier()
        if "b" not in self.parts:
            self._zero_y(1)
            return
        o = SCR0
        bufA = R1[:, o:o + 2 * T].bitcast(BF16).rearrange("p (k t) -> p k t", k=4); o += 2 * T
        bufBf = R1[:, o:o + 2 * T].bitcast(BF16); o += 2 * T
        tmv = bufBf.rearrange("p (i c) -> p i c", c=512)
        bufP = R1[:, o:o + 6144].bitcast(BF16).rearrange("p (j r c) -> p j r c", j=12, r=2); o += 6144
        assert o <= R1N
        TPAD = T + 12
        o = 0
        Cc = R1[:, o:o + TPAD]; o += TPAD
        Rr = R1[:, o:o + TPAD // 2].bitcast(BF16); o += TPAD // 2
        t12 = [R1[:, o + i * 512:o + (i + 1) * 512] for i in range(2)]; o += 1024
        assert o <= 5120
        hft = [self.wbuf[i][:, 4096:5120].rearrange("p (r c) -> p r c", r=2) for i in range(2)]
        seqs = [("S", 0, LS, 2), ("P", LS, LP, LS + 6), ("P", LS + LP, LP, LS + LP + 10)]
        sv = c["smallv"]
        with nc.allow_non_contiguous_dma(reason="small transposed vector loads"):
            for k in range(3):
                dma(sv[:, 8 + k * 12:8 + (k + 1) * 12], W["hy_conv_w"][l][k].rearrange("(m p) -> p m", p=128), w=["hyw"])
            dma(sv[:, 44:56], W["hy_conv_b"][l].rearrange("(m p) -> p m", p=128), w=["hyw"])
            for oo in range(2):
                dma(sv[:, 56 + oo * 4:60 + oo * 4], W["hy_bias"][l][oo].rearrange("(m p) -> p m", p=128), w=["hyw"])
        op("dve", lambda e: e.memset(Rr, 0.0), w=["Rr"])

        def proj_conv(part, dst, dkeyf):
            import os
            pc = int(os.environ.get("KB_PC", "9"))
            def evac(mi, tb, pi):
                m = part * 4 + mi
                segs = [(0, 512, 2 + tb * 512)] if tb < 4 else [(0, LP, LS + 6), (LP, 2 * LP, LS + LP + 10)]
                for (a, b, po) in segs:
                    op("dve", lambda e: e.tensor_copy(out=Rr[:, po:po + b - a], in_=PS[pi][:, a:b]), r=[psk(pi)], w=["Rr", ("rrseq", pi)])
                    op("act", lambda e: e.activation(out=Cc[:, po:po + b - a], in_=PS[pi][:, a:b], func=AF.Identity,
                                                     scale=sv[:, 8 + 12 + m:8 + 12 + m + 1], bias=sv[:, 44 + m:45 + m]),
                       r=[psk(pi), "hyw", ("rrseq", pi)], w=["Cc"])
                if tb == NTB - 1 and pc >= 3:
                    for (tn, t0, ln, po) in seqs:
                        op("dve", lambda e: e.scalar_tensor_tensor(out=Cc[:, po:po + ln], in0=Rr[:, po - 1:po - 1 + ln],
                                                                   scalar=sv[:, 8 + m:9 + m], op0=ALU.mult,
                                                                   in1=Cc[:, po:po + ln], op1=ALU.add),
                           r=["Rr", "Cc", "hyw"], w=["Cc"])
                        if pc >= 4:
                            op("dve", lambda e: e.scalar_tensor_tensor(out=dst[:, mi, t0:t0 + ln], in0=Rr[:, po + 1:po + 1 + ln],
                                                                   scalar=sv[:, 8 + 24 + m:8 + 24 + m + 1], op0=ALU.mult,
                                                                   in1=Cc[:, po:po + ln], op1=ALU.add),
                               r=["Rr", "Cc", "hyw"], w=[dkeyf(mi, tbb) for tbb in range(t0 // 512, (t0 + ln + 511) // 512)])
            c["proj"](C_UB + part * 512, 4, evac)

        def to_tm(src, skeyf):
            for tt in range(T // 128):
                pi = next_ps()
                pv = PS[pi][:].bitcast(BF16)
                for k in range(4):
                    op("pe", lambda e: e.transpose(pv[:, k * 128:(k + 1) * 128], src[:, k, tt * 128:(tt + 1) * 128], identb[:]),
                       r=[skeyf(k, tt // 4), "identb"], w=[psk(pi)])
                if tt % 2:
                    op("act", lambda e: e.activation(out=tmv[:, tt, :], in_=pv[:, 0:512], func=AF.Copy), r=[psk(pi)], w=[("tm", tt)])
                else:
                    op("dve", lambda e: e.tensor_copy(out=tmv[:, tt, :], in_=pv[:, 0:512]), r=[psk(pi)], w=[("tm", tt)])

        def forward(tn, t0, oo):
            tb = self.tabs[tn]
            nI, nJ, kws, Hf = tb["nI"], tb["nJ"], tb["kws"], tb["Hf"]
            I0 = t0 // 128
            for j in range(nJ):
                kw = kws[j]
                fc, fs, fkey = self._load_F(tn, j)
                hb = hft[j % 2]
                dma(hb[0:kw], Hf[j, 0:kw, :, oo, :], r=[("Hf" + tn, j)], w=[("hft", j % 2)])
                pr = next_ps()
                for I in range(nI):
                    op("pe", lambda e: e.matmul(PS[pr][0:kw, :], fc[:, I, 0:kw], tmv[:, I0 + I, :], start=(I == 0), stop=(I == nI - 1)),
                       r=[fkey, ("tm", I0 + I)], w=[psk(pr)])
                pim = next_ps()
                for I in range(nI):
                    op("pe", lambda e: e.matmul(PS[pim][0:kw, :], fs[:, I, 0:kw], tmv[:, I0 + I, :], start=(I == 0), stop=(I == nI - 1)),
                       r=[fkey, ("tm", I0 + I)], w=[psk(pim)])
                hk = [("hft", j % 2)]
                op("dve", lambda e: e.tensor_tensor(out=t12[0][0:kw], in0=PS[pr][0:kw, :], in1=hb[0:kw, 0, :], op=ALU.mult),
                   r=[psk(pr)] + hk, w=[("t12", 0)])
                op("dve", lambda e: e.tensor_tensor(out=t12[1][0:kw], in0=PS[pim][0:kw, :], in1=hb[0:kw, 1, :], op=ALU.mult),
                   r=[psk(pim)] + hk, w=[("t12", 1)])
                op("dve", lambda e: e.tensor_tensor(out=bufP[0:kw, j, 0, :], in0=t12[0][0:kw], in1=t12[1][0:kw], op=ALU.subtract),
                   r=[("t12", 0), ("t12", 1)], w=[("P", j)])
                op("dve", lambda e: e.tensor_tensor(out=t12[0][0:kw], in0=PS[pr][0:kw, :], in1=hb[0:kw, 1, :], op=ALU.mult),
                   r=[psk(pr)] + hk, w=[("t12", 0)])
                op("dve", lambda e: e.tensor_tensor(out=t12[1][0:kw], in0=PS[pim][0:kw, :], in1=hb[0:kw, 0, :], op=ALU.mult),
                   r=[psk(pim)] + hk, w=[("t12", 1)])
                op("dve", lambda e: e.tensor_tensor(out=bufP[0:kw, j, 1, :], in0=t12[0][0:kw], in1=t12[1][0:kw], op=ALU.add),
                   r=[("t12", 0), ("t12", 1)], w=[("P", j)])

        def inverse(tn, t0, evac):
            tb = self.tabs[tn]
            L, nJ, kws = tb["L"], tb["nJ"], tb["kws"]
            for b0 in range(0, L, 512):
                wd = min(512, L - b0)
                accs = [next_ps() for _ in range(4)]
                for j0 in range(0, nJ, 2):
                    si = self._slot_i
                    self._slot_i = (si + 1) % 4
                    gv = self.slots[si].rearrange("p (r j c) -> p r j c", r=2, j=2)
                    nj = min(2, nJ - j0)
                    for ri, tabn in enumerate(("Gc", "Gs")):
                        dma(gv[:, ri, 0:nj, 0:wd], tb[tabn][j0:j0 + nj, :, b0:b0 + wd].rearrange("j p c -> p j c"),
                            r=[(tabn + tn, j0 + jj) for jj in range(nj)], w=[self.slot_keys[si]])
                    for jj in range(nj):
                        j = j0 + jj
                        kw = kws[j]
                        for cc in range(4):
                            for ri in range(2):
                                op("pe", lambda e: e.matmul(PS[accs[cc]][:, 0:wd], bufP[0:kw, j, ri, cc * 128:(cc + 1) * 128],
                                                            gv[0:kw, ri, jj, 0:wd], start=(j == 0 and ri == 0),
                                                            stop=(j == nJ - 1 and ri == 1)),
                                   r=[("P", j), self.slot_keys[si]], w=[psk(accs[cc])])
                for cc in range(4):
                    evac(cc, t0 + b0, wd, accs[cc])

        kA = lambda k, tb_: ("bufA", k, tb_)
        kY = lambda k, tb_: ("y", 1, k, tb_)
        import os
        stop = int(os.environ.get("KB_STOP", "99"))
        proj_conv(0, bufA, kA)
        if stop == 1:
            S.barrier(); self._zero_y(1); return
        to_tm(bufA, kA)
        if stop == 2:
            S.barrier(); self._zero_y(1); return
        proj_conv(1, Yv[1], kY)
        if stop == 3:
            S.barrier(); self._zero_y(1); return
        if stop == 4:
            forward("S", 0, 0)
            S.barrier(); self._zero_y(1); return
        if stop == 5:
            forward("S", 0, 0)
            inverse("S", 0, lambda cc, ta, wd, pi: None)
            S.barrier(); self._zero_y(1); return

        def evac1(cc, ta, wd, pi):
            tbk = ta // 512
            ti = cc % 2
            op("dve", lambda e: e.scalar_tensor_tensor(out=t12[ti][:, 0:wd], in0=bufA[:, cc, ta:ta + wd], scalar=sv[:, 56 + cc:57 + cc],
                                                       op0=ALU.mult, in1=PS[pi][:, 0:wd], op1=ALU.add),
               r=[psk(pi), kA(cc, tbk), "hyw"], w=[("t12", ti)])
            op("dve", lambda e: e.tensor_tensor(out=Yv[1][:, cc, ta:ta + wd], in0=t12[ti][:, 0:wd], in1=Yv[1][:, cc, ta:ta + wd],
                                                 op=ALU.mult), r=[("t12", ti), kY(cc, tbk)], w=[kY(cc, tbk)])
        for (tn, t0, ln, po) in seqs:
            forward(tn, t0, 0)
            inverse(tn, t0, evac1)
        to_tm(Yv[1], kY)
        proj_conv(2, bufA, kA)

        def evac2(cc, ta, wd, pi):
            tbk = ta // 512
            ti = cc % 2
            op("dve", lambda e: e.scalar_tensor_tensor(out=t12[ti][:, 0:wd], in0=Yv[1][:, cc, ta:ta + wd], scalar=sv[:, 60 + cc:61 + cc],
                                                       op0=ALU.mult, in1=PS[pi][:, 0:wd], op1=ALU.add),
               r=[psk(pi), kY(cc, tbk), "hyw"], w=[("t12", ti)])
            op("dve", lambda e: e.tensor_tensor(out=bufA[:, cc, ta:ta + wd], in0=t12[ti][:, 0:wd], in1=bufA[:, cc, ta:ta + wd],
                                                 op=ALU.mult), r=[("t12", ti), kA(cc, tbk)], w=[kA(cc, tbk)])
        for (tn, t0, ln, po) in seqs:
            forward(tn, t0, 1)
            inverse(tn, t0, evac2)

        def evac_g(mi, tb, pi):
            ti = tb % 2
            op("act", lambda e: e.activation(out=t12[ti][:], in_=PS[pi][:], func=AF.Silu), r=[psk(pi)], w=[("t12", ti)])
            op("dve", lambda e: e.tensor_tensor(out=Yv[1][:, mi, tb * 512:(tb + 1) * 512], in0=t12[ti][:],
                                                 in1=bufA[:, mi, tb * 512:(tb + 1) * 512], op=ALU.mult),
               r=[("t12", ti), kA(mi, tb)], w=[kY(mi, tb)])
        c["proj"](C_GB, 4, evac_g)
        if "yb%d" % l in c["dbg_out"]:
            self._dump_bf16(Yv[1], c["dbg_out"]["yb%d" % l], [kY(k, tbb) for k in range(4) for tbb in range(NTB)])

    def _tmview(self, buf):
        return buf.rearrange("p k t -> p (k t)").rearrange("p (i c) -> p i c", c=512)


WEIGHT_SHAPES = {
    "norm_g": (2, 1024), "w_mod": (2, 1024, 3072), "b_mod": (2, 3072), "w_in": (2, 1024, 7168),
    "s5_lam_re": (2, 2, 32, 64), "s5_lam_im": (2, 2, 32, 64), "s5_log_dt": (2, 2, 32),
    "s5_b_re": (2, 2, 32, 64, 16), "s5_b_im": (2, 2, 32, 64, 16), "s5_c_re": (2, 2, 32, 16, 64),
    "s5_c_im": (2, 2, 32, 16, 64), "s5_d": (2, 512), "s5_w_glu": (2, 512, 512), "s5_b_glu": (2, 512),
    "hy_conv_w": (2, 3, 1536), "hy_conv_b": (2, 1536), "hy_f_w1": (2, 33, 64), "hy_f_b1": (2, 64),
    "hy_f_w2": (2, 64, 64), "hy_f_b2": (2, 64), "hy_f_freq": (2, 64), "hy_f_w3": (2, 64, 1024),
    "hy_bias": (2, 2, 512), "pool_w": (2, 4, 128, 128), "pool_scale": (2, 512),
    "w_br_a": (2, 512, 1024), "w_br_b": (2, 512, 1024), "w_br_c": (2, 512, 1024), "w_out": (2, 1024, 1024),
    "final_g": (1024,),
}

_NC_CACHE = {}


def make_in_maps(inputs):
    maps = []
    f = lambda a: np.ascontiguousarray(np.asarray(a, dtype=np.float32))
    xs, xp = f(inputs["x_sample"]), f(inputs["x_prompt"])
    cc, cctx = f(inputs["c"]), f(inputs["c_ctx"])
    sre, sim = f(inputs["state_s5_re"]), f(inputs["state_s5_im"])
    wts = {k: f(inputs[k]) for k in WEIGHT_SHAPES}
    for i in range(NCORES):
        m = dict(wts)
        m["xin"] = np.ascontiguousarray(np.concatenate([xs[i], xp[2 * i], xp[2 * i + 1]], axis=0))
        m["cond"] = np.ascontiguousarray(np.stack([cc[i], cctx], axis=0))
        m["st_re"] = sre[i]
        m["st_im"] = sim[i]
        maps.append(m)
    return maps


def run(inputs, dbg=None, parts="tpfba"):
    key = (tuple(sorted((dbg or {}).items())), parts)
    if key not in _NC_CACHE:
        _NC_CACHE[key] = Builder(dbg, parts).build()
    nc = _NC_CACHE[key]
    res = run_bass_kernel_spmd(nc, make_in_maps(inputs), core_ids=list(range(NCORES)))
    return res.results


def kernel(_parts="tpfba", **inputs):
    r = run(inputs, parts=_parts)
    y_prompt = np.empty((16, LP, D), np.float32)
    y_sample = np.empty((8, LS, D), np.float32)
    n_re = np.empty((16, DEPTH, 2, S5_G, S5_P), np.float32)
    n_im = np.empty((16, DEPTH, 2, S5_G, S5_P), np.float32)
    for i in range(NCORES):
        y = r[i]["yout"]
        y_sample[i] = y[0:LS]
        y_prompt[2 * i] = y[LS:LS + LP]
        y_prompt[2 * i + 1] = y[LS + LP:T]
        n_re[2 * i:2 * i + 2] = r[i]["ns_re"]
        n_im[2 * i:2 * i + 2] = r[i]["ns_im"]
    return (y_prompt, y_sample, n_re, n_im)
```

```python
import math
from contextlib import ExitStack

import numpy as np
import concourse.bass as bass
import concourse.mybir as mybir
from concourse.bass_utils import run_bass_kernel_spmd

F32 = mybir.dt.float32
BF16 = mybir.dt.bfloat16
I32 = mybir.dt.int32
AF = mybir.ActivationFunctionType
ALU = mybir.AluOpType

NCORES = 8
D = 1024
KT = D // 128
DEPTH = 2
LS = 2048
LP = 256
T = LS + 2 * LP
NTB = T // 512
IN_W = 7168
EPS = 1e-6
S5_G = 32
S5_P = 64
S5_H = 16
R1N = 32256
MAGIC = 12582912.0
TWO_PI_S = 2 * math.pi * (1 - 1e-6)
HY_SHIFT = 0.05

C_UA, C_GA, C_UB, C_GB, C_UC, C_GC, C_M = 0, 512, 1024, 2560, 3072, 3584, 4096


class StopBuild(Exception):
    pass


class Sched:
    def __init__(self, nc, es, n_dma_sems=24):
        self.nc = nc
        self.eng = {"pe": nc.tensor, "dve": nc.vector, "act": nc.scalar, "pool": nc.gpsimd, "sp": nc.sync}
        self.sem = {k: es.enter_context(nc.semaphore("s_" + k)) for k in self.eng}
        self.cnt = {k: 0 for k in self.eng}
        self.dsem = [es.enter_context(nc.semaphore("d%d" % i)) for i in range(n_dma_sems)]
        self.dcnt = [0] * n_dma_sems
        self.dnext = 0
        self.semobj = {}
        for k in self.eng:
            self.semobj[("e", k)] = self.sem[k]
        for i, s in enumerate(self.dsem):
            self.semobj[("d", i)] = s
        self.waited = {k: {} for k in self.eng}
        self.last_w = {}
        self.readers = {}
        self.nwaits = 0

    def _deps(self, e, r, w):
        need = {}

        def add(ev, self_ok):
            if ev is None:
                return
            sid, val = ev
            if sid == ("e", e) and not self_ok:
                return
            if need.get(sid, 0) < val:
                need[sid] = val

        for k in r:
            add(self.last_w.get(k), e != "pe")
        for k in w:
            add(self.last_w.get(k), False)
            for ev in self.readers.get(k, {}).items():
                add(ev, False)
        wt = self.waited[e]
        for sid, val in need.items():
            if wt.get(sid, 0) >= val:
                continue
            self.eng[e].wait_ge(self.semobj[sid], val)
            wt[sid] = val
            self.nwaits += 1

    def _commit(self, ev, r, w):
        sid, val = ev
        for k in w:
            self.last_w[k] = ev
            self.readers[k] = {}
        for k in r:
            d = self.readers.setdefault(k, {})
            if d.get(sid, 0) < val:
                d[sid] = val

    def op(self, e, fn, r=(), w=()):
        self._deps(e, r, w)
        ins = fn(self.eng[e])
        self.cnt[e] += 1
        ins.then_inc(self.sem[e], 1)
        self._commit((("e", e), self.cnt[e]), r, w)
        return ins

    def dma(self, out, in_, r=(), w=(), q=None, **kw):
        if q is None:
            q = "pool" if type(out.tensor).__name__ == "DRamTensorHandle" else "sp"
        i = self.dnext
        self.dnext = (self.dnext + 1) % len(self.dsem)
        wt = self.waited[q]
        if self.dcnt[i] > 0 and wt.get(("d", i), 0) < self.dcnt[i]:
            self.eng[q].wait_ge(self.dsem[i], self.dcnt[i])
            wt[("d", i)] = self.dcnt[i]
        self._deps(q, r, w)
        ins = self.eng[q].dma_start(out=out, in_=in_, **kw)
        self.dcnt[i] += 16
        ins.then_inc(self.dsem[i], 16)
        self._commit((("d", i), self.dcnt[i]), r, w)
        return ins

    def barrier(self):
        for e in self.eng:
            wt = self.waited[e]
            for k in self.eng:
                if k != e and self.cnt[k] > wt.get(("e", k), 0):
                    self.eng[e].wait_ge(self.sem[k], self.cnt[k])
                    wt[("e", k)] = self.cnt[k]
            for i, s in enumerate(self.dsem):
                if self.dcnt[i] > wt.get(("d", i), 0):
                    self.eng[e].wait_ge(s, self.dcnt[i])
                    wt[("d", i)] = self.dcnt[i]

    def finish(self):
        for i, s in enumerate(self.dsem):
            if self.dcnt[i] > 0:
                self.eng["sp"].wait_ge(s, self.dcnt[i])
        for k in ("pe", "dve", "act", "pool"):
            if self.cnt[k] > 0:
                self.eng["sp"].wait_ge(self.sem[k], self.cnt[k])


class Builder:
    def __init__(self, dbg=None, parts="tpfba"):
        self.dbg = dbg or {}
        self.parts = parts
        self.nc = bass.Bass("TRN2", target_bir_lowering=False)
        self.es = ExitStack()
        self.uid = 0

    def dram_in(self, name, shape, dt=F32):
        return self.nc.dram_tensor(name, list(shape), dt, kind="ExternalInput").ap()

    def dram_out(self, name, shape, dt=F32):
        return self.nc.dram_tensor(name, list(shape), dt, kind="ExternalOutput").ap()

    def dram_tmp(self, name, shape, dt=F32):
        return self.nc.dram_tensor(name, list(shape), dt, kind="Internal").ap()

    def sb(self, name, shape, dt=F32):
        return self.es.enter_context(self.nc.sbuf_tensor(name, list(shape), dt))

    def build(self):
        with self.es:
            self._build()
        return self.nc

    def _build(self):
        nc = self.nc
        S = self.S = Sched(nc, self.es)
        op, dma = S.op, S.dma

        xin = self.dram_in("xin", [T, D])
        cond = self.dram_in("cond", [2, D])
        st_re = self.dram_in("st_re", [DEPTH, 2, S5_G, S5_P])
        st_im = self.dram_in("st_im", [DEPTH, 2, S5_G, S5_P])
        W = {}
        for name, shape in WEIGHT_SHAPES.items():
            W[name] = self.dram_in(name, shape)
        yout = self.dram_out("yout", [T, D])
        ns_re = self.dram_out("ns_re", [2, DEPTH, 2, S5_G, S5_P])
        ns_im = self.dram_out("ns_im", [2, DEPTH, 2, S5_G, S5_P])
        xsp = self.dram_tmp("xsp", [128, KT * T])
        xsp3 = xsp.rearrange("p (k t) -> p k t", k=KT)
        dbg_out = {}
        for name, shape in self.dbg.items():
            dbg_out[name] = self.dram_out("dbg_" + name, shape)

        R1 = self.sb("R1", [128, R1N], F32)
        Hh = self.sb("Hh", [128, KT * T], BF16)
        h3 = Hh[:].rearrange("p (k t) -> p k t", k=KT)
        x3 = R1[:, 0:KT * T].rearrange("p (k t) -> p k t", k=KT)
        R1b = R1[:].bitcast(BF16)
        Yv = [R1b[:, i * 4 * T:(i + 1) * 4 * T].rearrange("p (k t) -> p k t", k=4) for i in range(3)]
        SCR0 = 3 * 4 * T // 2
        stage = [self.sb("stage%d" % i, [128, 2048], F32) for i in range(2)]
        self.stage_i = 0
        wbuf = [self.sb("wbuf%d" % i, [128, 5120], BF16) for i in range(2)]
        self.wbuf_i = 0
        self.wbuf = wbuf
        self.R1 = R1
        self.slots = [wbuf[0][:, 0:4096], wbuf[1][:, 0:4096], stage[0][:].bitcast(BF16), stage[1][:].bitcast(BF16)]
        self.slot_keys = [("wbuf", 0), ("wbuf", 1), ("stage", 0), ("stage", 1)]
        self._slot_i = 0
        ident = self.sb("ident", [128, 128], F32)
        identb = self.sb("identb", [128, 128], BF16)
        onesm = self.sb("onesm", [128, 128], F32)
        iot = self.sb("iot", [128, 128], I32)
        modT = self.sb("modT", [128, 24, 2], F32)
        gmul = self.sb("gmul", [128, KT, 2], F32)
        bmod = self.sb("bmod", [128, 24], F32)
        normg = self.sb("normg", [128, KT], F32)
        condT = self.sb("condT", [128, KT, 2], F32)
        rstd = R1[:, 22528:22528 + T]
        smallv = self.sb("smallv", [128, 64], F32)
        xtm = [R1[:, 20480 + i * 1024:20480 + (i + 1) * 1024] for i in range(2)]
        self._sq = [R1[:, 25088 + i * 512:25088 + i * 512 + 256].bitcast(BF16) for i in range(4)]
        PS = [self.es.enter_context(nc.psum_tensor("ps%d" % i, [128, 512], F32)) for i in range(8)]
        self.ps_i = 0

        def psk(i):
            return ("ps", i)

        MODBANK = 7

        def next_ps():
            i = self.ps_i
            self.ps_i = (i + 1) % 7
            return i

        op("pool", lambda e: e.iota(iot[:], pattern=[[1, 128]], base=0, channel_multiplier=-1), w=["iot"])
        op("dve", lambda e: e.tensor_scalar(out=ident[:], in0=iot[:], scalar1=0, scalar2=None, op0=ALU.is_equal),
           r=["iot"], w=["ident"])
        op("dve", lambda e: e.tensor_copy(out=identb[:], in_=ident[:]), r=["ident"], w=["identb"])
        op("pool", lambda e: e.memset(onesm[:], 1.0 / D), w=["onesm"])
        self._onesb = self.sb("onesb", [128, 128], BF16)
        op("dve", lambda e: e.tensor_copy(out=self._onesb[:], in_=onesm[:]), r=["onesm"], w=["onesm"])

        def wload(dst, src, key, eng="act"):
            i = self.stage_i
            self.stage_i = (i + 1) % len(stage)
            n = 1
            for s in dst.shape[1:]:
                n *= s
            assert n <= 2048
            st = stage[i][:, 0:n]
            if len(dst.shape) == 3:
                st = st.rearrange("p (a b) -> p a b", a=dst.shape[1])
            dma(st, src, w=[("stage", i)])
            wk_ = list(key) if isinstance(key, list) else [key]
            if eng == "act":
                op("act", lambda e: e.activation(out=dst, in_=st, func=AF.Copy), r=[("stage", i)], w=wk_)
            else:
                op(eng, lambda e: e.tensor_copy(out=dst, in_=st), r=[("stage", i)], w=wk_)

        def wdirect(dst, src, key):
            dma(dst, src, w=[key])

        with nc.allow_non_contiguous_dma(reason="small transposed vector loads"):
            for j in range(2):
                dma(condT[:, :, j], cond[j].rearrange("(k p) -> p k", p=128), w=["condT"])
        op("act", lambda e: e.activation(out=condT[:], in_=condT[:], func=AF.Silu), r=["condT"], w=["condT"])

        def mod_start(l):
            for mc in range(12):
                i = self.stage_i
                self.stage_i = (i + 1) % len(stage)
                st = stage[i][:].rearrange("p (k c) -> p k c", k=KT)
                dma(st, W["w_mod"][l][:, mc * 256:(mc + 1) * 256].rearrange("(k p) c -> p k c", p=128),
                    w=[("stage", i)])
                for mm in range(2):
                    m = mc * 2 + mm
                    for kt in range(KT):
                        op("pe", lambda e, m=m, mm=mm, kt=kt, st=st: e.matmul(
                            PS[MODBANK][:, m * 2:m * 2 + 2], st[:, kt, mm * 128:(mm + 1) * 128], condT[:, kt, :],
                            start=(kt == 0), stop=(kt == KT - 1)),
                           r=[("stage", i), "condT"], w=[psk(MODBANK)])
        self.mod_start = mod_start
        mod_start(0)

        if "t" in self.parts:
            self._setup_tables()
        S.barrier()
        for tt in range(T // 128):
            b = tt % 2
            dma(xtm[b][:], xin[tt * 128:(tt + 1) * 128, :], w=[("xtm", b)])
            for half in range(2):
                pi = next_ps()
                for j in range(4):
                    kt = half * 4 + j
                    op("pe", lambda e, kt=kt, j=j, pi=pi: e.transpose(PS[pi][:, j * 128:(j + 1) * 128],
                                                                     xtm[b][:, kt * 128:(kt + 1) * 128], ident[:]),
                       r=[("xtm", b), "ident"], w=[psk(pi)])
                eng = "act" if half == 0 else "dve"
                src = PS[pi][:].rearrange("p (j t) -> p j t", j=4)
                dst = x3[:, half * 4:half * 4 + 4, tt * 128:(tt + 1) * 128]
                if eng == "act":
                    op("act", lambda e, dst=dst, src=src: e.activation(out=dst, in_=src, func=AF.Copy),
                       r=[psk(pi)], w=[("x", tt // 4)])
                else:
                    op("dve", lambda e, dst=dst, src=src: e.tensor_copy(out=dst, in_=src),
                       r=[psk(pi)], w=[("x", tt // 4)])

        if "p" in self.parts:
            self._pos_embed(x3)

        for tb in range(NTB):
            dma(xsp3[:, :, tb * 512:(tb + 1) * 512], x3[:, :, tb * 512:(tb + 1) * 512],
                r=[("x", tb)], w=[("xsp", f, tb) for f in range(KT)])


        for l in range(DEPTH):
            with nc.allow_non_contiguous_dma(reason="small transposed vector loads"):
                dma(bmod[:], W["b_mod"][l].rearrange("(m p) -> p m", p=128), w=["bmod"])
                dma(normg[:], W["norm_g"][l].rearrange("(k p) -> p k", p=128), w=["normg"])
            op("dve", lambda e: e.tensor_tensor(out=modT[:], in0=PS[MODBANK][:, 0:48].rearrange("p (m j) -> p m j", j=2),
                                                in1=bmod[:].unsqueeze(2).broadcast_to([128, 24, 2]), op=ALU.add),
               r=[psk(MODBANK), "bmod"], w=["modT"])
            op("dve", lambda e: e.tensor_scalar(out=gmul[:], in0=modT[:, 8:16, :], scalar1=1.0, scalar2=None,
                                                op0=ALU.add), r=["modT"], w=["gmul"])
            op("dve", lambda e: e.tensor_tensor(out=gmul[:], in0=gmul[:],
                                                in1=normg[:].unsqueeze(2).broadcast_to([128, KT, 2]), op=ALU.mult),
               r=["gmul", "normg"], w=["gmul"])

            if l > 0:
                for tb in range(NTB):
                    dma(x3[:, :, tb * 512:(tb + 1) * 512], xsp3[:, :, tb * 512:(tb + 1) * 512],
                        r=[("xsp", f, tb) for f in range(KT)], w=[("x", tb)])
            self._norm_to(x3, rstd, onesm, PS, next_ps, psk)
            for kt in range(KT):
                for (t0, t1, j) in ((0, LS, 0), (LS, T, 1)):
                    keys = [("x", tb) for tb in range(t0 // 512, (t1 + 511) // 512)]
                    hkeys = [("h", kt, tb) for tb in range(t0 // 512, (t1 + 511) // 512)]
                    op("dve", lambda e, kt=kt, t0=t0, t1=t1, j=j: e.scalar_tensor_tensor(
                        out=x3[:, kt, t0:t1], in0=x3[:, kt, t0:t1], scalar=gmul[:, kt, j:j + 1], op0=ALU.mult,
                        in1=rstd[:, t0:t1], op1=ALU.mult), r=keys + ["gmul", "rstd"], w=keys)
                    op("act", lambda e, kt=kt, t0=t0, t1=t1, j=j: e.activation(
                        out=h3[:, kt, t0:t1], in_=x3[:, kt, t0:t1], func=AF.Identity,
                        bias=modT[:, kt, j:j + 1], scale=1.0), r=keys + ["modT"], w=hkeys)
            if "h%d" % l in dbg_out:
                hf = self.sb("dbg_hf%d" % l, [128, T], F32)
                for kt in range(KT):
                    op("dve", lambda e, kt=kt: e.tensor_copy(out=hf[:], in_=h3[:, kt, :]),
                       r=[("h", kt, tb) for tb in range(NTB)], w=["dbg_hf"])
                    dma(dbg_out["h%d" % l][kt * 128:(kt + 1) * 128, :], hf[:], r=["dbg_hf"])

            def proj(col0, ntiles, evac):
                for m0 in range(0, ntiles, 2):
                    nm = min(2, ntiles - m0)
                    bi = self.wbuf_i
                    self.wbuf_i = (bi + 1) % 2
                    wv = wbuf[bi][:, 0:KT * nm * 128].rearrange("p (k c) -> p k c", k=KT)
                    c0 = col0 + m0 * 128
                    wload(wv, W["w_in"][l][:, c0:c0 + nm * 128].rearrange("(k p) c -> p k c", p=128), ("wbuf", bi))
                    for mm in range(nm):
                        for tb in range(NTB):
                            pi = next_ps()
                            for kt in range(KT):
                                op("pe", lambda e, pi=pi, kt=kt, mm=mm, tb=tb, wv=wv: e.matmul(
                                    PS[pi][:], wv[:, kt, mm * 128:(mm + 1) * 128], h3[:, kt, tb * 512:(tb + 1) * 512],
                                    start=(kt == 0), stop=(kt == KT - 1)),
                                   r=[("wbuf", bi), ("h", kt, tb)], w=[psk(pi)])
                            evac(m0 + mm, tb, pi)

            self.ctx = dict(l=l, W=W, op=op, dma=dma, PS=PS, next_ps=next_ps, psk=psk, proj=proj, wload=wload,
                            Yv=Yv, R1=R1, R1b=R1b, SCR0=SCR0, h3=h3, wbuf=wbuf, stage=stage, smallv=smallv,
                            ident=ident, identb=identb, dbg_out=dbg_out, modT=modT, x3=x3, xsp3=xsp3,
                            st_re=st_re, st_im=st_im, ns_re=ns_re, ns_im=ns_im)
            S.barrier()
            self._branch_c()
            S.barrier()
            self._branch_b()
            S.barrier()
            self._branch_a()
            S.barrier()
            self._phase2()
            S.barrier()

        S.barrier()
        self._final(x3, xsp3, rstd, onesm, PS, next_ps, psk, W, yout, xtm, ident)
        S.finish()

    def _norm_to(self, x3, rstd, onesm, PS, next_ps, psk):
        op = self.S.op
        sq = self._sq
        if not hasattr(self, "_epsb"):
            self._epsb = self.sb("epsb", [128, 1], F32)
            op("pool", lambda e: e.memset(self._epsb[:], EPS), w=["epsb"])
        si = 0
        for tb in range(NTB):
            pi = next_ps()
            for kt in range(KT):
                b = si % 4
                si += 1
                op("act", lambda e, b=b, kt=kt, tb=tb: e.activation(out=sq[b][:], in_=x3[:, kt, tb * 512:(tb + 1) * 512],
                                                                    func=AF.Square), r=[("x", tb)], w=[("sq", b)])
                op("pe", lambda e, b=b, kt=kt, pi=pi: e.matmul(PS[pi][:], self._onesb[:], sq[b][:], start=(kt == 0),
                                                               stop=(kt == KT - 1)),
                   r=[("sq", b), "onesm"], w=[psk(pi)])
            op("act", lambda e, pi=pi, tb=tb: e.activation(out=rstd[:, tb * 512:(tb + 1) * 512], in_=PS[pi][:],
                                                           func=AF.Sqrt, bias=self._epsb[:], scale=1.0),
               r=[psk(pi), "epsb"], w=["rstd"])
        op("dve", lambda e: e.reciprocal(out=rstd[:], in_=rstd[:]), r=["rstd"], w=["rstd"])

    def _branch_c(self):
        c = self.ctx
        op, dma, PS, next_ps, psk, W, l = c["op"], c["dma"], c["PS"], c["next_ps"], c["psk"], c["W"], c["l"]
        R1, SCR0, Yv = c["R1"], c["SCR0"], c["Yv"]
        nc = self.nc
        PAD = 8
        TP = T + 6 * PAD
        seqs = [(0, LS), (LS, LP), (LS + LP, LP)]
        offs = []
        o = 0
        for (t0, ln) in seqs:
            offs.append(o + PAD)
            o += ln + 2 * PAD
        ucp = R1[:, SCR0:SCR0 + 4 * TP].rearrange("p (k t) -> p k t", k=4)
        o2 = SCR0 + 4 * TP
        tmpw = R1[:, o2:o2 + TP]
        o2 += TP
        dmean = R1[:, o2:o2 + T // 2].bitcast(BF16)
        o2 += T // 2
        sg = Yv[2]
        assert o2 <= R1N
        if not hasattr(self, "_rcb"):
            self._rcb = self.sb("rcb", [128, 4, 2, 8], F32)
            i8 = self.sb("i8", [128, 8], F32)
            op("pool", lambda e: e.iota(i8[:], pattern=[[1, 8]], base=0, channel_multiplier=0,
                                        allow_small_or_imprecise_dtypes=True), w=["i8"])
            for k in range(4):
                win = 2 << k
                op("dve", lambda e, k=k, win=win: e.tensor_scalar(out=self._rcb[:, k, 0, :], in0=i8[:], scalar1=float(win // 2),
                                                                scalar2=float(win), op0=ALU.add, op1=ALU.min),
                   r=["i8"], w=["rcb"])
                op("dve", lambda e, k=k, win=win: e.tensor_scalar(out=self._rcb[:, k, 1, :], in0=i8[:], scalar1=-1.0,
                                                                scalar2=float(8 + win // 2), op0=ALU.mult, op1=ALU.add),
                   r=["i8"], w=["rcb"])
                op("dve", lambda e, k=k, win=win: e.tensor_scalar(out=self._rcb[:, k, 1, :], in0=self._rcb[:, k, 1, :],
                                                                scalar1=float(win), scalar2=None, op0=ALU.min),
                   r=["rcb"], w=["rcb"])
            op("dve", lambda e: e.reciprocal(out=self._rcb[:], in_=self._rcb[:]), r=["rcb"], w=["rcb"])
        rcb = self._rcb
        smallv = c["smallv"]
        with nc.allow_non_contiguous_dma(reason="small transposed vector loads"):
            dma(smallv[:, 0:4], W["pool_scale"][l].rearrange("(k p) -> p k", p=128), w=["pool_scale"])
        op("dve", lambda e: e.memset(ucp[:], 0.0), w=["ucp"])

        def evac_u(mi, tb, pi):
            if tb < 4:
                dst = ucp[:, mi, offs[0] + tb * 512: offs[0] + (tb + 1) * 512]
                op("act", lambda e: e.activation(out=dst, in_=PS[pi][:], func=AF.Copy), r=[psk(pi)], w=["ucp"])
            else:
                for s in range(2):
                    dst = ucp[:, mi, offs[1 + s]: offs[1 + s] + LP]
                    op("act", lambda e, s=s, dst=dst: e.activation(out=dst, in_=PS[pi][:, s * LP:(s + 1) * LP], func=AF.Copy),
                       r=[psk(pi)], w=["ucp"])

        def evac_g(mi, tb, pi):
            op("act", lambda e: e.activation(out=sg[:, mi, tb * 512:(tb + 1) * 512], in_=PS[pi][:], func=AF.Silu),
               r=[psk(pi)], w=[("y", 2, mi, tb)])

        c["proj"](C_UC, 4, evac_u)
        c["proj"](C_GC, 4, evac_g)
        pw = c["wbuf"][self.wbuf_i]
        bi = self.wbuf_i
        self.wbuf_i = (bi + 1) % 2
        pwv = pw[:, 0:512].rearrange("p (g c) -> p g c", g=4)
        c["wload"](pwv, W["pool_w"][l].rearrange("g p c -> p g c"), ("wbuf", bi))
        for gi in range(4):
            win = 2 << gi
            u = ucp[:, gi, :]
            cur = u
            n = TP
            sh = 1
            for lev in range(gi + 1):
                n2 = n - sh
                eng = "dve"
                src = cur
                op(eng, lambda e, src=src, sh=sh, n2=n2: e.tensor_tensor(out=tmpw[:, 0:n2], in0=src[:, 0:n2],
                                                                         in1=src[:, sh:sh + n2], op=ALU.add),
                   r=["ucp", "tmpw"], w=["tmpw"])
                cur = tmpw
                n = n2
                sh *= 2
            for si, (t0, ln) in enumerate(seqs):
                a = offs[si] - win // 2
                op("dve", lambda e, a=a, t0=t0, ln=ln, si=si: e.scalar_tensor_tensor(
                    out=dmean[:, t0:t0 + ln], in0=tmpw[:, a:a + ln], scalar=1.0 / win, op0=ALU.mult,
                    in1=u[:, offs[si]:offs[si] + ln], op1=ALU.subtract), r=["tmpw", "ucp"], w=["dmean"])
                for side, p0 in ((0, 0), (1, ln - 8)):
                    tt = self._tmp8 if hasattr(self, "_tmp8") else None
                    if tt is None:
                        tt = self._tmp8 = self.sb("tmp8", [128, 8], F32)
                    op("dve", lambda e, a=a, p0=p0, side=side: e.tensor_tensor(
                        out=tt[:], in0=tmpw[:, a + p0:a + p0 + 8], in1=rcb[:, gi, side, :], op=ALU.mult),
                       r=["tmpw", "rcb", "tmp8"], w=["tmp8"])
                    op("dve", lambda e, p0=p0, t0=t0, si=si: e.tensor_tensor(
                        out=dmean[:, t0 + p0:t0 + p0 + 8], in0=tt[:], in1=u[:, offs[si] + p0:offs[si] + p0 + 8],
                        op=ALU.subtract), r=["tmp8", "ucp", "dmean"], w=["dmean"])
            for tb in range(NTB):
                pi = next_ps()
                op("pe", lambda e, pi=pi, tb=tb: e.matmul(PS[pi][:], pwv[:, gi, :], dmean[:, tb * 512:(tb + 1) * 512],
                                                          start=True, stop=True),
                   r=[("wbuf", bi), "dmean"], w=[psk(pi)])
                op("dve", lambda e, pi=pi, tb=tb: e.scalar_tensor_tensor(
                    out=Yv[2][:, gi, tb * 512:(tb + 1) * 512], in0=PS[pi][:], scalar=smallv[:, gi:gi + 1], op0=ALU.mult,
                    in1=sg[:, gi, tb * 512:(tb + 1) * 512], op1=ALU.mult),
                   r=[psk(pi), "pool_scale", ("y", 2, gi, tb)], w=[("y", 2, gi, tb)])
        if "yc%d" % l in c["dbg_out"]:
            self._dump_bf16(Yv[2], c["dbg_out"]["yc%d" % l], [("y", 2, gi, tb) for gi in range(4) for tb in range(NTB)])

    def _dump_bf16(self, src3, dst, keys):
        op, dma = self.S.op, self.S.dma
        if not hasattr(self, "_dbgf"):
            self._dbgf = self.sb("dbgf", [128, T], F32)
        for k in range(src3.shape[1]):
            op("dve", lambda e, k=k: e.tensor_copy(out=self._dbgf[:], in_=src3[:, k, :]), r=keys + ["dbgf"], w=["dbgf"])
            dma(dst[k * 128:(k + 1) * 128, :], self._dbgf[:], r=["dbgf"], w=["dbgo"])

    def _zero_y(self, i):
        op = self.S.op
        Yv = self.ctx["Yv"]
        op("pool", lambda e: e.memset(Yv[i][:], 0.0), w=[("y", i, gi, tb) for gi in range(4) for tb in range(NTB)])

    def _branch_b(self):
        self._zero_y(1)

    def _branch_a(self):
        try:
            self._branch_a_impl()
        except StopBuild:
            self.S.barrier()
            self._zero_y(0)

    def _ka(self, n):
        import os
        if int(os.environ.get("KA_STOP", "99")) == n:
            raise StopBuild()

    def _branch_a_impl(self):
        c = self.ctx
        if "a" not in self.parts:
            self._zero_y(0)
            return
        op, dma, PS, next_ps, psk, W, l = c["op"], c["dma"], c["PS"], c["next_ps"], c["psk"], c["W"], c["l"]
        R1, SCR0, Yv, ident, identb = c["R1"], c["SCR0"], c["Yv"], c["ident"], c["identb"]
        nc, S = self.nc, self.S
        NCH = T // 8
        CP = NCH
        SEQC = [(1, 256, 0), (259, 32, 256), (293, 32, 288)]
        if not hasattr(self, "_uaD"):
            self._uaD = self.dram_tmp("uaD", [4, 128, 8 * NCH], BF16)
            self._y2D = self.dram_tmp("y2D", [2, 128, 16 * CP], BF16)
            self._s5m = self.sb("s5m", [128, 256], BF16)
            self._s5n = self.sb("s5n", [128, 16], F32)
            msk = self._s5m
            ii = R1[:, SCR0:SCR0 + 130].bitcast(I32)
            thr = R1[:, SCR0 + 256:SCR0 + 258]
            colf = R1[:, SCR0 + 512:SCR0 + 640]
            op("pool", lambda e: e.iota(ii[:, 0:1], pattern=[[0, 1]], base=0, channel_multiplier=1), w=["s5i"])
            op("dve", lambda e: e.tensor_scalar(out=ii[:, 0:1], in0=ii[:, 0:1], scalar1=4, scalar2=None, op0=ALU.arith_shift_right),
               r=["s5i"], w=["s5i"])
            op("dve", lambda e: e.tensor_scalar(out=ii[:, 0:1], in0=ii[:, 0:1], scalar1=4, scalar2=None, op0=ALU.logical_shift_left),
               r=["s5i"], w=["s5i"])
            op("pool", lambda e: e.iota(ii[:, 2:130], pattern=[[1, 128]], base=0, channel_multiplier=0), r=["s5i"], w=["s5i"])
            op("dve", lambda e: e.tensor_copy(out=thr[:, 0:1], in_=ii[:, 0:1]), r=["s5i"], w=["s5thr"])
            op("dve", lambda e: e.tensor_scalar(out=thr[:, 1:2], in0=thr[:, 0:1], scalar1=15.0, scalar2=None, op0=ALU.add),
               r=["s5thr"], w=["s5thr"])
            op("dve", lambda e: e.tensor_copy(out=colf, in_=ii[:, 2:130]), r=["s5i"], w=["s5col"])
            op("dve", lambda e: e.tensor_scalar(out=msk[:, 0:128], in0=colf, scalar1=thr[:, 0:1], scalar2=None, op0=ALU.is_ge),
               r=["s5col", "s5thr"], w=["s5mask"])
            op("dve", lambda e: e.tensor_scalar(out=msk[:, 128:256], in0=colf, scalar1=thr[:, 1:2], scalar2=None, op0=ALU.is_le),
               r=["s5col", "s5thr"], w=["s5mask"])
            op("pool", lambda e: e.iota(self._s5n[:], pattern=[[1, 16]], base=-7, channel_multiplier=0,
                                        allow_small_or_imprecise_dtypes=True), w=["s5nvec"])
            S.barrier()
        maskf, maskb, nvec = self._s5m[:, 0:128], self._s5m[:, 128:256], self._s5n[:]
        uaD, y2D = self._uaD, self._y2D

        o = 0
        uaj = [R1[:, o + i * 1280:o + (i + 1) * 1280].bitcast(BF16).rearrange("p (s c) -> p s c", s=8) for i in range(2)]
        o += 2560

        def evac_u(mi, tb, pi):
            dst = uaj[mi % 2][:, :, tb * 64:(tb + 1) * 64]
            src = PS[pi][:].rearrange("p (c s) -> p s c", s=8)
            if tb % 2:
                op("act", lambda e: e.activation(out=dst, in_=src, func=AF.Copy), r=[psk(pi)], w=[("uaj", mi % 2)])
            else:
                op("dve", lambda e: e.tensor_copy(out=dst, in_=src), r=[psk(pi)], w=[("uaj", mi % 2)])
            if tb == NTB - 1:
                dma(uaD[mi].rearrange("p (s c) -> p s c", s=8), uaj[mi % 2], r=[("uaj", mi % 2)], w=[("uaD", mi)])
        c["proj"](C_UA, 4, evac_u)

        self._ka(1)
        o = 2560
        Ap = R1[:, o:o + 512].rearrange("p (r x n) -> p r x n", r=2, x=16); o += 512
        BB = R1[:, o:o + 512].rearrange("p (r x h) -> p r x h", r=2, x=16); o += 512
        CT = R1[:, o:o + 512].rearrange("p (r x h) -> p r x h", r=2, x=16); o += 512
        sm = R1[:, o:o + 512]; o += 512
        Braw = R1[:, o:o + 512].rearrange("p (r x h) -> p r x h", r=2, x=16); o += 512
        assert o <= 5120
        o = 0
        t1 = R1[:, o:o + 1024]; o += 1024
        t2 = R1[:, o:o + 1024]; o += 1024
        CN = R1[:, o:o + 128]; o += 128
        cur = R1[:, o:o + 32].rearrange("p (r d g) -> p r d g", r=2, d=2); o += 32
        curP = R1[:, o:o + 32].rearrange("p (r d g) -> p r d g", r=2, d=2); o += 32
        T1 = R1[:, o:o + 32].rearrange("p (r d g) -> p r d g", r=2, d=2); o += 32
        T2 = R1[:, o:o + 32].rearrange("p (r d g) -> p r d g", r=2, d=2); o += 32
        ARt = R1[:, o:o + 32].rearrange("p (r d g) -> p r d g", r=2, d=2); o += 32
        AIt = R1[:, o:o + 32].rearrange("p (r d g) -> p r d g", r=2, d=2); o += 32
        h0 = R1[:, o:o + 32].rearrange("p (r d g) -> p r d g", r=2, d=2); o += 32
        assert o <= 2560
        o = SCR0
        U2 = R1[:, o:o + 8 * CP].bitcast(BF16).rearrange("p (g c) -> p g c", g=16); o += 8 * CP
        Sx = R1[:, o:o + 16 * CP].bitcast(BF16).rearrange("p (r d g c) -> p r d g c", r=2, d=2, g=8); o += 16 * CP
        BS = R1[:, o:o + 2048].bitcast(BF16).rearrange("p (r x k) -> p r x k", r=2, x=16); o += 2048
        MI = R1[:, o:o + 1024].bitcast(BF16).rearrange("p (g k) -> p g k", g=16); o += 1024
        oXS = o
        xs_ = [R1[:, o + i * 320:o + (i + 1) * 320] for i in range(2)]; o += 640
        oTG = o
        tg_ = [R1[:, o + i * 320:o + (i + 1) * 320] for i in range(2)]; o += 640
        oRB = o
        RR = R1[:, o:o + 2048].bitcast(BF16).rearrange("p (r x k) -> p r x k", r=2, x=16); o += 2048
        BST = R1[:, o:o + 2048].bitcast(BF16).rearrange("p (r x q) -> p r x q", r=2, x=32); o += 2048
        assert o <= R1N, o
        Y2 = R1[:, oRB:oRB + 8 * CP].bitcast(BF16).rearrange("p (g c) -> p g c", g=16)
        yj = Yv[0].rearrange("p k (t c) -> p k t c", t=8)
        smv = lambda i: sm[:, i * 16:(i + 1) * 16]

        def swapped(ap4):
            a = ap4.ap
            return bass.AP(ap4.tensor, ap4.offset + a[1][0], [list(a[0]), [-a[1][0], 2]] + [list(x) for x in a[2:]])

        def cmul(out_re, out_im, a_re, a_im, b_re, b_im, shape, rk, wk, neg_im=False):
            n = 1
            for v in shape[1:]:
                n *= v
            names = "abcd"[:len(shape) - 1]
            pat = "p (%s) -> p %s" % (" ".join(names), " ".join(names))
            kw = {names[i]: shape[i + 1] for i in range(len(names) - 1)}
            v1 = t1[:, 0:n].rearrange(pat, **kw) if len(shape) > 2 else t1[:, 0:n]
            v2 = t2[:, 0:n].rearrange(pat, **kw) if len(shape) > 2 else t2[:, 0:n]
            op("dve", lambda e: e.tensor_tensor(out=v1, in0=a_re, in1=b_re, op=ALU.mult), r=rk + ["t1"], w=["t1"])
            op("dve", lambda e: e.tensor_tensor(out=v2, in0=a_im, in1=b_im, op=ALU.mult), r=rk + ["t2"], w=["t2"])
            op("dve", lambda e: e.tensor_tensor(out=out_re, in0=v1, in1=v2, op=ALU.subtract), r=["t1", "t2"], w=wk)
            op("dve", lambda e: e.tensor_tensor(out=v1, in0=a_re, in1=b_im, op=ALU.mult), r=rk + ["t1"], w=["t1"])
            op("dve", lambda e: e.tensor_tensor(out=v2, in0=a_im, in1=b_re, op=ALU.mult), r=rk + ["t2"], w=["t2"])
            if neg_im:
                op("dve", lambda e: e.scalar_tensor_tensor(out=out_im, in0=v1, scalar=-1.0, op0=ALU.mult, in1=v2, op1=ALU.subtract),
                   r=["t1", "t2"], w=wk)
            else:
                op("dve", lambda e: e.tensor_tensor(out=out_im, in0=v1, in1=v2, op=ALU.add), r=["t1", "t2"], w=wk)

        with nc.allow_non_contiguous_dma(reason="small transposed vector loads"):
            dma(c["smallv"][:, 4:8], W["s5_d"][l].rearrange("(k p) -> p k", p=128), w=["s5dv"])
            dma(c["smallv"][:, 60:64], W["s5_b_glu"][l].rearrange("(k p) -> p k", p=128), w=["s5bg"])
        Dcol = self._dcol if hasattr(self, "_dcol") else None
        if Dcol is None:
            Dcol = self._dcol = self.sb("dcol", [128, 32], F32)
        with nc.allow_non_contiguous_dma(reason="small transposed vector loads"):
            for s_ in range(8):
                dma(Dcol[s_ * 16:(s_ + 1) * 16, :], W["s5_d"][l].rearrange("(g h) -> h g", h=16), w=["dcol"])

        for hf in range(2):
            G0 = 16 * hf
            if hf == 0:
                S.barrier()
            with nc.allow_non_contiguous_dma(reason="small transposed parameter loads"):
                for d in range(2):
                    xs = slice(d * 8, (d + 1) * 8)
                    for gp in range(2):
                        pq = slice(gp * 64, (gp + 1) * 64)
                        dma(smv(0)[pq, xs], W["s5_lam_re"][l][d, G0:G0 + 16, :].rearrange("(g8 gp) p -> gp p g8", gp=2)[gp], w=["s5p"])
                        dma(smv(1)[pq, xs], W["s5_lam_im"][l][d, G0:G0 + 16, :].rearrange("(g8 gp) p -> gp p g8", gp=2)[gp], w=["s5p"])
                        dma(smv(2)[pq, xs], W["s5_log_dt"][l][d, G0:G0 + 16].rearrange("(g8 gp) -> gp g8", gp=2)[gp].partition_broadcast(64),
                            w=["s5p"])
                        for ri, nm in enumerate(("s5_b_re", "s5_b_im")):
                            dma(Braw[pq, ri, xs, :], W[nm][l][d, G0:G0 + 16].rearrange("(g8 gp) p h -> gp p g8 h", gp=2)[gp], w=["s5B"])
                        for ri, st in enumerate((c["st_re"], c["st_im"])):
                            dma(h0[pq, ri, d, :], st[l, d, G0:G0 + 16, :].rearrange("(g8 gp) p -> gp p g8", gp=2)[gp], w=["s5h0"])
            for d in range(2):
                for ri, nm in enumerate(("s5_c_re", "s5_c_im")):
                    for k2 in range(2):
                        srcC = W[nm][l][d, G0 + 8 * k2:G0 + 8 * k2 + 8].rearrange("g h p -> (g h) p")
                        dma(CN[:, 0:64], srcC, w=["CN"])
                        dma(CN[:, 64:128], srcC, w=["CN"])
                        pi = next_ps()
                        op("pe", lambda e: e.transpose(PS[pi][:, 0:128], CN, ident[:]), r=["CN", "ident"], w=[psk(pi)])
                        pv = PS[pi][:, 0:128].rearrange("p (j gp h) -> p j gp h", gp=2, h=16)
                        for gp in range(2):
                            op("dve", lambda e: e.tensor_copy(out=CT[gp * 64:(gp + 1) * 64, ri, d * 8 + 4 * k2:d * 8 + 4 * k2 + 4, :],
                                                              in_=pv[gp * 64:(gp + 1) * 64, :, gp, :]), r=[psk(pi)], w=["CT"])
            self._ka(2)
            op("act", lambda e: e.activation(out=smv(2), in_=smv(2), func=AF.Exp), r=["s5p"], w=["s5dt"])
            op("dve", lambda e: e.tensor_tensor(out=smv(3), in0=smv(0), in1=smv(2), op=ALU.mult), r=["s5p", "s5dt"], w=["s5lrd"])
            op("dve", lambda e: e.scalar_tensor_tensor(out=smv(4), in0=smv(1), scalar=1.0 / (2 * math.pi), op0=ALU.mult,
                                                       in1=smv(2), op1=ALU.mult), r=["s5p", "s5dt"], w=["s5th"])
            EA = t1[:, 0:256].rearrange("p (x n) -> p x n", x=16)
            UA = t2[:, 0:256].rearrange("p (x n) -> p x n", x=16)
            UC = t1[:, 256:512].rearrange("p (x n) -> p x n", x=16)
            TT = t2[:, 256:512].rearrange("p (x n) -> p x n", x=16)
            nb_ = nvec.unsqueeze(1).broadcast_to([128, 16, 16])
            op("dve", lambda e: e.tensor_tensor(out=EA, in0=smv(3).unsqueeze(2).broadcast_to([128, 16, 16]), in1=nb_, op=ALU.mult),
               r=["s5lrd", "s5nvec", "t1"], w=["t1"])
            op("dve", lambda e: e.tensor_tensor(out=UA, in0=smv(4).unsqueeze(2).broadcast_to([128, 16, 16]), in1=nb_, op=ALU.mult),
               r=["s5th", "s5nvec", "t2"], w=["t2"])
            op("act", lambda e: e.activation(out=EA, in_=EA, func=AF.Exp), r=["t1"], w=["t1"])
            op("dve", lambda e: e.tensor_scalar(out=UC, in0=UA, scalar1=0.25, scalar2=None, op0=ALU.add), r=["t2", "t1"], w=["t1b"])
            self.sin_turns(Ap[:, 1], UA, TT, ["t2"], ["ApS"], "t2b")
            self.sin_turns(Ap[:, 0], UC, TT, ["t1b"], ["ApC"], "t2b")
            op("dve", lambda e: e.tensor_tensor(out=Ap[:, 0], in0=Ap[:, 0], in1=EA, op=ALU.mult), r=["ApC", "t1"], w=["Ap"])
            op("dve", lambda e: e.tensor_tensor(out=Ap[:, 1], in0=Ap[:, 1], in1=EA, op=ALU.mult), r=["ApS", "t1", "Ap"], w=["Ap"])
            self._ka(3)
            a_re, a_im = Ap[:, 0, :, 8], Ap[:, 1, :, 8]
            op("dve", lambda e: e.tensor_scalar(out=smv(5), in0=a_re, scalar1=-1.0, scalar2=None, op0=ALU.add), r=["Ap"], w=["s5k"])
            op("dve", lambda e: e.tensor_tensor(out=smv(6), in0=smv(0), in1=smv(0), op=ALU.mult), r=["s5p"], w=["s5k6"])
            op("dve", lambda e: e.tensor_tensor(out=smv(7), in0=smv(1), in1=smv(1), op=ALU.mult), r=["s5p"], w=["s5k7"])
            op("dve", lambda e: e.tensor_tensor(out=smv(6), in0=smv(6), in1=smv(7), op=ALU.add), r=["s5k6", "s5k7"], w=["s5k6"])
            op("dve", lambda e: e.reciprocal(out=smv(6), in_=smv(6)), r=["s5k6"], w=["s5k6"])
            op("dve", lambda e: e.tensor_tensor(out=smv(7), in0=smv(5), in1=smv(0), op=ALU.mult), r=["s5k", "s5p", "s5k6"], w=["s5k7"])
            op("dve", lambda e: e.tensor_tensor(out=smv(8), in0=a_im, in1=smv(1), op=ALU.mult), r=["Ap", "s5p"], w=["s5k8"])
            op("dve", lambda e: e.tensor_tensor(out=smv(7), in0=smv(7), in1=smv(8), op=ALU.add), r=["s5k7", "s5k8"], w=["s5k7"])
            op("dve", lambda e: e.tensor_tensor(out=smv(9), in0=smv(7), in1=smv(6), op=ALU.mult), r=["s5k7", "s5k6"], w=["s5kre"])
            op("dve", lambda e: e.tensor_tensor(out=smv(7), in0=a_im, in1=smv(0), op=ALU.mult), r=["Ap", "s5p", "s5kre"], w=["s5k7"])
            op("dve", lambda e: e.tensor_tensor(out=smv(8), in0=smv(5), in1=smv(1), op=ALU.mult), r=["s5k", "s5p", "s5k7"], w=["s5k8"])
            op("dve", lambda e: e.tensor_tensor(out=smv(7), in0=smv(7), in1=smv(8), op=ALU.subtract), r=["s5k7", "s5k8"], w=["s5k7"])
            op("dve", lambda e: e.tensor_tensor(out=smv(10), in0=smv(7), in1=smv(6), op=ALU.mult), r=["s5k7", "s5k6"], w=["s5kim"])
            bc = lambda v: v.unsqueeze(2).broadcast_to([128, 16, 16])
            cmul(BB[:, 0], BB[:, 1], bc(smv(9)), bc(smv(10)), Braw[:, 0], Braw[:, 1], [128, 16, 16], ["s5kre", "s5kim", "s5B"], ["BB"])
            self._ka(4)
            def pw(ri, d, start, step):
                a = Ap[:, ri, d * 8:(d + 1) * 8, :]
                aa = a.ap
                return bass.AP(a.tensor, a.offset + start * aa[2][0], [list(aa[0]), list(aa[1]), [step * aa[2][0], 8], [0, 16]])
            for d in range(2):
                xs = slice(d * 8, (d + 1) * 8)
                bsv = lambda ri: BS[:, ri, xs, :].rearrange("p g (s h) -> p g s h", s=8)
                rrv = lambda ri: RR[:, ri, xs, :].rearrange("p g (s h) -> p g s h", s=8)
                bbv = lambda ri: BB[:, ri, xs, :].unsqueeze(2).broadcast_to([128, 8, 8, 16])
                ctv = lambda ri: CT[:, ri, xs, :].unsqueeze(2).broadcast_to([128, 8, 8, 16])
                st_b, sp_b = (14, -1) if d == 0 else (7, 1)
                st_r, sp_r = (0, 1) if d == 0 else (7, -1)
                cmul(bsv(0), bsv(1), pw(0, d, st_b, sp_b), pw(1, d, st_b, sp_b), bbv(0), bbv(1), [128, 8, 8, 16],
                     ["Ap", "BB"], [("BS", d)])
                cmul(rrv(0), rrv(1), pw(0, d, st_r, sp_r), pw(1, d, st_r, sp_r), ctv(0), ctv(1), [128, 8, 8, 16],
                     ["Ap", "CT"], [("RR", d), "Y2"], neg_im=True)
            self._ka(5)
            Dd = t1[:, 0:512]
            for gp in range(2):
                ps_ = slice(gp * 64, (gp + 1) * 64)
                for b4 in range(2):
                    pf, pb = next_ps(), next_ps()
                    for gi in range(4):
                        g8 = b4 * 4 + gi
                        for d, pp in ((0, pf), (1, pb)):
                            for ri in range(2):
                                op("pe", lambda e: e.matmul(PS[pp][:, gi * 128:(gi + 1) * 128], BS[ps_, ri, d * 8 + g8, :],
                                                            RR[ps_, ri, d * 8 + g8, :], start=(ri == 0), stop=(ri == 1)),
                                   r=[("BS", d), ("RR", d)], w=[psk(pp)])
                    mk = lambda m: m.unsqueeze(1).broadcast_to([128, 4, 128])
                    pv = lambda pp: PS[pp][:].rearrange("p (g k) -> p g k", g=4)
                    tv = lambda t: t[:, 0:512].rearrange("p (g k) -> p g k", g=4)
                    op("dve", lambda e: e.tensor_tensor(out=tv(t2), in0=pv(pf), in1=mk(maskf), op=ALU.mult), r=[psk(pf), "s5mask", "t2", "t2b"], w=["t2"])
                    op("dve", lambda e: e.tensor_tensor(out=tv(t1), in0=pv(pb), in1=mk(maskb), op=ALU.mult), r=[psk(pb), "s5mask", "t1", "t1b"], w=["t1"])
                    op("dve", lambda e: e.tensor_tensor(out=tv(t2), in0=tv(t2), in1=tv(t1), op=ALU.add), r=["t1", "t2"], w=["t2"])
                    for gi in range(4):
                        g = 2 * (b4 * 4 + gi) + gp
                        op("dve", lambda e: e.scalar_tensor_tensor(out=MI[:, g, :], in0=ident[:], scalar=Dcol[:, G0 + g:G0 + g + 1],
                                                                   op0=ALU.mult, in1=t2[:, gi * 128:(gi + 1) * 128], op1=ALU.add),
                           r=["t2", "dcol", "ident"], w=[("MI", g)])
            self._ka(6)
            ei = 0
            for ri in range(2):
                for d in range(2):
                    for gp in range(2):
                        ps_ = slice(gp * 64, (gp + 1) * 64)
                        pi = next_ps()
                        pvb = PS[pi][:].bitcast(BF16)
                        for g8 in range(8):
                            op("pe", lambda e: e.transpose(pvb[:, g8 * 64:(g8 + 1) * 64], BS[ps_, ri, d * 8 + g8, :], identb[ps_, ps_]),
                               r=[("BS", d), "identb"], w=[psk(pi)])
                        bdst = BST[:, ri, d * 16:(d + 1) * 16, :].rearrange("p (g8 gp) q -> p g8 gp q", gp=2)[:, :, gp, :]
                        bsrc = pvb[:, 0:512].rearrange("p (g q) -> p g q", g=8)
                        ei += 1
                        if ei % 2:
                            op("act", lambda e: e.activation(out=bdst, in_=bsrc, func=AF.Copy), r=[psk(pi)], w=["BST", "Y2", "h0b"])
                        else:
                            op("dve", lambda e: e.tensor_copy(out=bdst, in_=bsrc), r=[psk(pi)], w=["BST", "Y2", "h0b"])
            self._ka(7)
            a8r = lambda d: Ap[:, 0, d * 8:(d + 1) * 8, 15].unsqueeze(2).broadcast_to([128, 8, 128])
            a8i = lambda d: Ap[:, 1, d * 8:(d + 1) * 8, 15].unsqueeze(2).broadcast_to([128, 8, 128])
            for d in range(2):
                xs = slice(d * 8, (d + 1) * 8)
                v1 = t1[:, 0:1024].rearrange("p (g k) -> p g k", g=8)
                v2 = t2[:, 0:1024].rearrange("p (g k) -> p g k", g=8)
                op("dve", lambda e: e.tensor_tensor(out=v1, in0=RR[:, 0, xs, :], in1=a8r(d), op=ALU.mult), r=[("RR", d), "Ap", "t1", "t1b"], w=["t1"])
                op("dve", lambda e: e.tensor_tensor(out=v2, in0=RR[:, 1, xs, :], in1=a8i(d), op=ALU.mult), r=[("RR", d), "Ap", "t2", "t2b"], w=["t2"])
                op("dve", lambda e: e.tensor_tensor(out=BS[:, 0, xs, :], in0=v1, in1=v2, op=ALU.add), r=["t1", "t2", "BST"], w=[("BS", d)])
                op("dve", lambda e: e.tensor_tensor(out=v1, in0=RR[:, 1, xs, :], in1=a8r(d), op=ALU.mult), r=[("RR", d), "Ap", "t1"], w=["t1"])
                op("dve", lambda e: e.tensor_tensor(out=v2, in0=RR[:, 0, xs, :], in1=a8i(d), op=ALU.mult), r=[("RR", d), "Ap", "t2"], w=["t2"])
                op("dve", lambda e: e.tensor_tensor(out=BS[:, 1, xs, :], in0=v1, in1=v2, op=ALU.subtract), r=["t1", "t2"], w=[("BS", d)])
            self._ka(8)
            for s_ in range(8):
                for k2 in range(2):
                    srcU = uaD[2 * hf + k2].rearrange("(g h) (s c) -> h g s c", h=16, s=8)[:, :, s_, :]
                    dma(U2[s_ * 16:(s_ + 1) * 16, 8 * k2:8 * k2 + 8, :], srcU, r=[("uaD", 2 * hf + k2), "U2"], w=["U2"])
            self._ka(9)
            for d in range(2):
                for ri in range(2):
                    for g8 in range(8):
                        pi = next_ps()
                        for gp in range(2):
                            op("pe", lambda e: e.matmul(PS[pi][gp * 64:(gp + 1) * 64, 0:CP], BST[:, ri, d * 16 + 2 * g8 + gp, :],
                                                        U2[:, 2 * g8 + gp, :], start=True, stop=True),
                               r=["BST", "U2"], w=[psk(pi)])
                        if g8 % 2:
                            op("act", lambda e: e.activation(out=Sx[:, ri, d, g8, :], in_=PS[pi][:, 0:CP], func=AF.Copy), r=[psk(pi)], w=["Sx"])
                        else:
                            op("dve", lambda e: e.tensor_copy(out=Sx[:, ri, d, g8, :], in_=PS[pi][:, 0:CP]), r=[psk(pi)], w=["Sx"])
            self._ka(10)
            S.barrier()
            if hf == 0 and l + 1 < DEPTH:
                self.mod_start(l + 1)
            NB, BL = 20, 16
            cur5 = t1[:, 0:640].rearrange("p (r d g b) -> p r d g b", r=2, d=2, g=8)
            Ta = t2[:, 0:640]
            Tb = R1[:, oXS:oXS + 640]
            CF = R1[:, oTG:oTG + 640].rearrange("p (r d g b) -> p r d g b", r=2, d=2, g=8)
            v4 = lambda a: a.rearrange("p (r x b) -> p r x b", r=2, x=16)
            v3 = lambda a: a.rearrange("p (r d m) -> p r d m", r=2, d=2)
            curF = t1[:, 0:640]
            CFf = R1[:, oTG:oTG + 640]
            oz = oRB
            PRt = R1[:, oz:oz + 512].rearrange("p (r x k) -> p r x k", r=2, x=16); oz += 512
            PIt = R1[:, oz:oz + 512].rearrange("p (r x k) -> p r x k", r=2, x=16); oz += 512
            E8 = R1[:, oz:oz + 256].rearrange("p (x k) -> p x k", x=16); oz += 256
            U8 = R1[:, oz:oz + 256].rearrange("p (x k) -> p x k", x=16); oz += 256
            U8c = R1[:, oz:oz + 256].rearrange("p (x k) -> p x k", x=16); oz += 256
            W8 = R1[:, oz:oz + 256].rearrange("p (x k) -> p x k", x=16); oz += 256
            n8 = R1[:, oz:oz + 16]; oz += 16
            h0b = R1[:, oRB + 4000:oRB + 4016].bitcast(BF16).rearrange("p (r d g) -> p r d g", r=2, d=2)
            Ts1 = R1[:, oz:oz + 32].rearrange("p (r d g) -> p r d g", r=2, d=2); oz += 32
            Ts2 = R1[:, oz:oz + 32].rearrange("p (r d g) -> p r d g", r=2, d=2); oz += 32
            op("pool", lambda e: e.iota(n8, pattern=[[8, 16]], base=8, channel_multiplier=0, allow_small_or_imprecise_dtypes=True), w=["n8"])
            n8b = n8.unsqueeze(1).broadcast_to([128, 16, 16])
            op("dve", lambda e: e.tensor_tensor(out=E8, in0=smv(3).unsqueeze(2).broadcast_to([128, 16, 16]), in1=n8b, op=ALU.mult),
               r=["s5lrd", "n8"], w=["E8"])
            op("dve", lambda e: e.tensor_tensor(out=U8, in0=smv(4).unsqueeze(2).broadcast_to([128, 16, 16]), in1=n8b, op=ALU.mult),
               r=["s5th", "n8"], w=["U8"])
            op("act", lambda e: e.activation(out=E8, in_=E8, func=AF.Exp), r=["E8"], w=["E8"])
            op("dve", lambda e: e.tensor_scalar(out=U8c, in0=U8, scalar1=0.25, scalar2=None, op0=ALU.add), r=["U8"], w=["U8c"])
            self.sin_turns(PIt[:, 1], U8, W8, ["U8"], ["PIs"], "W8")
            self.sin_turns(PRt[:, 0], U8c, W8, ["U8c"], ["PRc"], "W8")
            op("dve", lambda e: e.tensor_tensor(out=PRt[:, 0], in0=PRt[:, 0], in1=E8, op=ALU.mult), r=["PRc", "E8"], w=["PRt"])
            op("dve", lambda e: e.tensor_copy(out=PRt[:, 1], in_=PRt[:, 0]), r=["PRt"], w=["PRt"])
            op("dve", lambda e: e.tensor_tensor(out=PIt[:, 1], in0=PIt[:, 1], in1=E8, op=ALU.mult), r=["PIs", "E8"], w=["PIt"])
            op("dve", lambda e: e.tensor_scalar(out=PIt[:, 0], in0=PIt[:, 1], scalar1=-1.0, scalar2=None, op0=ALU.mult), r=["PIt"], w=["PIt"])
            op("dve", lambda e: e.tensor_copy(out=h0b, in_=h0), r=["s5h0"], w=["h0b"])
            bb = lambda tab, k: tab[:, :, :, k].unsqueeze(3).broadcast_to([128, 2, 16, NB])
            sm3 = lambda tab, k: tab[:, :, :, k].rearrange("p r (d g) -> p r d g", d=2)

            def sx_at(kf, kb):
                a = Sx.ap
                return bass.AP(Sx.tensor, Sx.offset + kf * a[4][0],
                               [list(a[0]), list(a[1]), [a[2][0] + (kb - kf) * a[4][0], 2], [BL * a[4][0], 8 * NB]])

            def blk_at(t5, bf, bb_):
                a = t5.ap
                return bass.AP(t5.tensor, t5.offset + bf * a[4][0], [list(a[0]), list(a[1]), [a[2][0] + (bb_ - bf) * a[4][0], 2], list(a[3])])

            def cstep(dst, src_c, src_add, PR, PI, big):
                ta = v4(Ta) if big else Ts1
                tb_ = v4(Tb) if big else Ts2
                kk = ["Ta", "Tb"] if big else ["Ts1", "Ts2"]
                op("dve", lambda e: e.tensor_tensor(out=ta, in0=src_c, in1=PR, op=ALU.mult), r=["scan", "PRt", "PIt"], w=[kk[0]])
                op("dve", lambda e: e.tensor_tensor(out=tb_, in0=swapped(src_c), in1=PI, op=ALU.mult), r=["scan", "PRt", "PIt"], w=[kk[1]])
                op("dve", lambda e: e.tensor_tensor(out=ta, in0=ta, in1=tb_, op=ALU.add), r=kk, w=[kk[0]])
                return ta
            op("dve", lambda e: e.memset(curF, 0.0), r=["t1", "t1b"], w=["scan"])
            for k in range(BL):
                ta = cstep(None, v4(curF), None, bb(PRt, 0), bb(PIt, 0), True)
                sxk = sx_at(k, BL - 1 - k)
                op("dve", lambda e: e.tensor_tensor(out=v3(curF), in0=v3(Ta), in1=sxk, op=ALU.add), r=["Ta", "Sx"], w=["scan"])
                op("act", lambda e: e.activation(out=sxk, in_=v3(curF), func=AF.Copy), r=["scan"], w=["Sx"])
            op("dve", lambda e: e.memset(CFf, 0.0), w=["scan"])
            op("dve", lambda e: e.tensor_copy(out=blk_at(CF, 0, 15), in_=h0), r=["s5h0", "scan"], w=["scan"])
            for (bf_dst, bb_dst, bf_src, bb_src) in ((17, 16, 16, 17), (19, 18, 18, 19)):
                op("dve", lambda e: e.tensor_copy(out=blk_at(CF, bf_dst, bb_dst), in_=blk_at(cur5, bf_src, bb_src)), r=["scan"], w=["scan"])
            for j in range(1, 16):
                prev = blk_at(CF, j - 1, 16 - j)
                ta = cstep(None, prev, None, sm3(PRt, 15), sm3(PIt, 15), False)
                op("dve", lambda e: e.tensor_tensor(out=blk_at(CF, j, 15 - j), in0=ta, in1=blk_at(cur5, j - 1, 16 - j), op=ALU.add),
                   r=["Ts1", "scan"], w=["scan"])
            for si, (bf_, bb_) in ((1, (17, 16)), (2, (19, 18))):
                ta = cstep(None, blk_at(CF, bf_, bb_), None, sm3(PRt, 15), sm3(PIt, 15), False)
                op("dve", lambda e: e.tensor_tensor(out=curP, in0=ta, in1=blk_at(cur5, bf_, bb_), op=ALU.add), r=["Ts1", "scan", "curP"], w=["curP"])
                with nc.allow_non_contiguous_dma(reason="small transposed state stores"):
                    for d in range(2):
                        for ri, dst in enumerate((c["ns_re"], c["ns_im"])):
                            for gp in range(2):
                                dma(dst[si - 1, l, d, G0:G0 + 16, :].rearrange("(g8 gp) p -> gp p g8", gp=2)[gp],
                                    curP[gp * 64:(gp + 1) * 64, ri, d, :], r=["curP"], w=[("ns", si, l, d, ri, hf, gp)])
            Ta2 = t1[:, 0:640]
            Tb2 = R1[:, oRB + 2400:oRB + 3040]
            for k in range(BL):
                ta_, tb2_, kk = (v4(Ta), v4(Tb), ["Ta", "Tb"]) if k % 2 == 0 else (v4(Ta2), v4(Tb2), ["Ta2", "Tb2"])
                rk3 = ["scan", "PRt", "PIt"] + (["Ts1"] if k < 2 else [])
                op("dve", lambda e: e.tensor_tensor(out=ta_, in0=v4(CFf), in1=bb(PRt, k), op=ALU.mult), r=rk3, w=[kk[0]])
                op("dve", lambda e: e.tensor_tensor(out=tb2_, in0=swapped(v4(CFf)), in1=bb(PIt, k), op=ALU.mult), r=rk3, w=[kk[1]])
                op("dve", lambda e: e.tensor_tensor(out=ta_, in0=ta_, in1=tb2_, op=ALU.add), r=kk, w=[kk[0]])
                sxk = sx_at(k, BL - 1 - k)
                tsrc = Ta if k % 2 == 0 else Ta2
                op("dve", lambda e: e.tensor_tensor(out=sxk, in0=v3(tsrc), in1=sxk, op=ALU.add), r=[kk[0], "Sx"], w=[("Sxk", k)])
            self._ka(11)
            S.barrier()
            SEQB = [(0, 256), (256, 32), (288, 32)]
            for g in range(16):
                g8, gp = g // 2, g % 2
                ps_ = slice(gp * 64, (gp + 1) * 64)
                pi = next_ps()
                mms = [(PS[pi][:, 0:CP], MI[:, g, :], U2[:, g, :], [("MI", g), "U2"])]
                for ri in range(2):
                    Of, Ob = BS[ps_, ri, g8, :], BS[ps_, ri, 8 + g8, :]
                    for (c0, n) in SEQB:
                        mms.append((PS[pi][:, c0 + 1:c0 + n], Of, Sx[ps_, ri, 0, g8, c0:c0 + n - 1], [("BS", 0), "Sx"]))
                        mms.append((PS[pi][:, c0:c0 + n - 1], Ob, Sx[ps_, ri, 1, g8, c0 + 1:c0 + n], [("BS", 1), "Sx"]))
                    mms.append((PS[pi][:, 0:1], Of, h0b[ps_, ri, 0, g8:g8 + 1], [("BS", 0), "h0b"]))
                    mms.append((PS[pi][:, 255:256], Ob, h0b[ps_, ri, 1, g8:g8 + 1], [("BS", 1), "h0b"]))
                for mi_, (o_, l_, r_, k_) in enumerate(mms):
                    op("pe", lambda e: e.matmul(o_, l_, r_, start=(mi_ == 0), stop=(mi_ == len(mms) - 1)), r=k_, w=[psk(pi)])
                xv, tv_ = xs_[g % 2], tg_[g % 2]
                op("act", lambda e: e.activation(out=xv, in_=PS[pi][:, 0:CP], func=AF.Copy), r=[psk(pi), "scan", "Tb"], w=[("xs", g % 2)])
                op("act", lambda e: e.activation(out=tv_, in_=PS[pi][:, 0:CP], func=AF.Square), r=[psk(pi), "scan"], w=[("tg", g % 2)])
                op("dve", lambda e: e.tensor_scalar(out=tv_, in0=tv_, scalar1=0.044715, scalar2=1.0, op0=ALU.mult, op1=ALU.add),
                   r=[("tg", g % 2)], w=[("tg", g % 2)])
                op("dve", lambda e: e.tensor_tensor(out=tv_, in0=tv_, in1=xv, op=ALU.mult), r=[("tg", g % 2), ("xs", g % 2)], w=[("tg", g % 2)])
                op("act", lambda e: e.activation(out=tv_, in_=tv_, func=AF.Sigmoid, scale=1.5957691216057308), r=[("tg", g % 2)], w=[("tg", g % 2)])
                op("dve", lambda e: e.tensor_tensor(out=Y2[:, g, :], in0=xv, in1=tv_, op=ALU.mult), r=[("tg", g % 2), ("xs", g % 2)], w=["Y2"])
            dma(y2D[hf].rearrange("p (g c) -> p g c", g=16), Y2, r=["Y2"], w=[("y2D", hf)])
        bi = self.wbuf_i
        self.wbuf_i = (bi + 1) % 2
        wg = self.wbuf[bi][:, 0:2048].rearrange("p (k c) -> p k c", k=4)
        c["wload"](wg, W["s5_w_glu"][l].rearrange("(k p) c -> p k c", p=128), ("wbuf", bi))
        S.barrier()
        self._ka(12)
        o = SCR0
        ygl = R1[:, o:o + 2 * T].bitcast(BF16).rearrange("p (k t c) -> p k t c", k=4, t=8); o += 2 * T
        sgt = [R1[:, o + i * 512:o + (i + 1) * 512] for i in range(2)]; o += 1024
        for k4 in range(4):
            hf, k2 = k4 // 2, k4 % 2
            for gl in range(8):
                srcY = y2D[hf].rearrange("(t h) (g c) -> h g t c", h=16, g=16)[:, 8 * k2 + gl, :, :]
                dma(ygl[gl * 16:(gl + 1) * 16, k4, :, :], srcY, r=[("y2D", hf)], w=[("ygl", k4)])
        self._ka(13)
        yglf = ygl.rearrange("p k t c -> p k (t c)")
        yjf = Yv[0]
        for m in range(4):
            for tb in range(NTB):
                ts = slice(tb * 512, (tb + 1) * 512)
                pi = next_ps()
                for k4 in range(4):
                    op("pe", lambda e: e.matmul(PS[pi][:], wg[:, k4, m * 128:(m + 1) * 128], yglf[:, k4, ts], start=(k4 == 0), stop=(k4 == 3)),
                       r=[("wbuf", bi), ("ygl", k4)], w=[psk(pi)])
                st_ = sgt[tb % 2]
                op("act", lambda e: e.activation(out=st_, in_=PS[pi][:], func=AF.Sigmoid, bias=c["smallv"][:, 60 + m:61 + m], scale=1.0),
                   r=[psk(pi), "s5bg"], w=[("sgt", tb % 2)])
                op("dve", lambda e: e.tensor_tensor(out=yjf[:, m, ts], in0=st_, in1=yglf[:, m, ts], op=ALU.mult),
                   r=[("sgt", tb % 2), ("ygl", m)], w=[("y", 0, m, tb2) for tb2 in range(NTB)])
        def evac_g(mi, tb, pi):
            st_ = sgt[tb % 2]
            op("act", lambda e: e.activation(out=st_, in_=PS[pi][:], func=AF.Silu), r=[psk(pi)], w=[("sgt", tb % 2)])
            dstv = yj[:, mi, :, tb * 64:(tb + 1) * 64]
            op("dve", lambda e: e.tensor_tensor(out=dstv, in0=st_.rearrange("p (c s) -> p s c", s=8), in1=dstv, op=ALU.mult),
               r=[("sgt", tb % 2)] + [("y", 0, mi, tb2) for tb2 in range(NTB)], w=[("y", 0, mi, tb2) for tb2 in range(NTB)])
        c["proj"](C_GA, 4, evac_g)

    def _phase2(self):
        c = self.ctx
        op, dma, PS, next_ps, psk, W, l = c["op"], c["dma"], c["PS"], c["next_ps"], c["psk"], c["W"], c["l"]
        R1, R1b, SCR0, Yv, h3, wbuf, modT, xsp3 = (c["R1"], c["R1b"], c["SCR0"], c["Yv"], c["h3"], c["wbuf"], c["modT"],
                                                    c["xsp3"])
        wload = c["wload"]
        mg = R1[:, SCR0:SCR0 + 4 * T].bitcast(BF16).rearrange("p (k t) -> p k t", k=KT)
        o2 = SCR0 + 4 * T
        sig = [R1[:, o2 + i * 512:o2 + (i + 1) * 512] for i in range(3)]
        o2 += 3 * 512
        acc = [R1[:, o2 + i * 512:o2 + (i + 1) * 512] for i in range(2)]
        o2 += 2 * 512
        xo = [R1[:, o2 + i * 512:o2 + (i + 1) * 512] for i in range(3)]
        o2 += 3 * 512
        assert o2 <= R1N
        br_names = ["w_br_a", "w_br_b", "w_br_c"]
        for f in range(KT):
            bi = self.wbuf_i
            self.wbuf_i = (bi + 1) % 2
            wm = wbuf[bi][:, 0:3 * KT * 128].rearrange("p (b k c) -> p b k c", b=3, k=KT)
            wb = wbuf[bi][:, 3072:3072 + 3 * 4 * 128].rearrange("p (b k c) -> p b k c", b=3, k=4)
            engs = ["act", "dve", "act"]
            for b in range(3):
                c0 = C_M + b * D + f * 128
                wload(wm[:, b], W["w_in"][l][:, c0:c0 + 128].rearrange("(k p) c -> p k c", p=128), ("wbuf", bi, "m", b),
                      eng=engs[b])
                wload(wb[:, b], W[br_names[b]][l][:, f * 128:(f + 1) * 128].rearrange("(k p) c -> p k c", p=128),
                      ("wbuf", bi, "b", b), eng=engs[b])
            for tb in range(NTB):
                ts = slice(tb * 512, (tb + 1) * 512)
                a = acc[tb % 2]
                for b in range(3):
                    pm = next_ps()
                    for kt in range(KT):
                        op("pe", lambda e, pm=pm, kt=kt, b=b: e.matmul(PS[pm][:], wm[:, b, kt, :], h3[:, kt, ts],
                                                                       start=(kt == 0), stop=(kt == KT - 1)),
                           r=[("wbuf", bi, "m", b), ("h", kt, tb)], w=[psk(pm)])
                    pp = next_ps()
                    for k4 in range(4):
                        yrhs = (Yv[b][:, k4, ts] if b else
                                Yv[0][:, k4, :].rearrange("p (t c) -> p c t", t=8)[:, tb * 64:(tb + 1) * 64, :])
                        op("pe", lambda e, pp=pp, k4=k4, b=b, yrhs=yrhs: e.matmul(PS[pp][:], wb[:, b, k4, :], yrhs,
                                                                       start=(k4 == 0), stop=(k4 == 3)),
                           r=[("wbuf", bi, "b", b), ("y", b, k4, tb)], w=[psk(pp)])
                    op("act", lambda e, pm=pm, b=b: e.activation(out=sig[b][:], in_=PS[pm][:], func=AF.Sigmoid),
                       r=[psk(pm)], w=[("sig", b)])
                    if b == 0:
                        op("dve", lambda e, pp=pp, b=b, a=a: e.tensor_tensor(out=a, in0=PS[pp][:], in1=sig[b][:], op=ALU.mult),
                           r=[psk(pp), ("sig", b)], w=[("acc", tb % 2)])
                    else:
                        op("dve", lambda e, pp=pp, b=b: e.tensor_tensor(out=sig[b][:], in0=PS[pp][:], in1=sig[b][:], op=ALU.mult),
                           r=[psk(pp), ("sig", b)], w=[("sig", b)])
                        last = (b == 2)
                        dst = mg[:, f, ts] if last else a
                        wk = [("mg", f, tb)] if last else [("acc", tb % 2)]
                        op("dve", lambda e, b=b, dst=dst, a=a: e.tensor_tensor(out=dst, in0=a, in1=sig[b][:], op=ALU.add),
                           r=[("acc", tb % 2), ("sig", b)], w=wk)
        if "mg%d" % l in c["dbg_out"]:
            self._dump_bf16(mg, c["dbg_out"]["mg%d" % l], [("mg", f, tb) for f in range(KT) for tb in range(NTB)])
        for f2 in range(0, KT, 2):
            bi = self.wbuf_i
            self.wbuf_i = (bi + 1) % 2
            wo = wbuf[bi][:, 0:KT * 256].rearrange("p (k c) -> p k c", k=KT)
            wload(wo, W["w_out"][l][:, f2 * 128:f2 * 128 + 256].rearrange("(k p) c -> p k c", p=128),
                  [("wbuf", bi)] + [("wbuf", bi, t_, b_) for t_ in ("m", "b") for b_ in range(3)])
            for ff in range(2):
                f = f2 + ff
                for tb in range(NTB):
                    ts = slice(tb * 512, (tb + 1) * 512)
                    j = 0 if tb < 4 else 1
                    xi = (f * NTB + tb) % 3
                    dma(xo[xi], xsp3[:, f, ts], r=[("xsp", f, tb)], w=[("xo", xi)])
                    pi = next_ps()
                    for kt in range(KT):
                        op("pe", lambda e, pi=pi, kt=kt, ff=ff: e.matmul(PS[pi][:], wo[:, kt, ff * 128:(ff + 1) * 128],
                                                                         mg[:, kt, ts], start=(kt == 0), stop=(kt == KT - 1)),
                           r=[("wbuf", bi), ("mg", kt, tb)], w=[psk(pi)])
                    op("dve", lambda e, pi=pi, xi=xi, f=f, j=j: e.scalar_tensor_tensor(
                        out=xo[xi], in0=PS[pi][:], scalar=modT[:, 16 + f, j:j + 1], op0=ALU.mult, in1=xo[xi], op1=ALU.add),
                       r=[psk(pi), ("xo", xi), "modT"], w=[("xo", xi)])
                    dma(xsp3[:, f, ts], xo[xi], r=[("xo", xi)], w=[("xsp", f, tb)])

    def _final(self, x3, xsp3, rstd, onesm, PS, next_ps, psk, W, yout, xtm, ident):
        op, dma = self.S.op, self.S.dma
        nc = self.nc
        fg = self.sb("fg", [128, KT], F32)
        with nc.allow_non_contiguous_dma(reason="small transposed vector loads"):
            dma(fg[:], W["final_g"].rearrange("(k p) -> p k", p=128), w=["fg"])
        for tb in range(NTB):
            dma(x3[:, :, tb * 512:(tb + 1) * 512], xsp3[:, :, tb * 512:(tb + 1) * 512],
                r=[("xsp", f, tb) for f in range(KT)], w=[("x", tb)])
        self._norm_to(x3, rstd, onesm, PS, next_ps, psk)
        for kt in range(KT):
            for tb in range(NTB):
                ts = slice(tb * 512, (tb + 1) * 512)
                op("dve", lambda e, kt=kt, ts=ts: e.scalar_tensor_tensor(
                    out=x3[:, kt, ts], in0=x3[:, kt, ts], scalar=fg[:, kt:kt + 1], op0=ALU.mult, in1=rstd[:, ts], op1=ALU.mult),
                   r=[("x", tb), "rstd", "fg"], w=[("x", tb)])
        for tt in range(T // 128):
            b = tt % 2
            tsl = slice(tt * 128, (tt + 1) * 128)
            for half in range(2):
                pi = next_ps()
                for j in range(4):
                    kt = half * 4 + j
                    op("pe", lambda e, kt=kt, j=j, pi=pi: e.transpose(PS[pi][:, j * 128:(j + 1) * 128], x3[:, kt, tsl], ident[:]),
                       r=[("x", tt // 4), "ident"], w=[psk(pi)])
                dst = xtm[b][:, half * 512:(half + 1) * 512]
                if half == 0:
                    op("act", lambda e, dst=dst, pi=pi: e.activation(out=dst, in_=PS[pi][:], func=AF.Copy),
                       r=[psk(pi)], w=[("xtm", b)])
                else:
                    op("dve", lambda e, dst=dst, pi=pi: e.tensor_copy(out=dst, in_=PS[pi][:]), r=[psk(pi)], w=[("xtm", b)])
            dma(yout[tsl, :], xtm[b][:], r=[("xtm", b)], w=[("yout", tt)])


    def sin_turns(self, out, u, tmp, rk, wk, tk, eng="dve"):
        op = self.S.op
        op(eng, lambda e: e.tensor_scalar(out=tmp, in0=u, scalar1=MAGIC, scalar2=MAGIC, op0=ALU.add, op1=ALU.subtract),
           r=rk, w=[tk])
        op(eng, lambda e: e.tensor_tensor(out=tmp, in0=u, in1=tmp, op=ALU.subtract), r=list(rk) + [tk], w=[tk])
        op("act", lambda e: e.activation(out=out, in_=tmp, func=AF.Sin, scale=TWO_PI_S), r=[tk], w=wk)

    def _pos_embed(self, x3):
        op = self.S.op
        R1 = self.R1
        o = 26112
        omg = R1[:, o:o + 2]; o += 2
        posv = R1[:, o:o + 64]; o += 64
        pu = R1[:, o:o + 384]; o += 384
        pt = R1[:, o:o + 384]; o += 384
        pe = R1[:, o:o + 384]; o += 384
        op("pool", lambda e: e.iota(omg, pattern=[[128, 2]], base=0, channel_multiplier=1,
                                    allow_small_or_imprecise_dtypes=True), w=["omg"])
        op("pool", lambda e: e.iota(posv, pattern=[[1, 64]], base=0, channel_multiplier=0,
                                    allow_small_or_imprecise_dtypes=True), w=["posv"])
        op("act", lambda e: e.activation(out=omg, in_=omg, func=AF.Exp, scale=-math.log(10000.0) / 256.0),
           r=["omg"], w=["omg"])
        op("dve", lambda e: e.tensor_scalar(out=omg, in0=omg, scalar1=1.0 / (2 * math.pi), scalar2=None, op0=ALU.mult),
           r=["omg"], w=["omg"])
        for kt in range(8):
            n = 32 if kt < 4 else 64
            off = kt * 32 if kt < 4 else 128 + (kt - 4) * 64
            ph = 0.25 if (kt % 4) >= 2 else 0.0
            c = kt % 2
            op("dve", lambda e, n=n, off=off, ph=ph, c=c: e.tensor_scalar(
                out=pu[:, off:off + n], in0=posv[:, 0:n], scalar1=omg[:, c:c + 1], scalar2=ph, op0=ALU.mult, op1=ALU.add),
               r=["posv", "omg"], w=["pu"])
        self.sin_turns(pe, pu, pt, ["pu"], ["pe"], "pt")
        for kt in range(8):
            xs = x3[:, kt, 0:LS].rearrange("p (r c) -> p r c", c=64)
            if kt < 4:
                tb_ = pe[:, kt * 32:(kt + 1) * 32].unsqueeze(2).broadcast_to([128, 32, 64])
            else:
                tb_ = pe[:, 128 + (kt - 4) * 64:128 + (kt - 3) * 64].unsqueeze(1).broadcast_to([128, 32, 64])
            op("dve", lambda e, xs=xs, tb_=tb_: e.tensor_tensor(out=xs, in0=xs, in1=tb_, op=ALU.add),
               r=["pe"] + [("x", b) for b in range(4)], w=[("x", b) for b in range(4)])

    def _setup_tables(self):
        op, dma = self.S.op, self.S.dma
        R1 = self.R1
        self.tabs = {}
        o = 0
        kk2 = R1[:, o:o + 1536]; o += 1536
        tpos = R1[:, o:o + 2048]; o += 2048
        mm = [R1[:, o + i * 2048:o + (i + 1) * 2048] for i in range(2)]; o += 4096
        qq = [R1[:, o + i * 2048:o + (i + 1) * 2048] for i in range(2)]; o += 4096
        ob = [[R1[:, o + (2 * i + c) * 1024:o + (2 * i + c + 1) * 1024].bitcast(BF16) for c in range(2)] for i in range(2)]
        o += 4096
        pcol = R1[:, o:o + 16]; o += 16
        kcol = R1[:, o:o + 16]; o += 16
        op("pool", lambda e: e.iota(pcol, pattern=[[128, 16]], base=0, channel_multiplier=1,
                                    allow_small_or_imprecise_dtypes=True), w=["pcol"])
        op("pool", lambda e: e.iota(kcol, pattern=[[256, 16]], base=1, channel_multiplier=2,
                                    allow_small_or_imprecise_dtypes=True), w=["kcol"])
        op("pool", lambda e: e.iota(kk2, pattern=[[2, 1536]], base=1, channel_multiplier=0,
                                    allow_small_or_imprecise_dtypes=True), w=["kk2"])
        cnt = [0]

        def gen(freev, pscal, Wd, N, sink):
            i = cnt[0] % 2
            cnt[0] += 1
            m, q = mm[i][:, 0:Wd], qq[i][:, 0:Wd]
            oc, os_ = ob[i][0][:, 0:Wd], ob[i][1][:, 0:Wd]
            op("dve", lambda e: e.tensor_scalar(out=m, in0=freev, scalar1=pscal, scalar2=None, op0=ALU.mult),
               r=["kk2", "tpos", "pcol", "kcol"], w=[("m", i)])
            op("dve", lambda e: e.tensor_scalar(out=q, in0=m, scalar1=1.0 / (2 * N), scalar2=MAGIC, op0=ALU.mult, op1=ALU.add),
               r=[("m", i)], w=[("q", i)])
            op("dve", lambda e: e.tensor_scalar(out=q, in0=q, scalar1=MAGIC, scalar2=-2.0 * N, op0=ALU.subtract, op1=ALU.mult),
               r=[("q", i)], w=[("q", i)])
            op("dve", lambda e: e.tensor_tensor(out=m, in0=m, in1=q, op=ALU.add), r=[("m", i), ("q", i)], w=[("m", i)])
            op("act", lambda e: e.activation(out=os_, in_=m, func=AF.Sin, scale=-math.pi / N * (1 - 1e-6)),
               r=[("m", i)], w=[("os", i)])
            op("dve", lambda e: e.scalar_tensor_tensor(out=q, in0=m, scalar=-1.0, op0=ALU.mult, in1=m, op1=ALU.max),
               r=[("m", i)], w=[("q", i)])
            op("act", lambda e: e.activation(out=oc, in_=q, func=AF.Sin, scale=-math.pi / N * (1 - 1e-6), bias=self._halfpi[:]),
               r=[("q", i), "halfpi"], w=[("oc", i)])
            sink(oc, os_, [("oc", i), ("os", i)])

        self._halfpi = self.sb("halfpi", [128, 1], F32)
        op("pool", lambda e: e.memset(self._halfpi[:], math.pi / 2 * (1 - 1e-6)), w=["halfpi"])
        for name, L in (("S", LS), ("P", LP)):
            N = 3 * L // 2
            NK = N // 2
            nI = L // 128
            nJ = (NK + 127) // 128
            kws = [min(128, NK - j * 128) for j in range(nJ)]
            Fc = self.dram_tmp("Fc" + name, [nJ, 128, nI, 128], BF16)
            Fs = self.dram_tmp("Fs" + name, [nJ, 128, nI, 128], BF16)
            Gc = self.dram_tmp("Gc" + name, [nJ, 128, L], BF16)
            Gs = self.dram_tmp("Gs" + name, [nJ, 128, L], BF16)
            self.tabs[name] = dict(L=L, N=N, NK=NK, nI=nI, nJ=nJ, kws=kws, Fc=Fc, Fs=Fs, Gc=Gc, Gs=Gs)
            op("pool", lambda e: e.iota(tpos[:, 0:L], pattern=[[1, L]], base=L // 2, channel_multiplier=0,
                                        allow_small_or_imprecise_dtypes=True), r=["tpos"], w=["tpos"])
            for I in range(nI):
                def sinkF(oc, os_, keys, I=I):
                    for j in range(nJ):
                        kw = kws[j]
                        dma(Fc[j, :, I, 0:kw], oc[:, j * 128:j * 128 + kw], r=keys, w=[("Fc" + name, j)], q="sp")
                        dma(Fs[j, :, I, 0:kw], os_[:, j * 128:j * 128 + kw], r=keys, w=[("Fs" + name, j)], q="sp")
                gen(kk2[:, 0:NK], pcol[:, I:I + 1], NK, N, sinkF)
            for j in range(nJ):
                def sinkG(oc, os_, keys, j=j):
                    dma(Gc[j], oc, r=keys, w=[("Gc" + name, j)], q="sp")
                    dma(Gs[j], os_, r=keys, w=[("Gs" + name, j)], q="sp")
                gen(tpos[:, 0:L], kcol[:, j:j + 1], L, N, sinkG)

    def _filters(self, name):
        c = self.ctx
        op, dma, PS, next_ps, psk, W, l = c["op"], c["dma"], c["PS"], c["next_ps"], c["psk"], c["W"], c["l"]
        R1, SCR0 = c["R1"], c["SCR0"]
        nc = self.nc
        tb = self.tabs[name]
        L, N, NK, nI, nJ, kws = tb["L"], tb["N"], tb["NK"], tb["nI"], tb["nJ"], tb["kws"]
        if "Hf" not in tb:
            tb["Hf"] = self.dram_tmp("Hf" + name, [nJ, 128, 2, 2, 512], BF16)
        Hf = tb["Hf"]
        o = SCR0
        htm = R1[:, o:o + nI * 512].bitcast(BF16).rearrange("p (i c) -> p i c", i=nI); o += 8192
        z = R1[0:65, o:o + L]; o += 2048
        h1 = R1[0:64, o:o + L]; o += 2048
        h2 = R1[0:64, o:o + L]; o += 2048
        tmp = R1[0:65, o:o + L]; o += 2048
        assert o <= R1N
        o = 0
        tio = R1[:, o:o + L]; o += 2048
        dl = R1[:, o:o + 512]; o += 512
        wt = R1[:, o:o + 512]; o += 512
        hst = [R1[:, o + i * 1024:o + (i + 1) * 1024].bitcast(BF16).rearrange("p (r q c) -> p r q c", r=2, q=2) for i in range(2)]
        o += 2048
        sv = self._fsv if hasattr(self, "_fsv") else None
        if sv is None:
            sv = self._fsv = self.sb("fsv", [128, 48], F32)
            self._w1p = self.sb("w1p", [65, 64], F32)
            self._w2 = self.sb("w2f", [64, 64], F32)
        w1p, w2 = self._w1p, self._w2
        w3 = R1[0:64, 5120:6144]
        op("pool", lambda e: e.iota(tio, pattern=[[1, L]], base=0, channel_multiplier=0,
                                    allow_small_or_imprecise_dtypes=True), w=["tio"])
        op("pool", lambda e: e.iota(dl, pattern=[[1, 512]], base=0, channel_multiplier=0,
                                    allow_small_or_imprecise_dtypes=True), w=["dl"])
        dmin = abs(math.log(1e-2) / 1.5)
        dmax = abs(math.log(1e-2) / 0.3)
        op("dve", lambda e: e.tensor_scalar(out=dl, in0=dl, scalar1=(dmax - dmin) / 511.0, scalar2=dmin, op0=ALU.mult, op1=ALU.add),
           r=["dl"], w=["dl"])
        op("pool", lambda e: e.memset(sv[:, 0:2], 0.0), w=["sv01"])
        for p0 in (0, 32):
            op("pool", lambda e, p0=p0: e.iota(sv[p0:p0 + 16, 0:1], pattern=[[0, 1]], base=0, channel_multiplier=1,
                                               allow_small_or_imprecise_dtypes=True), r=["sv01"], w=["sv01"])
            fstep = (15.0 - 1e-4) / 15.0
            op("dve", lambda e, p0=p0: e.tensor_scalar(out=sv[p0:p0 + 16, 0:1], in0=sv[p0:p0 + 16, 0:1], scalar1=fstep / L,
                                                       scalar2=1e-4 / L, op0=ALU.mult, op1=ALU.add), r=["sv01"], w=["sv01"])
            op("pool", lambda e, p0=p0: e.memset(sv[p0:p0 + 16, 1:2], 0.25 if p0 == 0 else 0.5), r=["sv01"], w=["sv01"])
        op("pool", lambda e: e.iota(sv[:, 8:8 + nI], pattern=[[128, nI]], base=-(L // 2), channel_multiplier=1,
                                    allow_small_or_imprecise_dtypes=True), w=["negoff"])
        op("dve", lambda e: e.scalar_tensor_tensor(out=sv[:, 24:24 + nI], in0=sv[:, 8:8 + nI], scalar=-1.0, op0=ALU.mult,
                                                   in1=sv[:, 8:8 + nI], op1=ALU.max), r=["negoff"], w=["negoff2"])
        op("dve", lambda e: e.tensor_scalar(out=sv[:, 8:8 + nI], in0=sv[:, 24:24 + nI], scalar1=-2.0 / L, scalar2=None,
                                            op0=ALU.mult), r=["negoff2"], w=["negoff"])
        op("pool", lambda e: e.memset(w1p[:], 0.0), w=["w1p"])
        with nc.allow_non_contiguous_dma(reason="small transposed vector loads"):
            dma(w1p[0:16, :], W["hy_f_w1"][l][1:17, :], w=["w1p"])
            dma(w1p[32:48, :], W["hy_f_w1"][l][17:33, :], w=["w1p"])
            dma(w1p[64:65, :], W["hy_f_w1"][l][0:1, :], w=["w1p"])
            dma(w2[:], W["hy_f_w2"][l], w=["w2f"])
            dma(w3[:], W["hy_f_w3"][l], w=["w3f"])
            dma(sv[0:64, 2:3], W["hy_f_b1"][l].rearrange("(p o) -> p o", o=1), w=["svw"])
            dma(sv[0:64, 3:4], W["hy_f_freq"][l].rearrange("(p o) -> p o", o=1), w=["svw"])
            dma(sv[0:64, 4:5], W["hy_f_b2"][l].rearrange("(p o) -> p o", o=1), w=["svw"])
        op("dve", lambda e: e.tensor_scalar(out=sv[0:64, 5:6], in0=sv[0:64, 3:4], scalar1=1.0 / (2 * math.pi), scalar2=None,
                                            op0=ALU.mult), r=["svw"], w=["svs"])
        op("dve", lambda e: e.tensor_tensor(out=sv[0:64, 6:7], in0=sv[0:64, 5:6], in1=sv[0:64, 2:3], op=ALU.mult),
           r=["svs", "svw"], w=["svc1"])
        op("dve", lambda e: e.tensor_tensor(out=sv[0:64, 7:8], in0=sv[0:64, 5:6], in1=sv[0:64, 4:5], op=ALU.mult),
           r=["svs", "svw"], w=["svc2"])
        op("dve", lambda e: e.tensor_scalar(out=tmp[0:64, :], in0=tio[0:64, :], scalar1=sv[0:64, 0:1], scalar2=sv[0:64, 1:2],
                                            op0=ALU.mult, op1=ALU.add), r=["tio", "sv01"], w=["fu"])
        self.sin_turns(z[0:64, :], tmp[0:64, :], h1[0:64, :], ["fu"], ["fz"], "ft")
        op("dve", lambda e: e.tensor_scalar(out=z[64:65, :], in0=tio[64:65, :], scalar1=1.0 / (L - 1), scalar2=None, op0=ALU.mult),
           r=["tio"], w=["fz64"])
        nb = (L + 511) // 512
        for (src, ks, wgt, wkey, ccol, dst, rkeys, dkey) in ((z, 65, w1p, "w1p", 6, h1, ["fz", "fz64"], "fh1"),
                                                             (h1, 64, w2, "w2f", 7, h2, ["fh1"], "fh2")):
            for b in range(nb):
                cs = slice(b * 512, min(L, (b + 1) * 512))
                pi = next_ps()
                wd = cs.stop - cs.start
                op("pe", lambda e: e.matmul(PS[pi][0:64, 0:wd], wgt[0:ks, :], src[0:ks, cs], start=True, stop=True),
                   r=rkeys + [wkey], w=[psk(pi)])
                op("dve", lambda e: e.tensor_scalar(out=tmp[0:64, cs], in0=PS[pi][0:64, 0:wd], scalar1=sv[0:64, 5:6],
                                                    scalar2=sv[0:64, ccol:ccol + 1], op0=ALU.mult, op1=ALU.add),
                   r=[psk(pi), "svs", "svc1", "svc2"], w=["fu"])
            self.sin_turns(dst[0:64, :], tmp[0:64, :], z[0:64, :] if dst is h1 else h1[0:64, :], ["fu"], [dkey],
                           "fz" if dst is h1 else "fh1")
        for I in range(nI):
            op("act", lambda e: e.activation(out=wt, in_=dl, func=AF.Exp, scale=sv[:, 8 + I:9 + I]), r=["dl", "negoff"], w=["wt"])
            for oo in range(2):
                pi = next_ps()
                op("pe", lambda e: e.matmul(PS[pi][:], h2[0:64, I * 128:(I + 1) * 128], w3[:, oo * 512:(oo + 1) * 512],
                                            start=True, stop=True), r=["fh2", "w3f"], w=[psk(pi)])
                op("dve", lambda e: e.scalar_tensor_tensor(out=htm[:, I, oo * 512:(oo + 1) * 512], in0=wt, scalar=HY_SHIFT,
                                                           op0=ALU.add, in1=PS[pi][:], op1=ALU.mult),
                   r=["wt", psk(pi)], w=[("htm", I)])
        for j in range(nJ):
            kw = kws[j]
            fc, fs, fkey = self._load_F(name, j)
            hs = hst[j % 2]
            for ri, ft in enumerate((fc, fs)):
                for oo in range(2):
                    pi = next_ps()
                    for I in range(nI):
                        op("pe", lambda e: e.matmul(PS[pi][0:kw, :], ft[:, I, 0:kw], htm[:, I, oo * 512:(oo + 1) * 512],
                                                    start=(I == 0), stop=(I == nI - 1)), r=[fkey, ("htm", I)], w=[psk(pi)])
                    if (ri + oo) % 2 == 0:
                        op("act", lambda e: e.activation(out=hs[0:kw, ri, oo, :], in_=PS[pi][0:kw, :], func=AF.Copy, scale=2.0 / N),
                           r=[psk(pi)], w=[("hst", j % 2)])
                    else:
                        op("dve", lambda e: e.tensor_scalar(out=hs[0:kw, ri, oo, :], in0=PS[pi][0:kw, :], scalar1=2.0 / N,
                                                            scalar2=None, op0=ALU.mult), r=[psk(pi)], w=[("hst", j % 2)])
            dma(Hf[j, 0:kw], hs[0:kw], r=[("hst", j % 2)], w=[("Hf" + name, j)])

    def _load_F(self, name, j):
        tb = self.tabs[name]
        nI = tb["nI"]
        si = self._slot_i
        self._slot_i = (si + 1) % 4
        key = self.slot_keys[si]
        fc = self.slots[si][:, 0:nI * 128].rearrange("p (i k) -> p i k", i=nI)
        fs = self.slots[si][:, 2048:2048 + nI * 128].rearrange("p (i k) -> p i k", i=nI)
        self.S.dma(fc, tb["Fc"][j], r=[("Fc" + name, j)], w=[key])
        self.S.dma(fs, tb["Fs"][j], r=[("Fs" + name, j)], w=[key])
        return fc, fs, key

    def _branch_b(self):
        c = self.ctx
        op, dma, PS, next_ps, psk, W, l = c["op"], c["dma"], c["PS"], c["next_ps"], c["psk"], c["W"], c["l"]
        R1, SCR0, Yv, identb = c["R1"], c["SCR0"], c["Yv"], c["identb"]
        nc = self.nc
        S = self.S
        if "f" in self.parts:
            self._filters("S")
            self._filters("P")
            S.barrier()
        if "b" not in self.parts:
            self._zero_y(1)
            return
        o = SCR0
        bufA = R1[:, o:o + 2 * T].bitcast(BF16).rearrange("p (k t) -> p k t", k=4); o += 2 * T
        bufBf = R1[:, o:o + 2 * T].bitcast(BF16); o += 2 * T
        tmv = bufBf.rearrange("p (i c) -> p i c", c=512)
        bufP = R1[:, o:o + 6144].bitcast(BF16).rearrange("p (j r c) -> p j r c", j=12, r=2); o += 6144
        assert o <= R1N
        TPAD = T + 12
        o = 0
        Cc = R1[:, o:o + TPAD]; o += TPAD
        Rr = R1[:, o:o + TPAD // 2].bitcast(BF16); o += TPAD // 2
        t12 = [R1[:, o + i * 512:o + (i + 1) * 512] for i in range(2)]; o += 1024
        assert o <= 5120
        hft = [self.wbuf[i][:, 4096:5120].rearrange("p (r c) -> p r c", r=2) for i in range(2)]
        seqs = [("S", 0, LS, 2), ("P", LS, LP, LS + 6), ("P", LS + LP, LP, LS + LP + 10)]
        sv = c["smallv"]
        with nc.allow_non_contiguous_dma(reason="small transposed vector loads"):
            for k in range(3):
                dma(sv[:, 8 + k * 12:8 + (k + 1) * 12], W["hy_conv_w"][l][k].rearrange("(m p) -> p m", p=128), w=["hyw"])
            dma(sv[:, 44:56], W["hy_conv_b"][l].rearrange("(m p) -> p m", p=128), w=["hyw"])
            for oo in range(2):
                dma(sv[:, 56 + oo * 4:60 + oo * 4], W["hy_bias"][l][oo].rearrange("(m p) -> p m", p=128), w=["hyw"])
        op("dve", lambda e: e.memset(Rr, 0.0), w=["Rr"])

        def proj_conv(part, dst, dkeyf):
            import os
            pc = int(os.environ.get("KB_PC", "9"))
            def evac(mi, tb, pi):
                m = part * 4 + mi
                segs = [(0, 512, 2 + tb * 512)] if tb < 4 else [(0, LP, LS + 6), (LP, 2 * LP, LS + LP + 10)]
                for (a, b, po) in segs:
                    op("dve", lambda e: e.tensor_copy(out=Rr[:, po:po + b - a], in_=PS[pi][:, a:b]), r=[psk(pi)], w=["Rr", ("rrseq", pi)])
                    op("act", lambda e: e.activation(out=Cc[:, po:po + b - a], in_=PS[pi][:, a:b], func=AF.Identity,
                                                     scale=sv[:, 8 + 12 + m:8 + 12 + m + 1], bias=sv[:, 44 + m:45 + m]),
                       r=[psk(pi), "hyw", ("rrseq", pi)], w=["Cc"])
                if tb == NTB - 1 and pc >= 3:
                    for (tn, t0, ln, po) in seqs:
                        op("dve", lambda e: e.scalar_tensor_tensor(out=Cc[:, po:po + ln], in0=Rr[:, po - 1:po - 1 + ln],
                                                                   scalar=sv[:, 8 + m:9 + m], op0=ALU.mult,
                                                                   in1=Cc[:, po:po + ln], op1=ALU.add),
                           r=["Rr", "Cc", "hyw"], w=["Cc"])
                        if pc >= 4:
                            op("dve", lambda e: e.scalar_tensor_tensor(out=dst[:, mi, t0:t0 + ln], in0=Rr[:, po + 1:po + 1 + ln],
                                                                   scalar=sv[:, 8 + 24 + m:8 + 24 + m + 1], op0=ALU.mult,
                                                                   in1=Cc[:, po:po + ln], op1=ALU.add),
                               r=["Rr", "Cc", "hyw"], w=[dkeyf(mi, tbb) for tbb in range(t0 // 512, (t0 + ln + 511) // 512)])
            c["proj"](C_UB + part * 512, 4, evac)

        def to_tm(src, skeyf):
            for tt in range(T // 128):
                pi = next_ps()
                pv = PS[pi][:].bitcast(BF16)
                for k in range(4):
                    op("pe", lambda e: e.transpose(pv[:, k * 128:(k + 1) * 128], src[:, k, tt * 128:(tt + 1) * 128], identb[:]),
                       r=[skeyf(k, tt // 4), "identb"], w=[psk(pi)])
                if tt % 2:
                    op("act", lambda e: e.activation(out=tmv[:, tt, :], in_=pv[:, 0:512], func=AF.Copy), r=[psk(pi)], w=[("tm", tt)])
                else:
                    op("dve", lambda e: e.tensor_copy(out=tmv[:, tt, :], in_=pv[:, 0:512]), r=[psk(pi)], w=[("tm", tt)])

        def forward(tn, t0, oo):
            tb = self.tabs[tn]
            nI, nJ, kws, Hf = tb["nI"], tb["nJ"], tb["kws"], tb["Hf"]
            I0 = t0 // 128
            for j in range(nJ):
                kw = kws[j]
                fc, fs, fkey = self._load_F(tn, j)
                hb = hft[j % 2]
                dma(hb[0:kw], Hf[j, 0:kw, :, oo, :], r=[("Hf" + tn, j)], w=[("hft", j % 2)])
                pr = next_ps()
                for I in range(nI):
                    op("pe", lambda e: e.matmul(PS[pr][0:kw, :], fc[:, I, 0:kw], tmv[:, I0 + I, :], start=(I == 0), stop=(I == nI - 1)),
                       r=[fkey, ("tm", I0 + I)], w=[psk(pr)])
                pim = next_ps()
                for I in range(nI):
                    op("pe", lambda e: e.matmul(PS[pim][0:kw, :], fs[:, I, 0:kw], tmv[:, I0 + I, :], start=(I == 0), stop=(I == nI - 1)),
                       r=[fkey, ("tm", I0 + I)], w=[psk(pim)])
                hk = [("hft", j % 2)]
                op("dve", lambda e: e.tensor_tensor(out=t12[0][0:kw], in0=PS[pr][0:kw, :], in1=hb[0:kw, 0, :], op=ALU.mult),
                   r=[psk(pr)] + hk, w=[("t12", 0)])
                op("dve", lambda e: e.tensor_tensor(out=t12[1][0:kw], in0=PS[pim][0:kw, :], in1=hb[0:kw, 1, :], op=ALU.mult),
                   r=[psk(pim)] + hk, w=[("t12", 1)])
                op("dve", lambda e: e.tensor_tensor(out=bufP[0:kw, j, 0, :], in0=t12[0][0:kw], in1=t12[1][0:kw], op=ALU.subtract),
                   r=[("t12", 0), ("t12", 1)], w=[("P", j)])
                op("dve", lambda e: e.tensor_tensor(out=t12[0][0:kw], in0=PS[pr][0:kw, :], in1=hb[0:kw, 1, :], op=ALU.mult),
                   r=[psk(pr)] + hk, w=[("t12", 0)])
                op("dve", lambda e: e.tensor_tensor(out=t12[1][0:kw], in0=PS[pim][0:kw, :], in1=hb[0:kw, 0, :], op=ALU.mult),
                   r=[psk(pim)] + hk, w=[("t12", 1)])
                op("dve", lambda e: e.tensor_tensor(out=bufP[0:kw, j, 1, :], in0=t12[0][0:kw], in1=t12[1][0:kw], op=ALU.add),
                   r=[("t12", 0), ("t12", 1)], w=[("P", j)])

        def inverse(tn, t0, evac):
            tb = self.tabs[tn]
            L, nJ, kws = tb["L"], tb["nJ"], tb["kws"]
            for b0 in range(0, L, 512):
                wd = min(512, L - b0)
                accs = [next_ps() for _ in range(4)]
                for j0 in range(0, nJ, 2):
                    si = self._slot_i
                    self._slot_i = (si + 1) % 4
                    gv = self.slots[si].rearrange("p (r j c) -> p r j c", r=2, j=2)
                    nj = min(2, nJ - j0)
                    for ri, tabn in enumerate(("Gc", "Gs")):
                        dma(gv[:, ri, 0:nj, 0:wd], tb[tabn][j0:j0 + nj, :, b0:b0 + wd].rearrange("j p c -> p j c"),
                            r=[(tabn + tn, j0 + jj) for jj in range(nj)], w=[self.slot_keys[si]])
                    for jj in range(nj):
                        j = j0 + jj
                        kw = kws[j]
                        for cc in range(4):
                            for ri in range(2):
                                op("pe", lambda e: e.matmul(PS[accs[cc]][:, 0:wd], bufP[0:kw, j, ri, cc * 128:(cc + 1) * 128],
                                                            gv[0:kw, ri, jj, 0:wd], start=(j == 0 and ri == 0),
                                                            stop=(j == nJ - 1 and ri == 1)),
                                   r=[("P", j), self.slot_keys[si]], w=[psk(accs[cc])])
                for cc in range(4):
                    evac(cc, t0 + b0, wd, accs[cc])

        kA = lambda k, tb_: ("bufA", k, tb_)
        kY = lambda k, tb_: ("y", 1, k, tb_)
        import os
        stop = int(os.environ.get("KB_STOP", "99"))
        proj_conv(0, bufA, kA)
        if stop == 1:
            S.barrier(); self._zero_y(1); return
        to_tm(bufA, kA)
        if stop == 2:
            S.barrier(); self._zero_y(1); return
        proj_conv(1, Yv[1], kY)
        if stop == 3:
            S.barrier(); self._zero_y(1); return
        if stop == 4:
            forward("S", 0, 0)
            S.barrier(); self._zero_y(1); return
        if stop == 5:
            forward("S", 0, 0)
            inverse("S", 0, lambda cc, ta, wd, pi: None)
            S.barrier(); self._zero_y(1); return

        def evac1(cc, ta, wd, pi):
            tbk = ta // 512
            ti = cc % 2
            op("dve", lambda e: e.scalar_tensor_tensor(out=t12[ti][:, 0:wd], in0=bufA[:, cc, ta:ta + wd], scalar=sv[:, 56 + cc:57 + cc],
                                                       op0=ALU.mult, in1=PS[pi][:, 0:wd], op1=ALU.add),
               r=[psk(pi), kA(cc, tbk), "hyw"], w=[("t12", ti)])
            op("dve", lambda e: e.tensor_tensor(out=Yv[1][:, cc, ta:ta + wd], in0=t12[ti][:, 0:wd], in1=Yv[1][:, cc, ta:ta + wd],
                                                 op=ALU.mult), r=[("t12", ti), kY(cc, tbk)], w=[kY(cc, tbk)])
        for (tn, t0, ln, po) in seqs:
            forward(tn, t0, 0)
            inverse(tn, t0, evac1)
        to_tm(Yv[1], kY)
        proj_conv(2, bufA, kA)

        def evac2(cc, ta, wd, pi):
            tbk = ta // 512
            ti = cc % 2
            op("dve", lambda e: e.scalar_tensor_tensor(out=t12[ti][:, 0:wd], in0=Yv[1][:, cc, ta:ta + wd], scalar=sv[:, 60 + cc:61 + cc],
                                                       op0=ALU.mult, in1=PS[pi][:, 0:wd], op1=ALU.add),
               r=[psk(pi), kY(cc, tbk), "hyw"], w=[("t12", ti)])
            op("dve", lambda e: e.tensor_tensor(out=bufA[:, cc, ta:ta + wd], in0=t12[ti][:, 0:wd], in1=bufA[:, cc, ta:ta + wd],
                                                 op=ALU.mult), r=[("t12", ti), kA(cc, tbk)], w=[kA(cc, tbk)])
        for (tn, t0, ln, po) in seqs:
            forward(tn, t0, 1)
            inverse(tn, t0, evac2)

        def evac_g(mi, tb, pi):
            ti = tb % 2
            op("act", lambda e: e.activation(out=t12[ti][:], in_=PS[pi][:], func=AF.Silu), r=[psk(pi)], w=[("t12", ti)])
            op("dve", lambda e: e.tensor_tensor(out=Yv[1][:, mi, tb * 512:(tb + 1) * 512], in0=t12[ti][:],
                                                 in1=bufA[:, mi, tb * 512:(tb + 1) * 512], op=ALU.mult),
               r=[("t12", ti), kA(mi, tb)], w=[kY(mi, tb)])
        c["proj"](C_GB, 4, evac_g)
        if "yb%d" % l in c["dbg_out"]:
            self._dump_bf16(Yv[1], c["dbg_out"]["yb%d" % l], [kY(k, tbb) for k in range(4) for tbb in range(NTB)])

    def _tmview(self, buf):
        return buf.rearrange("p k t -> p (k t)").rearrange("p (i c) -> p i c", c=512)


WEIGHT_SHAPES = {
    "norm_g": (2, 1024), "w_mod": (2, 1024, 3072), "b_mod": (2, 3072), "w_in": (2, 1024, 7168),
    "s5_lam_re": (2, 2, 32, 64), "s5_lam_im": (2, 2, 32, 64), "s5_log_dt": (2, 2, 32),
    "s5_b_re": (2, 2, 32, 64, 16), "s5_b_im": (2, 2, 32, 64, 16), "s5_c_re": (2, 2, 32, 16, 64),
    "s5_c_im": (2, 2, 32, 16, 64), "s5_d": (2, 512), "s5_w_glu": (2, 512, 512), "s5_b_glu": (2, 512),
    "hy_conv_w": (2, 3, 1536), "hy_conv_b": (2, 1536), "hy_f_w1": (2, 33, 64), "hy_f_b1": (2, 64),
    "hy_f_w2": (2, 64, 64), "hy_f_b2": (2, 64), "hy_f_freq": (2, 64), "hy_f_w3": (2, 64, 1024),
    "hy_bias": (2, 2, 512), "pool_w": (2, 4, 128, 128), "pool_scale": (2, 512),
    "w_br_a": (2, 512, 1024), "w_br_b": (2, 512, 1024), "w_br_c": (2, 512, 1024), "w_out": (2, 1024, 1024),
    "final_g": (1024,),
}

_NC_CACHE = {}


def make_in_maps(inputs):
    maps = []
    f = lambda a: np.ascontiguousarray(np.asarray(a, dtype=np.float32))
    xs, xp = f(inputs["x_sample"]), f(inputs["x_prompt"])
    cc, cctx = f(inputs["c"]), f(inputs["c_ctx"])
    sre, sim = f(inputs["state_s5_re"]), f(inputs["state_s5_im"])
    wts = {k: f(inputs[k]) for k in WEIGHT_SHAPES}
    for i in range(NCORES):
        m = dict(wts)
        m["xin"] = np.ascontiguousarray(np.concatenate([xs[i], xp[2 * i], xp[2 * i + 1]], axis=0))
        m["cond"] = np.ascontiguousarray(np.stack([cc[i], cctx], axis=0))
        m["st_re"] = sre[i]
        m["st_im"] = sim[i]
        maps.append(m)
    return maps


def run(inputs, dbg=None, parts="tpfba"):
    key = (tuple(sorted((dbg or {}).items())), parts)
    if key not in _NC_CACHE:
        _NC_CACHE[key] = Builder(dbg, parts).build()
    nc = _NC_CACHE[key]
    res = run_bass_kernel_spmd(nc, make_in_maps(inputs), core_ids=list(range(NCORES)))
    return res.results


def kernel(_parts="tpfba", **inputs):
    r = run(inputs, parts=_parts)
    y_prompt = np.empty((16, LP, D), np.float32)
    y_sample = np.empty((8, LS, D), np.float32)
    n_re = np.empty((16, DEPTH, 2, S5_G, S5_P), np.float32)
    n_im = np.empty((16, DEPTH, 2, S5_G, S5_P), np.float32)
    for i in range(NCORES):
        y = r[i]["yout"]
        y_sample[i] = y[0:LS]
        y_prompt[2 * i] = y[LS:LS + LP]
        y_prompt[2 * i + 1] = y[LS + LP:T]
        n_re[2 * i:2 * i + 2] = r[i]["ns_re"]
        n_im[2 * i:2 * i + 2] = r[i]["ns_im"]
    return (y_prompt, y_sample, n_re, n_im)
```
